# Optimizing a Trainium2 kernel written in Bass

```python
import math
import jax, jax.numpy as jnp
from jax import lax
import numpy as np

D_MODEL = 1024
BATCH = 8
SEQ = 2048
DEPTH = 2

BLOCK_Q = 128
DIFF_HEADS = 4
DIFF_QK_DIM = 32
DIFF_V_DIM = 64
FOX_HEADS = 6
FOX_HEAD_DIM = 64
MLA_HEADS = 6
MLA_Q_RANK = 256
MLA_KV_RANK = 128
MLA_NOPE_DIM = 64
MLA_ROPE_DIM = 32
MLA_V_DIM = 64
ROPE_THETA = 10000.0
REL_BUCKETS = 32
REL_MAX_DIST = 128
DIFF_WIDTH = DIFF_HEADS * DIFF_V_DIM
FOX_WIDTH = FOX_HEADS * FOX_HEAD_DIM
MLA_WIDTH = MLA_HEADS * MLA_V_DIM
MIX_WIDTH = DIFF_WIDTH + FOX_WIDTH + MLA_WIDTH
IN_SIZES = (
    DIFF_HEADS * 2 * DIFF_QK_DIM,
    DIFF_HEADS * 2 * DIFF_QK_DIM,
    DIFF_WIDTH,
    FOX_WIDTH,
    FOX_WIDTH,
    FOX_WIDTH,
    FOX_HEADS,
    MLA_Q_RANK,
    MLA_KV_RANK,
    MLA_ROPE_DIM,
)
IN_WIDTH = sum(IN_SIZES)
IN_OFFSETS = tuple(int(o) for o in np.cumsum(IN_SIZES)[:-1])
D_FF_DENSE = 2816
N_EXPERTS = 8
TOP_K = 2
D_FF_EXPERT = 3584
N_DENSE = (DEPTH + 1) // 2
N_MOE = DEPTH // 2
NORM_EPS = 1e-6

kernel_name = "hymba_style_diff_fox_mla_moe_trunk"


def _rmsnorm(x, g):
    xf = x.astype(jnp.float32)
    y = xf * lax.rsqrt(jnp.mean(xf * xf, axis=-1, keepdims=True) + NORM_EPS)
    return (y * g.astype(jnp.float32)).astype(x.dtype)


def _sweep(block_fn, seq):
    return jnp.concatenate([block_fn(i * BLOCK_Q, (i + 1) * BLOCK_Q) for i in range(seq // BLOCK_Q)], axis=1)


def _rel_dist(q0, q1):
    return (q0 + jnp.arange(q1 - q0))[:, None] - jnp.arange(q1)[None, :]


def _causal_softmax(logits, n):
    logits = jnp.where(n >= 0, logits, -jnp.inf)
    return jax.nn.softmax(logits, axis=-1)


def _t5_bucket(n):
    max_exact = REL_BUCKETS // 2
    n = jnp.maximum(n, 0)
    nf = jnp.maximum(n, 1).astype(jnp.float32)
    large = max_exact + (jnp.log(nf / max_exact) / math.log(REL_MAX_DIST / max_exact)
                         * (REL_BUCKETS - max_exact)).astype(jnp.int32)
    large = jnp.minimum(large, REL_BUCKETS - 1)
    return jnp.where(n < max_exact, n, large)


def _rope_tables(seq):
    half = MLA_ROPE_DIM // 2
    freqs = ROPE_THETA ** (-jnp.arange(half, dtype=jnp.float32) / half)
    ang = jnp.arange(seq, dtype=jnp.float32)[:, None] * freqs[None, :]
    return jnp.cos(ang), jnp.sin(ang)


def _rope(x, cos, sin):
    x1, x2 = jnp.split(x, 2, axis=-1)
    cs, sn = cos[:, None, :], sin[:, None, :]
    return jnp.concatenate([x1 * cs - x2 * sn, x1 * sn + x2 * cs], axis=-1).astype(x.dtype)


def _lambda_init(layer_idx):
    return 0.8 - 0.6 * math.exp(-0.3 * layer_idx)


def _diff_attention(q, k, v, diff_lambda, subln_g, rel_bias, lam_init):
    B, S = q.shape[0], q.shape[1]
    scale = DIFF_QK_DIM ** -0.5
    lam_p = diff_lambda.astype(jnp.float32)
    lam = jnp.exp(jnp.sum(lam_p[0] * lam_p[1])) - jnp.exp(jnp.sum(lam_p[2] * lam_p[3])) + lam_init

    def block(q0, q1):
        n = _rel_dist(q0, q1)
        logits = jnp.einsum('btmhd,bsmhd->bhmts',
                            jnp.swapaxes(q[:, q0:q1], 2, 3), jnp.swapaxes(k[:, :q1], 2, 3)).astype(jnp.float32) * scale
        bias = jnp.transpose(rel_bias[_t5_bucket(n)], (2, 0, 1)).astype(jnp.float32)
        p = _causal_softmax(logits + bias[None, :, None], n)
        w = p[:, :, 0] - lam * p[:, :, 1]
        return jnp.einsum('bhts,bshd->bthd', w.astype(v.dtype), v[:, :q1])

    o = _sweep(block, S)
    o = _rmsnorm(o, subln_g) * (1.0 - lam_init)
    return o.reshape(B, S, DIFF_WIDTH)


def _forgetting_attention(q, k, v, f_logit):
    B, S = q.shape[0], q.shape[1]
    scale = FOX_HEAD_DIM ** -0.5
    log_f = jax.nn.log_sigmoid(f_logit.astype(jnp.float32))
    F = jnp.transpose(jnp.cumsum(log_f, axis=1), (0, 2, 1))

    def block(q0, q1):
        n = _rel_dist(q0, q1)
        logits = jnp.einsum('bthd,bshd->bhts', q[:, q0:q1], k[:, :q1]).astype(jnp.float32) * scale
        decay = F[:, :, q0:q1, None] - F[:, :, None, :q1]
        p = _causal_softmax(logits + decay, n)
        return jnp.einsum('bhts,bshd->bthd', p.astype(v.dtype), v[:, :q1])

    return _sweep(block, S).reshape(B, S, FOX_WIDTH)


def _latent_attention(cq, ckv, kr, q_norm_g, kv_norm_g, w_uq, w_ukv, cos, sin):
    B, S = cq.shape[0], cq.shape[1]
    scale = (MLA_NOPE_DIM + MLA_ROPE_DIM) ** -0.5
    q = (_rmsnorm(cq, q_norm_g) @ w_uq).reshape(B, S, MLA_HEADS, MLA_NOPE_DIM + MLA_ROPE_DIM)
    q_nope, q_rope = q[..., :MLA_NOPE_DIM], _rope(q[..., MLA_NOPE_DIM:], cos, sin)
    kv = (_rmsnorm(ckv, kv_norm_g) @ w_ukv).reshape(B, S, MLA_HEADS, MLA_NOPE_DIM + MLA_V_DIM)
    k_nope, v = kv[..., :MLA_NOPE_DIM], kv[..., MLA_NOPE_DIM:]
    k_rope = _rope(kr[:, :, None, :], cos, sin)[:, :, 0]

    def block(q0, q1):
        n = _rel_dist(q0, q1)
        logits = (jnp.einsum('bthd,bshd->bhts', q_nope[:, q0:q1], k_nope[:, :q1])
                  + jnp.einsum('bthr,bsr->bhts', q_rope[:, q0:q1], k_rope[:, :q1])).astype(jnp.float32) * scale
        p = _causal_softmax(logits, n)
        return jnp.einsum('bhts,bshd->bthd', p.astype(v.dtype), v[:, :q1])

    return _sweep(block, S).reshape(B, S, MLA_WIDTH)


def _token_mixer(h, w_in, b_forget, diff_lambda, diff_subln, rel_bias, mla_q_norm, mla_kv_norm,
                 w_uq, w_ukv, w_out, cos, sin, lam_init):
    B, S, _ = h.shape
    proj = h @ w_in
    dq, dk, dv, fq, fk, fv, ff, cq, ckv, kr = jnp.split(proj, IN_OFFSETS, axis=-1)
    diff_out = _diff_attention(dq.reshape(B, S, DIFF_HEADS, 2, DIFF_QK_DIM),
                               dk.reshape(B, S, DIFF_HEADS, 2, DIFF_QK_DIM),
                               dv.reshape(B, S, DIFF_HEADS, DIFF_V_DIM),
                               diff_lambda, diff_subln, rel_bias, lam_init)
    fox_out = _forgetting_attention(fq.reshape(B, S, FOX_HEADS, FOX_HEAD_DIM),
                                    fk.reshape(B, S, FOX_HEADS, FOX_HEAD_DIM),
                                    fv.reshape(B, S, FOX_HEADS, FOX_HEAD_DIM),
                                    ff + b_forget)
    mla_out = _latent_attention(cq, ckv, kr, mla_q_norm, mla_kv_norm, w_uq, w_ukv, cos, sin)
    merged = jnp.concatenate([diff_out, fox_out, mla_out], axis=-1)
    return merged @ w_out


def _swiglu(h, w_gate, w_up, w_down):
    return (jax.nn.silu(h @ w_gate) * (h @ w_up)) @ w_down


def _moe(h, router_w, w_gate, w_up, w_down):
    B, S, D = h.shape
    t = h.reshape(B * S, D)
    logits = (t @ router_w).astype(jnp.float32)
    top_val, top_idx = lax.top_k(logits, TOP_K)
    top_w = jax.nn.softmax(top_val, axis=-1)
    combine = jnp.sum(jax.nn.one_hot(top_idx, N_EXPERTS, dtype=jnp.float32) * top_w[..., None], axis=1)
    y = jnp.zeros_like(t)
    for e in range(N_EXPERTS):
        y = y + combine[:, e:e + 1].astype(t.dtype) * _swiglu(t, w_gate[e], w_up[e], w_down[e])
    return y.reshape(B, S, D)


def setup_inputs(seed: int = 0) -> dict:
    key = jax.random.key(seed)
    ks = jax.random.split(key, 24)
    f32 = jnp.float32

    def nrm(k, shape, scale):
        return jax.random.normal(k, shape, f32) * scale

    def gain(k, shape):
        return 1.0 + 0.02 * jax.random.normal(k, shape, f32)

    D = D_MODEL
    return {
        "x": nrm(ks[0], (BATCH, SEQ, D), 1.0),
        "c": nrm(ks[1], (BATCH, D), 1.0),
        "w_ada": nrm(ks[2], (DEPTH, D, 6 * D), 0.5 * D ** -0.5),
        "b_ada": nrm(ks[3], (DEPTH, 6 * D), 0.02),
        "attn_norm": gain(ks[4], (DEPTH, D)),
        "ffn_norm": gain(ks[5], (DEPTH, D)),
        "w_in": nrm(ks[6], (DEPTH, D, IN_WIDTH), D ** -0.5),
        "b_forget": jax.random.uniform(ks[7], (DEPTH, FOX_HEADS), f32, 1.0, 4.0),
        "diff_lambda": nrm(ks[8], (DEPTH, 4, DIFF_QK_DIM), 0.1),
        "diff_subln": gain(ks[9], (DEPTH, DIFF_V_DIM)),
        "rel_bias": nrm(ks[10], (REL_BUCKETS, DIFF_HEADS), 0.5),
        "mla_q_norm": gain(ks[11], (DEPTH, MLA_Q_RANK)),
        "mla_kv_norm": gain(ks[12], (DEPTH, MLA_KV_RANK)),
        "w_uq": nrm(ks[13], (DEPTH, MLA_Q_RANK, MLA_HEADS * (MLA_NOPE_DIM + MLA_ROPE_DIM)), MLA_Q_RANK ** -0.5),
        "w_ukv": nrm(ks[14], (DEPTH, MLA_KV_RANK, MLA_HEADS * (MLA_NOPE_DIM + MLA_V_DIM)), MLA_KV_RANK ** -0.5),
        "w_out": nrm(ks[15], (DEPTH, MIX_WIDTH, D), MIX_WIDTH ** -0.5),
        "ffn_w_gate": nrm(ks[16], (N_DENSE, D, D_FF_DENSE), D ** -0.5),
        "ffn_w_up": nrm(ks[17], (N_DENSE, D, D_FF_DENSE), D ** -0.5),
        "ffn_w_down": nrm(ks[18], (N_DENSE, D_FF_DENSE, D), D_FF_DENSE ** -0.5),
        "router_w": nrm(ks[19], (N_MOE, D, N_EXPERTS), D ** -0.5),
        "moe_w_gate": nrm(ks[20], (N_MOE, N_EXPERTS, D, D_FF_EXPERT), D ** -0.5),
        "moe_w_up": nrm(ks[21], (N_MOE, N_EXPERTS, D, D_FF_EXPERT), D ** -0.5),
        "moe_w_down": nrm(ks[22], (N_MOE, N_EXPERTS, D_FF_EXPERT, D), D_FF_EXPERT ** -0.5),
        "final_norm": gain(ks[23], (D,)),
    }


def reference(x, c, w_ada, b_ada, attn_norm, ffn_norm, w_in, b_forget, diff_lambda, diff_subln,
              rel_bias, mla_q_norm, mla_kv_norm, w_uq, w_ukv, w_out, ffn_w_gate, ffn_w_up,
              ffn_w_down, router_w, moe_w_gate, moe_w_up, moe_w_down, final_norm):
    S = x.shape[1]
    cos, sin = _rope_tables(S)
    c_act = jax.nn.silu(c)
    for l in range(DEPTH):
        mod = c_act @ w_ada[l] + b_ada[l]
        sh_a, sc_a, g_a, sh_f, sc_f, g_f = [m[:, None, :] for m in jnp.split(mod, 6, axis=-1)]
        h = _rmsnorm(x, attn_norm[l]) * (1.0 + sc_a) + sh_a
        mix = _token_mixer(h, w_in[l], b_forget[l], diff_lambda[l], diff_subln[l], rel_bias,
                           mla_q_norm[l], mla_kv_norm[l], w_uq[l], w_ukv[l], w_out[l],
                           cos, sin, _lambda_init(l))
        x = x + g_a * mix
        h = _rmsnorm(x, ffn_norm[l]) * (1.0 + sc_f) + sh_f
        i = l // 2
        if l % 2 == 0:
            y = _swiglu(h, ffn_w_gate[i], ffn_w_up[i], ffn_w_down[i])
        else:
            y = _moe(h, router_w[i], moe_w_gate[i], moe_w_up[i], moe_w_down[i])
        x = x + g_f * y
    return _rmsnorm(x, final_norm)
```

```python
import math
import types
import numpy as np
from contextlib import ExitStack
import concourse.bass as bass
import concourse.mybir as mybir
from concourse.bass_utils import run_bass_kernel_spmd

F32 = mybir.dt.float32
BF16 = mybir.dt.bfloat16
ALU = mybir.AluOpType
AF = mybir.ActivationFunctionType
AX = mybir.AxisListType

D = 1024
S_LEN = 2048
NT = 16
DEPTH = 2
IN_W = 2342
EPS = 1e-6
DFF = 2816
NEXP = 8
DFE = 3584
O_DQ, O_DK, O_DV, O_FQ, O_FK, O_FV, O_FF, O_CQ, O_CKV, O_KR = 0, 256, 512, 768, 1152, 1536, 1920, 1926, 2182, 2310


class SemRef:
    def __init__(self, sem, name):
        self.sem = sem
        self.name = name
        self.count = 0


class Eng:
    def __init__(self, name, obj, semref):
        self.name = name
        self.obj = obj
        self.sr = semref
        self.seen = {}


class Dep:
    __slots__ = ("w", "r", "excl")

    def __init__(self, excl=False):
        self.w = None
        self.r = {}
        self.excl = excl


class Sched:
    def __init__(self, nc, es, n_dma_sems=32):
        self.nc = nc
        self.E = {}
        for name, obj in [("pe", nc.tensor), ("act", nc.scalar), ("dve", nc.vector),
                          ("pool", nc.gpsimd), ("sp", nc.sync)]:
            sem = es.enter_context(nc.semaphore("s_" + name))
            self.E[name] = Eng(name, obj, SemRef(sem, name))
        self.dma_sems = {q: [SemRef(es.enter_context(nc.semaphore("d%s%d" % (q, i))), "d%s%d" % (q, i))
                             for i in range(n_dma_sems if q != "act" else 8)] for q in ("sp", "pool", "act")}
        self.dma_rr = {"sp": 0, "pool": 0, "act": 0}
        self.ninst = 0

    defer = None

    def _emit(self, eng, thunk):
        if self.defer is not None:
            self.defer[eng.name].append(thunk)
        else:
            thunk()

    @staticmethod
    def _snap(fn):
        if fn.__closure__ is None:
            return fn
        cells = tuple(types.CellType(c.cell_contents) for c in fn.__closure__)
        return types.FunctionType(fn.__code__, fn.__globals__, fn.__name__, fn.__defaults__, cells)

    def _wait(self, eng, needs):
        for sr, c in needs.items():
            if c <= 0:
                continue
            if sr is eng.sr and eng.name == "pe":
                continue
            if eng.seen.get(sr, 0) >= c:
                continue
            self._emit(eng, lambda o=eng.obj, s_=sr.sem, c_=c: o.wait_ge(s_, c_))
            eng.seen[sr] = c

    @staticmethod
    def _needs(R, W):
        needs = {}
        for d in R:
            if d.w is not None:
                sr, c = d.w
                needs[sr] = max(needs.get(sr, 0), c)
        for d in W:
            if d.w is not None:
                sr, c = d.w
                needs[sr] = max(needs.get(sr, 0), c)
            for sr, c in d.r.items():
                needs[sr] = max(needs.get(sr, 0), c)
        return needs

    def op(self, ename, fn, R=(), W=(), inc=True):
        if any(d.excl for d in R):
            W = list(W) + [d for d in R if d.excl]
            R = [d for d in R if not d.excl]
        eng = self.E[ename]
        self._wait(eng, self._needs(R, W))
        self.ninst += 1
        sr = eng.sr
        if self.defer is not None:
            fn = self._snap(fn)

        def thunk(fn=fn, o=eng.obj, s_=sr.sem, inc=inc):
            ins = fn(o)
            if inc:
                ins.then_inc(s_, 1)
        self._emit(eng, thunk)
        if inc:
            sr.count += 1
            tok = sr.count
        else:
            tok = sr.count + 1
        for d in R:
            d.r[sr] = max(d.r.get(sr, 0), tok)
        for d in W:
            d.w = (sr, tok)
            d.r = {}

    def dma(self, ename, out, in_, R=(), W=(), **kw):
        eng = self.E[ename]
        pool_ = self.dma_sems[ename]
        ds = pool_[self.dma_rr[ename]]
        self.dma_rr[ename] = (self.dma_rr[ename] + 1) % len(pool_)
        needs = self._needs(R, W)
        if ds.count > 0:
            needs[ds] = max(needs.get(ds, 0), ds.count)
        self._wait(eng, needs)

        def thunk(o=eng.obj, out=out, in_=in_, kw=kw, s_=ds.sem):
            o.dma_start(out=out, in_=in_, **kw).then_inc(s_, 16)
        self._emit(eng, thunk)
        self.ninst += 1
        ds.count += 16
        tok = ds.count
        for d in R:
            d.r[ds] = tok
        for d in W:
            d.w = (ds, tok)
            d.r = {}

    def barrier(self):
        srs = [e.sr for e in self.E.values()] + self.dma_sems["sp"] + self.dma_sems["pool"] + self.dma_sems["act"]
        for eng in self.E.values():
            needs = {sr: sr.count for sr in srs if sr is not eng.sr}
            if eng.name != "pe":
                needs[eng.sr] = eng.sr.count
            self._wait(eng, needs)


def bmid(a, n):
    l = [list(v) for v in a.ap]
    return bass.AP(tensor=a.tensor, offset=a.offset, ap=[l[0], [0, n]] + l[1:])


def host_consts():
    half = 16
    freqs = (10000.0 ** (-np.arange(half, dtype=np.float32) / half)).astype(np.float32)
    t = np.arange(S_LEN, dtype=np.float32)
    ang = (t[:, None] * freqs[None, :]).astype(np.float32)
    cs = np.concatenate([np.cos(ang), np.sin(ang)], axis=1).astype(np.float32)
    cs = np.ascontiguousarray(cs.reshape(NT, 128, 32).transpose(1, 0, 2))
    n = np.arange(-127, 256)
    nn = np.maximum(n, 0)
    nf = np.maximum(nn, 1).astype(np.float32)
    large = 16 + (np.log(nf / np.float32(16)) / np.float32(math.log(128 / 16)) * np.float32(16)).astype(np.int32)
    large = np.minimum(large, 31)
    bucket = np.where(nn < 16, nn, large)
    oh = np.zeros((32, 383), np.float32)
    oh[bucket, np.arange(383)] = 1.0
    step = np.broadcast_to((n >= 0).astype(np.float32)[None, :], (4, 383)).copy()
    neg = ((step - 1.0) * 30000.0).astype(np.float32)
    return {"cs_tab": cs, "t5_oh": oh, "t5_step": step, "t5_neg": neg}


def build(stage="full"):
    nc = bass.Bass("TRN2", target_bir_lowering=False)
    dt_in = lambda name, shape: nc.dram_tensor(name, list(shape), F32, kind="ExternalInput").ap()
    x_d = dt_in("x", (S_LEN, D))
    c_d = dt_in("c", (D,))
    w_ada = dt_in("w_ada", (DEPTH, D, 6 * D))
    b_ada = dt_in("b_ada", (DEPTH, 6 * D))
    attn_norm = dt_in("attn_norm", (DEPTH, D))
    ffn_norm = dt_in("ffn_norm", (DEPTH, D))
    w_in = dt_in("w_in", (DEPTH, D, IN_W))
    b_forget = dt_in("b_forget", (DEPTH, 6))
    diff_lambda = dt_in("diff_lambda", (DEPTH, 4, 32))
    diff_subln = dt_in("diff_subln", (DEPTH, 64))
    rel_bias = dt_in("rel_bias", (32, 4))
    mla_q_norm = dt_in("mla_q_norm", (DEPTH, 256))
    mla_kv_norm = dt_in("mla_kv_norm", (DEPTH, 128))
    w_uq = dt_in("w_uq", (DEPTH, 256, 576))
    w_ukv = dt_in("w_ukv", (DEPTH, 128, 768))
    w_out = dt_in("w_out", (DEPTH, D, D))
    ffn_w_gate = dt_in("ffn_w_gate", (1, D, DFF))
    ffn_w_up = dt_in("ffn_w_up", (1, D, DFF))
    ffn_w_down = dt_in("ffn_w_down", (1, DFF, D))
    router_w = dt_in("router_w", (1, D, NEXP))
    moe_w_gate = dt_in("moe_w_gate", (1, NEXP, D, DFE))
    moe_w_up = dt_in("moe_w_up", (1, NEXP, D, DFE))
    moe_w_down = dt_in("moe_w_down", (1, NEXP, DFE, D))
    final_norm = dt_in("final_norm", (D,))
    cs_d = dt_in("cs_tab", (128, NT, 32))
    oh_d = dt_in("t5_oh", (32, 383))
    step_d = dt_in("t5_step", (4, 383))
    neg_d = dt_in("t5_neg", (4, 383))
    y_d = nc.dram_tensor("y", [S_LEN, D], F32, kind="ExternalOutput").ap()
    mod_scr = nc.dram_tensor("mod_scr", [DEPTH, 6 * D], F32, kind="Internal")
    g_scr = nc.dram_tensor("g_scr", [4, 383], F32, kind="Internal")

    es = ExitStack()
    with es:
        S = Sched(nc, es)
        op, dma = S.op, S.dma

        uniq = [0]

        def sbt(stack, name, shape, dt):
            uniq[0] += 1
            return stack.enter_context(nc.sbuf_tensor("%s_%d" % (name, uniq[0]), list(shape), dt))

        PB = [es.enter_context(nc.psum_tensor("pb%d" % i, [128, 512], F32)) for i in range(8)]
        PD = [Dep(excl=True) for _ in range(8)]

        xs = sbt(es, "xs", (128, NT, D), F32)
        XD = [Dep() for _ in range(NT)]
        hT = sbt(es, "hT", (128, 8, S_LEN), BF16)
        HDa = [Dep() for _ in range(NT)]; HDb = [Dep() for _ in range(NT)]
        class _HD:
            def __getitem__(self, k):
                if isinstance(k, slice):
                    return HDa[k] + HDb[k]
                return HDa[k]
        HD = _HD()
        identb = sbt(es, "identb", (128, 128), BF16)
        identf = sbt(es, "identf", (128, 128), F32)
        onesf = sbt(es, "onesf", (128, 128), F32)
        cs = sbt(es, "cs", (128, NT, 32), F32)
        mt5 = sbt(es, "mt5", (128, 4, 2, 128), BF16)
        negmask = sbt(es, "negmask", (128, 128), BF16)
        b31 = sbt(es, "b31", (128, 4), F32)
        cact = sbt(es, "cact", (128, 8), F32)
        cols = sbt(es, "cols", (128, 64), F32)
        amod = sbt(es, "amod", (128, 4, 8), F32)
        gab = sbt(es, "gab", (128, D), F32)
        gfb = sbt(es, "gfb", (128, D), F32)
        rstd = sbt(es, "rstd", (128, NT), F32)
        ssq = sbt(es, "ssq", (128, NT), F32)
        epsc = sbt(es, "epsc", (128, 1), F32)
        neglam = sbt(es, "neglam", (128, 1), F32)
        gsub = sbt(es, "gsub", (64, 1), F32)
        dCONST = Dep(); dCS = Dep(); dMT5 = Dep(); dB31 = Dep(); dCACT = Dep(); dCOLS = Dep()
        dAMOD = Dep(); dGAB = Dep(); dGFB = Dep(); dRSTD = Dep(); dSSQ = Dep(); dLAM = Dep(); dGSUB = Dep()
        dMODSCR = Dep(); dGSCR = Dep()

        op("pool", lambda e: e.memset(onesf[:], 1.0), W=[dCONST])
        op("pool", lambda e: e.memset(epsc[:], EPS), W=[dCONST])
        op("pool", lambda e: e.affine_select(out=identf[:], in_=onesf[:], pattern=[[-1, 128]], compare_op=ALU.is_equal,
                                             fill=0.0, base=0, channel_multiplier=1), R=[dCONST], W=[dCONST])
        op("pool", lambda e: e.affine_select(out=identb[:], in_=onesf[:], pattern=[[-1, 128]], compare_op=ALU.is_equal,
                                             fill=0.0, base=0, channel_multiplier=1), R=[dCONST], W=[dCONST])
        zf = sbt(es, "zf", (128, 128), F32)
        op("pool", lambda e: e.memset(zf[:], 0.0), W=[dCONST])
        op("pool", lambda e: e.affine_select(out=negmask[:], in_=zf[:], pattern=[[1, 128]], compare_op=ALU.is_ge,
                                             fill=-30000.0, base=0, channel_multiplier=-1), R=[dCONST], W=[dCONST])
        dma("sp", cs[:], cs_d, W=[dCS])
        dma("sp", b31[:], rel_bias[31, :].partition_broadcast(128), W=[dB31])
        xv = x_d.rearrange("(i p) f -> p i f", p=128)
        for q in range(4):
            dma("sp", xs[:, 4 * q:4 * q + 4, :], xv[:, 4 * q:4 * q + 4, :], W=XD[4 * q:4 * q + 4])
        with ExitStack() as s0:
            crow = sbt(s0, "crow", (8, 128), F32); dcrow = Dep()
            jmat = sbt(s0, "jmat", (128, 128), F32)
            rbs = sbt(s0, "rbs", (32, 4), F32)
            ohs = sbt(s0, "ohs", (32, 383), F32)
            stp = sbt(s0, "stp", (4, 383), F32)
            nb31c = sbt(s0, "nb31c", (4, 1), F32)
            gsb = sbt(s0, "gsb", (4, 383), F32)
            hank = sbt(s0, "hank", (128, 2, 128), F32)
            dT5 = Dep(); dHK = [Dep(), Dep()]
            dma("sp", crow[:], c_d.rearrange("(k p) -> k p", p=128), W=[dcrow])
            op("pe", lambda e: e.transpose(out=PB[0][:, 0:8], in_=crow[:], identity=identf[0:8, 0:8]), R=[dcrow, dCONST], W=[PD[0]])
            op("act", lambda e: e.activation(out=cact[:], in_=PB[0][:, 0:8], func=AF.Silu), R=[PD[0]], W=[dCACT])
            op("pool", lambda e: e.affine_select(out=jmat[:], in_=onesf[:], pattern=[[1, 128]], compare_op=ALU.is_equal,
                                                 fill=0.0, base=-127, channel_multiplier=1), R=[dCONST], W=[dT5])
            dma("sp", rbs[:], rel_bias, W=[dT5])
            dma("sp", ohs[:], oh_d, W=[dT5])
            dma("sp", stp[:], step_d, W=[dT5])
            dma("sp", nb31c[:], rel_bias[31:32, :].rearrange("a h -> h a"), W=[dT5], allow_slow_non_contiguous=True)
            S32 = 32 ** 0.5
            ngt = sbt(s0, "ngt", (4, 383), F32)
            dma("sp", ngt[:], neg_d, W=[dT5])
            op("dve", lambda e: e.tensor_scalar(out=nb31c[:], in0=nb31c[:], scalar1=-S32, scalar2=None, op0=ALU.mult), R=[dT5], W=[dT5])
            op("pe", lambda e: e.matmul(PB[1][0:4, 0:383], lhsT=rbs[:, :], rhs=ohs[:, :], start=True, stop=True), R=[dT5], W=[PD[1]])
            op("act", lambda e: e.activation(out=gsb[:], in_=PB[1][0:4, 0:383], func=AF.Identity, bias=nb31c[:, 0:1], scale=S32), R=[PD[1], dT5], W=[dT5])
            op("dve", lambda e: e.tensor_tensor(out=gsb[:], in0=gsb[:], in1=stp[:], op=ALU.mult), R=[dT5], W=[dT5])
            op("dve", lambda e: e.tensor_tensor(out=gsb[:], in0=gsb[:], in1=ngt[:], op=ALU.add), R=[dT5], W=[dT5])
            dma("sp", g_scr.ap(), gsb[:], R=[dT5], W=[dGSCR])
            for h in range(4):
                for dl in range(2):
                    hk = dHK[dl]
                    dma("sp", hank[:, dl, :], bass.AP(tensor=g_scr, offset=h * 383 + 128 * dl, ap=[[1, 128], [1, 128]]),
                        R=[dGSCR], W=[hk])
                    pb = 2 + dl
                    op("pe", lambda e: e.matmul(PB[pb][:, 0:128], lhsT=jmat[:, :], rhs=hank[:, dl, :], start=True, stop=True),
                       R=[dT5, hk], W=[PD[pb]])
                    op("act", lambda e: e.activation(out=mt5[:, h, dl, :], in_=PB[pb][:, 0:128], func=AF.Identity), R=[PD[pb]], W=[dMT5])
            S.barrier()

        def adaln(l):
            with ExitStack() as s1:
                wb = [sbt(s1, "adaw%d" % i, (128, 3072), F32) for i in range(5)]
                dwb = [Dep() for _ in range(5)]
                brow = sbt(s1, "brow", (1, 6 * D), F32); dbrow = Dep()
                mrow = [sbt(s1, "mrow%d" % i, (1, 512), F32) for i in range(2)]
                dmrow = [Dep(), Dep()]
                rows = sbt(s1, "rows", (64, 128), F32); drows = Dep()
                dma("sp", brow[:], b_ada[l:l + 1, :], W=[dbrow])
                nb = 0
                for half in range(2):
                    for k in range(8):
                        b = nb % 5
                        nb += 1
                        dma(("sp", "pool", "act")[nb % 3], wb[b][:], w_ada[l, k * 128:(k + 1) * 128, half * 3072:(half + 1) * 3072], W=[dwb[b]])
                        for cb in range(6):
                            op("pe", lambda e: e.matmul(PB[cb][0:1, :], lhsT=cact[:, k:k + 1], rhs=wb[b][:, cb * 512:(cb + 1) * 512],
                                                        start=(k == 0), stop=(k == 7)),
                               R=[dCACT, dwb[b]], W=[PD[cb]], inc=(k == 7 or cb == 5))
                    for cb in range(6):
                        cc = half * 6 + cb
                        m_ = cb % 2
                        op("dve", lambda e: e.tensor_tensor(out=mrow[m_][:], in0=PB[cb][0:1, :], in1=brow[0:1, cc * 512:(cc + 1) * 512],
                                                            op=ALU.add), R=[PD[cb], dbrow], W=[dmrow[m_]])
                        dma("sp", mod_scr.ap()[l:l + 1, cc * 512:(cc + 1) * 512], mrow[m_][:], R=[dmrow[m_]], W=[dMODSCR])
                dma("sp", rows[0:48, :], mod_scr.ap()[l, :].rearrange("(j p) -> j p", p=128), R=[dMODSCR], W=[drows])
                dma("sp", rows[48:56, :], attn_norm[l, :].rearrange("(j p) -> j p", p=128), W=[drows])
                dma("sp", rows[56:64, :], ffn_norm[l, :].rearrange("(j p) -> j p", p=128), W=[drows])
                op("pe", lambda e: e.transpose(out=PB[2][:, 0:64], in_=rows[:, :], identity=identf[0:64, 0:64]),
                   R=[drows, dCONST], W=[PD[2]])
                op("act", lambda e: e.activation(out=cols[:], in_=PB[2][:, 0:64], func=AF.Identity), R=[PD[2]], W=[dCOLS])
                op("dve", lambda e: e.scalar_tensor_tensor(out=amod[:, 0, :], in0=cols[:, 8:16], scalar=1.0, in1=cols[:, 48:56],
                                                           op0=ALU.add, op1=ALU.mult), R=[dCOLS], W=[dAMOD])
                op("dve", lambda e: e.tensor_copy(out=amod[:, 1, :], in_=cols[:, 0:8]), R=[dCOLS], W=[dAMOD])
                op("dve", lambda e: e.scalar_tensor_tensor(out=amod[:, 2, :], in0=cols[:, 32:40], scalar=1.0, in1=cols[:, 56:64],
                                                           op0=ALU.add, op1=ALU.mult), R=[dCOLS], W=[dAMOD])
                op("dve", lambda e: e.tensor_copy(out=amod[:, 3, :], in_=cols[:, 24:32]), R=[dCOLS], W=[dAMOD])
                dma("sp", gab[:], mod_scr.ap()[l, 2 * D:3 * D].partition_broadcast(128), R=[dMODSCR], W=[dGAB])
                dma("sp", gfb[:], mod_scr.ap()[l, 5 * D:6 * D].partition_broadcast(128), R=[dMODSCR], W=[dGFB])
                S.barrier()

        def norm_phase(stack, which, fp32_router=None):
            ia, ish = 2 * which, 2 * which + 1
            with ExitStack() as s1:
                junk = sbt(s1, "junk", (128, D), BF16); djunk = Dep()
                if fp32_router is None:
                    xn = [sbt(s1, "xn%d" % i, (128, D), BF16) for i in range(2)]
                else:
                    xn = [sbt(s1, "xn%d" % i, (128, D), F32) for i in range(2)]
                    h2f = [sbt(s1, "h2f%d" % i, (128, 8, 128), F32) for i in range(2)]
                    dh2f = [Dep(), Dep()]
                    rws, drws, logits, dlog = fp32_router
                dxn = [Dep(), Dep()]
                op("dve", lambda e: e.memset(ssq[:], 0.0), W=[dSSQ])
                for i in range(NT):
                    op("act", lambda e: e.activation(out=junk[:], in_=xs[:, i, :], func=AF.Square, accum_out=ssq[:, i:i + 1]),
                       R=[XD[i]], W=[djunk, dSSQ])
                op("act", lambda e: e.activation(out=rstd[:], in_=ssq[:], func=AF.Ln, bias=epsc[:, 0:1], scale=1.0 / D),
                   R=[dSSQ, dCONST], W=[dRSTD])
                op("act", lambda e: e.activation(out=rstd[:], in_=rstd[:], func=AF.Exp, scale=-0.5), R=[dRSTD], W=[dRSTD])
                for i in range(NT):
                    b = i % 2
                    op("dve", lambda e: e.tensor_scalar(out=xn[b][:], in0=xs[:, i, :], scalar1=rstd[:, i:i + 1], scalar2=None,
                                                        op0=ALU.mult), R=[XD[i], dRSTD], W=[dxn[b]])
                    if fp32_router is None:
                        pb = i % 2
                        pv = PB[pb][:, :].bitcast(BF16)
                        for k in range(8):
                            op("pe", lambda e: e.transpose(out=pv[:, k * 128:(k + 1) * 128], in_=xn[b][:, k * 128:(k + 1) * 128],
                                                           identity=identb[:]), R=[dxn[b], dCONST], W=[PD[pb]], inc=(k == 7))
                        for k in range(8):
                            if i % 2 == 0:
                                op("act", lambda e: e.activation(out=hT[:, k, i * 128:(i + 1) * 128], in_=pv[:, k * 128:(k + 1) * 128],
                                                                 func=AF.Identity, scale=amod[:, ia, k:k + 1], bias=amod[:, ish, k:k + 1]),
                                   R=[PD[pb], dAMOD], W=[HDa[i]])
                            else:
                                op("dve", lambda e: e.tensor_scalar(out=hT[:, k, i * 128:(i + 1) * 128], in0=pv[:, k * 128:(k + 1) * 128],
                                                                    scalar1=amod[:, ia, k:k + 1], scalar2=amod[:, ish, k:k + 1],
                                                                    op0=ALU.mult, op1=ALU.add), R=[PD[pb], dAMOD], W=[HDb[i]])
                    else:
                        for half in range(2):
                            pb = half
                            for kk in range(4):
                                k = half * 4 + kk
                                op("pe", lambda e: e.transpose(out=PB[pb][:, kk * 128:(kk + 1) * 128], in_=xn[b][:, k * 128:(k + 1) * 128],
                                                               identity=identf[:]), R=[dxn[b], dCONST], W=[PD[pb]], inc=(kk == 3))
                            for kk in range(4):
                                k = half * 4 + kk
                                op("act", lambda e: e.activation(out=h2f[b][:, k, :], in_=PB[pb][:, kk * 128:(kk + 1) * 128],
                                                                 func=AF.Identity, scale=amod[:, ia, k:k + 1], bias=amod[:, ish, k:k + 1]),
                                   R=[PD[pb], dAMOD], W=[dh2f[b]])
                        op("dve", lambda e: e.tensor_copy(out=hT[:, :, i * 128:(i + 1) * 128], in_=h2f[b][:, :, :]), R=[dh2f[b]], W=[HDa[i], HDb[i]])
                        for k in range(8):
                            op("pe", lambda e: e.matmul(PB[2][:, 0:8], lhsT=h2f[b][:, k, :], rhs=rws[:, k, :], start=(k == 0), stop=(k == 7)),
                               R=[dh2f[b], drws], W=[PD[2]], inc=(k == 7))
                        op("dve", lambda e: e.tensor_copy(out=logits[:, i, :], in_=PB[2][:, 0:8]), R=[PD[2]], W=[dlog])
                S.barrier()

        def attention(l):
            lam_init = 0.8 - 0.6 * math.exp(-0.3 * l)
            win_v = w_in[l].rearrange("(k p) n -> p k n", p=128)
            with ExitStack() as s1:
                KT = sbt(s1, "KT", (128, 3, S_LEN), BF16); dKT = [[Dep() for _ in range(4)] for _ in range(3)]
                QT = sbt(s1, "QT", (128, 3, 2, 512), BF16); dQT = [[Dep(), Dep()] for _ in range(3)]
                V = sbt(s1, "V", (128, NT, 3, 65), BF16); dV = [Dep() for _ in range(NT)]
                OT = sbt(s1, "OT", (64, 3, 512), BF16); dOT = [Dep() for _ in range(3)]
                PT = [sbt(s1, "PT%d" % i, (128, 512), BF16) for i in range(4)]; dPT = [Dep() for _ in range(4)]
                wkv = sbt(s1, "wkv", (128, 8, 387), BF16); dwkv = Dep()
                wq = sbt(s1, "wq", (128, 8, 256), BF16); dwq = Dep()
                wo = sbt(s1, "wo", (64, 3, D), BF16); dwo = Dep()
                fa = [sbt(s1, "fa%d" % i, (128, 512), F32) for i in range(6)]; dfa = [Dep() for _ in range(6)]
                dl_b = sbt(s1, "dl_b", (128, 128), F32); ddl = Dep()
                lt = sbt(s1, "lt", (128, 8), F32)
                negb = sbt(s1, "negb", (3, 1), F32); dnegb = Dep()
                ftok = sbt(s1, "ftok", (128, NT, 3), F32); dftok = Dep()
                cb = sbt(s1, "cb", (128, NT, 3), F32); dcb = Dep()
                biask = sbt(s1, "biask", (128, 3, 4, NT), F32); dbk = Dep()
                carry = sbt(s1, "carry", (3, 1), F32); dcarry = Dep()
                ones3 = sbt(s1, "ones3", (3, 512), F32)
                gq_f = sbt(s1, "gq_f", (3, S_LEN), BF16); dgqf = Dep()
                selg = sbt(s1, "selg", (3, 3, 65), BF16)
                wukv_f = sbt(s1, "wukv_f", (128, 768), F32); wukv_b = sbt(s1, "wukv_b", (128, 768), BF16); dwukv = Dep()
                wuq_f = sbt(s1, "wuq_f", (128, 2, 576), F32); wuq_b = sbt(s1, "wuq_b", (128, 2, 576), BF16); dwuq = Dep()
                gq = sbt(s1, "gq", (128, 2), F32); gkv = sbt(s1, "gkv", (128, 1), F32); dgn = Dep()
                ckvT = sbt(s1, "ckvT", (128, 512), BF16); dckvT = Dep()
                cqT = sbt(s1, "cqT", (128, 2, 512), BF16); dcqT = Dep()
                rtok = sbt(s1, "rtok", (128, 4), F32); drtok = Dep()
                krp4 = sbt(s1, "krp4", (128, 4, 96), BF16); dkrp = Dep()
                rk = [sbt(s1, "rk%d" % i, (128, 4, 16), F32) for i in range(4)]
                qsb4 = sbt(s1, "qsb4", (128, 4, 3, 96), F32)
                qrot4 = sbt(s1, "qrot4", (128, 4, 3, 96), BF16)
                rq = [sbt(s1, "rq%d" % i, (128, 4, 3, 16), F32) for i in range(4)]
                dqsb = Dep(); dqrot = Dep(); drt = Dep()

                op("pool", lambda e: e.memset(V[:, :, :, 64:65], 1.0), W=dV)
                op("pool", lambda e: e.memset(krp4[:], 0.0), W=[dkrp])
                op("pool", lambda e: e.memset(ones3[:], 1.0), W=[dcarry])
                op("pool", lambda e: e.memset(selg[:], 0.0), W=[dgqf])
                for hl_ in range(3):
                    op("pool", lambda e: e.tensor_copy(out=selg[0:3, hl_, 64:65], in_=identf[0:3, hl_:hl_ + 1]), R=[dCONST], W=[dgqf])
                dma("sp", dl_b[:], diff_lambda[l].rearrange("a b -> (a b)").partition_broadcast(128), W=[ddl])
                op("dve", lambda e: e.tensor_tensor(out=dl_b[:, 0:32], in0=dl_b[:, 0:32], in1=dl_b[:, 32:64], op=ALU.mult), R=[ddl], W=[ddl])
                op("dve", lambda e: e.tensor_tensor(out=dl_b[:, 64:96], in0=dl_b[:, 64:96], in1=dl_b[:, 96:128], op=ALU.mult), R=[ddl], W=[ddl])
                op("dve", lambda e: e.reduce_sum(out=lt[:, 0:1], in_=dl_b[:, 0:32], axis=AX.X), R=[ddl], W=[ddl])
                op("dve", lambda e: e.reduce_sum(out=lt[:, 1:2], in_=dl_b[:, 64:96], axis=AX.X), R=[ddl], W=[ddl])
                op("act", lambda e: e.activation(out=lt[:, 2:4], in_=lt[:, 0:2], func=AF.Exp), R=[ddl], W=[ddl])
                op("dve", lambda e: e.tensor_tensor(out=lt[:, 4:5], in0=lt[:, 3:4], in1=lt[:, 2:3], op=ALU.subtract), R=[ddl], W=[ddl])
                op("dve", lambda e: e.tensor_scalar(out=neglam[:], in0=lt[:, 4:5], scalar1=-lam_init, scalar2=None, op0=ALU.add), R=[ddl], W=[dLAM])
                dma("sp", gsub[:], diff_subln[l:l + 1, :].rearrange("a d -> d a"), W=[dGSUB], allow_slow_non_contiguous=True)
                op("dve", lambda e: e.tensor_scalar(out=gsub[:], in0=gsub[:], scalar1=1.0 - lam_init, scalar2=None, op0=ALU.mult), R=[dGSUB], W=[dGSUB])
                dma("sp", gq[:], mla_q_norm[l, :].rearrange("(r p) -> p r", p=128), W=[dgn], allow_slow_non_contiguous=True)
                dma("sp", gkv[:], mla_kv_norm[l:l + 1, :].rearrange("a p -> p a"), W=[dgn], allow_slow_non_contiguous=True)

                state = {"s": 0, "p": 0, "a": 0, "f": 0, "x": 0}

                fring = [0, 6]

                def nxt(key, n, base):
                    if key == "f":
                        base, n = fring
                    v = base + state[key] % n
                    state[key] += 1
                    return v

                pending = []

                def pop_pending(all_=False):
                    if all_:
                        while pending:
                            pending.pop(0)[1]()
                        return
                    if pending:
                        pending[0][0] -= 1
                        if pending[0][0] <= 0:
                            pending.pop(0)[1]()

                def normalize_to(acc_pb, dest_fn, last=False):
                    f1 = nxt("f", 6, 0)
                    if last:
                        op("act", lambda e: e.activation(out=fa[f1][64:65, :], in_=PB[acc_pb][64:65, :], func=AF.Ln), R=[PD[acc_pb]], W=[dfa[f1]])
                        op("act", lambda e: e.activation(out=fa[f1][64:65, :], in_=fa[f1][64:65, :], func=AF.Exp, scale=-1.0), R=[dfa[f1]], W=[dfa[f1]])
                    else:
                        op("dve", lambda e: e.reciprocal(out=fa[f1][64:65, :], in_=PB[acc_pb][64:65, :]), R=[PD[acc_pb]], W=[dfa[f1]])
                    f2 = nxt("f", 6, 0)

                    def stage_b():
                        op("pe", lambda e: e.matmul(PB[7][0:64, :], lhsT=onesf[64:65, 0:64], rhs=fa[f1][64:65, :], start=True, stop=True),
                           R=[dCONST, dfa[f1]], W=[PD[7]])
                        op("dve", lambda e: e.tensor_copy(out=fa[f2][0:64, :], in_=PB[7][0:64, :]), R=[PD[7]], W=[dfa[f2]])
                        dest_fn(acc_pb, f2)
                    pending.append([2 if last else 4, stage_b])

                def attn_chunk(c, maps):
                    nj = 4 * c + 4
                    seq = [(mi, j) for mi in range(len(maps)) for j in range(nj)]
                    LA = 2
                    sbank = {}
                    accs = {}

                    def qk(idx):
                        mi, j = seq[idx]
                        M = maps[mi]
                        r = j - 4 * c
                        lo = max(0, r) * 128
                        sb_ = nxt("s", 3, 2)
                        sbank[idx] = sb_
                        fx = M["fix"](r)
                        op("pe", lambda e: e.matmul(PB[sb_][:, lo:512], lhsT=M["kfn"](j), rhs=M["qfn"](lo), start=True, stop=(len(fx) == 0)),
                           R=[M["kdeps"][j // 4], M["qdep"]], W=[PD[sb_]], inc=(len(fx) == 0))
                        for n_, (rr, tile_ap) in enumerate(fx):
                            op("pe", lambda e: e.matmul(PB[sb_][:, rr * 128:(rr + 1) * 128], lhsT=identb[:, :], rhs=tile_ap,
                                                        start=False, stop=(n_ == len(fx) - 1)),
                               R=[dCONST, dMT5], W=[PD[sb_]], inc=(n_ == len(fx) - 1))
                    for idx in range(min(LA, len(seq))):
                        qk(idx)
                    for idx, (mi, j) in enumerate(seq):
                        M = maps[mi]
                        if j == 0:
                            accs[mi] = nxt("a", 2, 5)
                        acc = accs[mi]
                        r = j - 4 * c
                        lo = max(0, r) * 128
                        p = nxt("p", 4, 0)
                        M["act_fn"](j, p, sbank[idx], lo)
                        if idx + LA < len(seq):
                            qk(idx + LA)
                        op("pe", lambda e: e.matmul(PB[acc][0:65, lo:512], lhsT=V[:, j, M["hl"], :], rhs=PT[p][:, lo:512],
                                                    start=(j == 0), stop=(j == nj - 1)),
                           R=[dV[j], dPT[p]], W=[PD[acc]])
                        if j == nj - 1:
                            M["done"](acc, mi == len(maps) - 1)
                        pop_pending()

                def wout_group(c, nh, t, half):
                    pb = nxt("x", 2, 0)
                    for hl in range(nh):
                        op("pe", lambda e: e.matmul(PB[pb][:, :], lhsT=OT[0:64, hl, t * 128:(t + 1) * 128],
                                                    rhs=wo[0:64, hl, half * 512:(half + 1) * 512], start=(hl == 0), stop=(hl == nh - 1)),
                           R=[dOT[hl], dwo], W=[PD[pb]], inc=(hl == nh - 1))
                    i = 4 * c + t
                    op("dve", lambda e: e.tensor_tensor(out=xs[:, i, half * 512:(half + 1) * 512], in0=PB[pb][:, :],
                                                        in1=xs[:, i, half * 512:(half + 1) * 512], op=ALU.add),
                       R=[PD[pb]], W=[XD[i]])

                def wout_phase(c, nh):
                    for t in range(4):
                        for half in range(2):
                            if c < 3:
                                pending.append([1, lambda c=c, nh=nh, t=t, half=half: wout_group(c, nh, t, half)])
                            else:
                                wout_group(c, nh, t, half)

                def load_wo(row0, nh):
                    dma("pool", wo[0:64, 0:nh, :], w_out[l, row0:row0 + 64 * nh, :].rearrange("(h d) n -> d h n", d=64), W=[dwo])
                    for hl in range(nh):
                        op("pool", lambda e: e.tensor_tensor(out=wo[0:64, hl, :], in0=wo[0:64, hl, :], in1=gab[0:64, :], op=ALU.mult),
                           R=[dGAB], W=[dwo])

                def proj_fm(pb, wt, dw, col0, m, c):
                    for k in range(8):
                        op("pe", lambda e: e.matmul(PB[pb][0:m, :], lhsT=wt[:, k, col0:col0 + m], rhs=hT[:, k, c * 512:(c + 1) * 512],
                                                    start=(k == 0), stop=(k == 7)),
                           R=[dw] + HD[4 * c:4 * c + 4], W=[PD[pb]], inc=(k == 7))

                def proj_tm(pb, wt, dw, col0, n, i):
                    for k in range(8):
                        op("pe", lambda e: e.matmul(PB[pb][:, 0:n], lhsT=hT[:, k, i * 128:(i + 1) * 128], rhs=wt[:, k, col0:col0 + n],
                                                    start=(k == 0), stop=(k == 7)),
                           R=[dw] + HD[i:i + 1], W=[PD[pb]], inc=(k == 7))

                def evac(eng, out, in_, R, W):
                    if eng == "act":
                        op("act", lambda e: e.activation(out=out, in_=in_, func=AF.Identity), R=R, W=W)
                    else:
                        op(eng, lambda e: e.tensor_copy(out=out, in_=in_), R=R, W=W)

                def causal_fix(r):
                    return [(r, negmask[:, :])] if r >= 0 else []

                for sg in range(2):
                    heads = [2 * sg, 2 * sg + 1]
                    dma("pool", wkv[:, :, 0:128], win_v[:, :, O_DK + 128 * sg:O_DK + 128 * sg + 128], W=[dwkv])
                    dma("pool", wkv[:, :, 128:256], win_v[:, :, O_DV + 128 * sg:O_DV + 128 * sg + 128], W=[dwkv])
                    dma("pool", wq[:, :, 0:128], win_v[:, :, O_DQ + 128 * sg:O_DQ + 128 * sg + 128], W=[dwq])
                    load_wo(128 * sg, 2)
                    for c in range(4):
                        for hl in range(2):
                            pbx = nxt("x", 2, 0)
                            proj_fm(pbx, wkv, dwkv, 64 * hl, 64, c)
                            evac("dve", KT[0:64, hl, c * 512:(c + 1) * 512], PB[pbx][0:64, :], [PD[pbx]], [dKT[hl][c]])
                        for t in range(4):
                            i = 4 * c + t
                            pby = nxt("s", 3, 2)
                            proj_tm(pby, wkv, dwkv, 128, 128, i)
                            op("act", lambda e: e.activation(out=V[:, i, 0:2, 0:64], in_=PB[pby][:, 0:128].rearrange("p (h d) -> p h d", h=2), func=AF.Identity),
                               R=[PD[pby]], W=[dV[i]])
                    def qprep(c):
                        for hl in range(2):
                            pbx = nxt("x", 2, 0)
                            proj_fm(pbx, wq, dwq, 64 * hl, 64, c)
                            evac("dve", QT[0:64, hl, c % 2, :], PB[pbx][0:64, :], [PD[pbx]], [dQT[hl][c % 2]])
                            pop_pending()
                    qprep(0)
                    for c in range(4):
                        maps = []
                        for hl in range(2):
                            h = heads[hl]
                            om = []
                            for m in range(2):
                                def act_fn(j, p, sb_, lo, h=h):
                                    op("act", lambda e: e.activation(out=PT[p][:, lo:512], in_=PB[sb_][:, lo:512], func=AF.Exp,
                                                                     bias=b31[:, h:h + 1], scale=32 ** -0.5),
                                       R=[PD[sb_], dB31], W=[dPT[p]])

                                def fix_fn(r, h=h):
                                    return [(r + dlt, mt5[:, h, dlt, :]) for dlt in range(2) if 0 <= r + dlt <= 3]

                                def done(acc, last, m=m, hl=hl, om=om):
                                    fo = nxt("f", 6, 0)

                                    def dest(acc_pb, f2, fo=fo):
                                        op("dve", lambda e: e.tensor_tensor(out=fa[fo][0:64, :], in0=PB[acc_pb][0:64, :], in1=fa[f2][0:64, :],
                                                                            op=ALU.mult), R=[PD[acc_pb], dfa[f2]], W=[dfa[fo]])
                                    normalize_to(acc, dest, last)
                                    om.append(fo)
                                    if m == 0:
                                        return
                                    fo = nxt("f", 6, 0)
                                    while fo in om:
                                        fo = nxt("f", 6, 0)
                                    fs = nxt("f", 6, 0)
                                    while fs in om or fs == fo:
                                        fs = nxt("f", 6, 0)

                                    def stage_d(om=om, fo=fo, fs=fs):
                                        op("dve", lambda e: e.scalar_tensor_tensor(out=fa[fo][0:64, :], in0=fa[om[1]][0:64, :], scalar=neglam[0:64, 0:1],
                                                                                   in1=fa[om[0]][0:64, :], op0=ALU.mult, op1=ALU.add),
                                           R=[dfa[om[0]], dfa[om[1]], dLAM], W=[dfa[fo]])
                                        op("act", lambda e: e.activation(out=fa[fs][0:64, :], in_=fa[fo][0:64, :], func=AF.Square), R=[dfa[fo]], W=[dfa[fs]])

                                    def stage_e(fo=fo, fs=fs, hl=hl):
                                        op("pe", lambda e: e.matmul(PB[7][0:64, :], lhsT=onesf[0:64, 0:64], rhs=fa[fs][0:64, :], start=True, stop=True),
                                           R=[dCONST, dfa[fs]], W=[PD[7]])
                                        op("act", lambda e: e.activation(out=fa[fs][0:64, :], in_=PB[7][0:64, :], func=AF.Ln, bias=epsc[0:64, 0:1],
                                                                         scale=1.0 / 64), R=[PD[7], dCONST], W=[dfa[fs]])
                                        op("act", lambda e: e.activation(out=fa[fs][0:64, :], in_=fa[fs][0:64, :], func=AF.Exp, scale=-0.5), R=[dfa[fs]], W=[dfa[fs]])
                                        op("dve", lambda e: e.scalar_tensor_tensor(out=OT[0:64, hl, :], in0=fa[fo][0:64, :], scalar=gsub[0:64, 0:1],
                                                                                   in1=fa[fs][0:64, :], op0=ALU.mult, op1=ALU.mult),
                                           R=[dfa[fo], dfa[fs], dGSUB], W=[dOT[hl]])
                                    pending.append([1, stage_d])
                                    pending.append([2, stage_e])
                                maps.append(dict(kfn=lambda j, m=m, hl=hl: KT[32 * m:32 * m + 32, hl, j * 128:(j + 1) * 128], kdeps=dKT[hl],
                                                 qfn=lambda lo, m=m, hl=hl, c=c: QT[32 * m:32 * m + 32, hl, c % 2, lo:512], qdep=dQT[hl][c % 2],
                                                 hl=hl, act_fn=act_fn, fix=fix_fn, done=done))
                        attn_chunk(c, maps)
                        if c < 3:
                            qprep(c + 1)
                        pop_pending(all_=True)
                        wout_phase(c, 2)
                if stage == "diff":
                    S.barrier()
                    return

                for sg in range(2):
                    h0 = 3 * sg
                    dma("pool", wkv[:, :, 0:192], win_v[:, :, O_FK + 64 * h0:O_FK + 64 * h0 + 192], W=[dwkv])
                    dma("pool", wkv[:, :, 192:384], win_v[:, :, O_FV + 64 * h0:O_FV + 64 * h0 + 192], W=[dwkv])
                    dma("pool", wkv[:, :, 384:387], win_v[:, :, O_FF + h0:O_FF + h0 + 3], W=[dwkv])
                    op("pool", lambda e: e.memset(wq[:, :, :], 0.0), W=[dwq])
                    for hl in range(3):
                        dma("pool", wq[:, :, 65 * hl:65 * hl + 64], win_v[:, :, O_FQ + 64 * (h0 + hl):O_FQ + 64 * (h0 + hl) + 64], W=[dwq])
                    load_wo(256 + 64 * h0, 3)
                    dma("sp", negb[:], b_forget[l:l + 1, h0:h0 + 3].rearrange("a h -> h a"), W=[dnegb], allow_slow_non_contiguous=True)
                    op("dve", lambda e: e.tensor_scalar(out=negb[:], in0=negb[:], scalar1=-1.0, scalar2=None, op0=ALU.mult), R=[dnegb], W=[dnegb])
                    for hl in range(3):
                        op("pool", lambda e: e.memset(KT[64:65, hl, :], 1.0), W=dKT[hl])
                    for c in range(4):
                        for hl in range(3):
                            pbx = nxt("x", 2, 0)
                            proj_fm(pbx, wkv, dwkv, 64 * hl, 64, c)
                            evac("dve", KT[0:64, hl, c * 512:(c + 1) * 512], PB[pbx][0:64, :], [PD[pbx]], [dKT[hl][c]])
                        for t in range(4):
                            i = 4 * c + t
                            pby = nxt("s", 3, 2)
                            proj_tm(pby, wkv, dwkv, 192, 192, i)
                            op("act", lambda e: e.activation(out=V[:, i, 0:3, 0:64], in_=PB[pby][:, 0:192].rearrange("p (h d) -> p h d", h=3), func=AF.Identity),
                               R=[PD[pby]], W=[dV[i]])
                        proj_fm(0, wkv, dwkv, 384, 3, c)
                        op("act", lambda e: e.activation(out=fa[0][0:3, :], in_=PB[0][0:3, :], func=AF.Exp, bias=negb[:, 0:1], scale=-1.0),
                           R=[PD[0], dnegb], W=[dfa[0]])
                        op("act", lambda e: e.activation(out=fa[1][0:3, :], in_=fa[0][0:3, :], func=AF.Ln, bias=1.0), R=[dfa[0]], W=[dfa[1]])
                        if c == 0:
                            op("dve", lambda e: e.tensor_tensor_scan(out=fa[2][0:3, :], data0=ones3[:, :], data1=fa[1][0:3, :], initial=0.0,
                                                                     op0=ALU.mult, op1=ALU.add), R=[dfa[1], dcarry], W=[dfa[2]])
                        else:
                            op("dve", lambda e: e.tensor_tensor_scan(out=fa[2][0:3, :], data0=ones3[:, :], data1=fa[1][0:3, :],
                                                                     initial=carry[:, 0:1], op0=ALU.mult, op1=ALU.add),
                               R=[dfa[1], dcarry], W=[dfa[2]])
                        op("dve", lambda e: e.tensor_copy(out=carry[:, 0:1], in_=fa[2][0:3, 511:512]), R=[dfa[2]], W=[dcarry])
                        op("dve", lambda e: e.tensor_scalar(out=gq_f[0:3, c * 512:(c + 1) * 512], in0=fa[2][0:3, :], scalar1=fa[2][0:3, 0:1], scalar2=-8.0,
                                                            op0=ALU.subtract, op1=ALU.mult), R=[dfa[2]], W=[dgqf])
                        for t in range(4):
                            op("pe", lambda e: e.transpose(out=PB[1][:, 3 * t:3 * t + 3], in_=fa[2][0:3, t * 128:(t + 1) * 128],
                                                           identity=identf[0:3, 0:3]), R=[dfa[2], dCONST], W=[PD[1]], inc=(t == 3))
                        op("dve", lambda e: e.tensor_copy(out=ftok[:, 4 * c:4 * c + 4, :], in_=PB[1][:, 0:12].rearrange("p (t h) -> p t h", h=3)),
                           R=[PD[1]], W=[dftok])
                    op("pe", lambda e: e.matmul(PB[0][:, 0:48], lhsT=onesf[0:1, 0:128], rhs=ftok[0:1, :, :].rearrange("p t h -> p (t h)"),
                                                start=True, stop=True), R=[dCONST, dftok], W=[PD[0]])
                    op("dve", lambda e: e.tensor_copy(out=cb[:, :, :], in_=PB[0][:, 0:48].rearrange("p (t h) -> p t h", h=3)), R=[PD[0]], W=[dcb])
                    for hl in range(3):
                        for c in range(4):
                            op("dve", lambda e: e.tensor_scalar(out=biask[:, hl, c, 0:4 * c + 4], in0=ftok[:, 0:4 * c + 4, hl], scalar1=cb[:, 4 * c, hl:hl + 1],
                                                                scalar2=None, op0=ALU.subtract), R=[dftok, dcb], W=[dbk])
                    fring[:] = [2, 4]

                    def qprep(c):
                        for hl in range(3):
                            pbx = nxt("x", 2, 0)
                            for k in range(8):
                                op("pe", lambda e: e.matmul(PB[pbx][0:65, :], lhsT=wq[:, k, 65 * hl:65 * hl + 65], rhs=hT[:, k, c * 512:(c + 1) * 512],
                                                            start=(k == 0), stop=False),
                                   R=[dwq] + HD[4 * c:4 * c + 4], W=[PD[pbx]], inc=False)
                            op("pe", lambda e: e.matmul(PB[pbx][0:65, :], lhsT=selg[0:3, hl, :], rhs=gq_f[0:3, c * 512:(c + 1) * 512], start=False, stop=True),
                               R=[dgqf], W=[PD[pbx]])
                            evac("dve", QT[0:65, hl, c % 2, :], PB[pbx][0:65, :], [PD[pbx]], [dQT[hl][c % 2]])
                            pop_pending()
                    qprep(0)
                    for c in range(4):
                        maps = []
                        for hl in range(3):
                            def act_fn(j, p, sb_, lo, hl=hl, c=c):
                                op("act", lambda e: e.activation(out=PT[p][:, lo:512], in_=PB[sb_][:, lo:512],
                                                                 func=AF.Exp, bias=biask[:, hl, c, j:j + 1], scale=64 ** -0.5),
                                   R=[PD[sb_], dbk], W=[dPT[p]])

                            def dest(acc_pb, f2, hl=hl):
                                op("dve", lambda e: e.tensor_tensor(out=OT[0:64, hl, :], in0=PB[acc_pb][0:64, :], in1=fa[f2][0:64, :],
                                                                    op=ALU.mult), R=[PD[acc_pb], dfa[f2]], W=[dOT[hl]])
                            maps.append(dict(kfn=lambda j, hl=hl: KT[0:65, hl, j * 128:(j + 1) * 128], kdeps=dKT[hl],
                                             qfn=lambda lo, hl=hl, c=c: QT[0:65, hl, c % 2, lo:512], qdep=dQT[hl][c % 2],
                                             hl=hl, act_fn=act_fn, fix=causal_fix, done=lambda acc, last, dest=dest: normalize_to(acc, dest, last)))
                        attn_chunk(c, maps)
                        if c < 3:
                            qprep(c + 1)
                        pop_pending(all_=True)
                        wout_phase(c, 3)
                if stage == "fox":
                    S.barrier()
                    return

                dma("sp", wukv_f[:], w_ukv[l], W=[dwukv])
                op("pool", lambda e: e.tensor_scalar(out=wukv_b[:], in0=wukv_f[:], scalar1=gkv[:, 0:1], scalar2=None, op0=ALU.mult),
                   R=[dgn], W=[dwukv])
                dma("sp", wuq_f[:], w_uq[l].rearrange("(r p) n -> p r n", p=128), W=[dwuq])
                for r_ in range(2):
                    op("pool", lambda e: e.tensor_scalar(out=wuq_b[:, r_, :], in0=wuq_f[:, r_, :], scalar1=gq[:, r_:r_ + 1], scalar2=None,
                                                         op0=ALU.mult), R=[dgn], W=[dwuq])
                for sg in range(2):
                    h0 = 3 * sg
                    dma("pool", wkv[:, :, 0:160], win_v[:, :, O_CKV:O_CKV + 160], W=[dwkv])
                    dma("pool", wq[:, :, 0:256], win_v[:, :, O_CQ:O_CQ + 256], W=[dwq])
                    load_wo(640 + 64 * h0, 3)
                    for c in range(4):
                        proj_fm(0, wkv, dwkv, 0, 128, c)
                        evac("dve", ckvT[:, :], PB[0][:, :], [PD[0]], [dckvT])
                        op("act", lambda e: e.activation(out=fa[0][:, :], in_=PB[0][:, :], func=AF.Square), R=[PD[0]], W=[dfa[0]])
                        op("pe", lambda e: e.matmul(PB[1][:, :], lhsT=onesf[:, :], rhs=fa[0][:, :], start=True, stop=True),
                           R=[dCONST, dfa[0]], W=[PD[1]])
                        op("act", lambda e: e.activation(out=fa[1][:, :], in_=PB[1][:, :], func=AF.Ln, bias=epsc[:, 0:1], scale=1.0 / 128),
                           R=[PD[1], dCONST], W=[dfa[1]])
                        op("act", lambda e: e.activation(out=fa[1][:, :], in_=fa[1][:, :], func=AF.Exp, scale=-0.5), R=[dfa[1]], W=[dfa[1]])
                        for t in range(4):
                            op("pe", lambda e: e.matmul(PB[1][:, t:t + 1], lhsT=fa[0][:, t * 128:(t + 1) * 128], rhs=onesf[:, 0:1],
                                                        start=True, stop=True), R=[dCONST, dfa[0]], W=[PD[1]], inc=(t == 3))
                        op("act", lambda e: e.activation(out=rtok[:, :], in_=PB[1][:, 0:4], func=AF.Ln, bias=epsc[:, 0:1], scale=1.0 / 128),
                           R=[PD[1], dCONST], W=[drtok])
                        op("act", lambda e: e.activation(out=rtok[:, :], in_=rtok[:, :], func=AF.Exp, scale=-0.5), R=[drtok], W=[drtok])
                        for hl in range(3):
                            h = h0 + hl
                            op("pe", lambda e: e.matmul(PB[0][0:64, :], lhsT=wukv_b[:, h * 128:h * 128 + 64], rhs=ckvT[:, :], start=True, stop=True),
                               R=[dwukv, dckvT], W=[PD[0]])
                            op("dve", lambda e: e.tensor_tensor(out=KT[0:64, hl, c * 512:(c + 1) * 512], in0=PB[0][0:64, :], in1=fa[1][0:64, :],
                                                                op=ALU.mult), R=[PD[0], dfa[1]], W=[dKT[hl][c]])
                        for t in range(4):
                            i = 4 * c + t
                            wv3 = wukv_b[:, h0 * 128:(h0 + 3) * 128].rearrange("p (h d) -> p h d", h=3)[:, :, 64:128]
                            op("pe", lambda e: e.matmul(PB[0][:, 0:192].rearrange("p (h d) -> p h d", h=3), lhsT=ckvT[:, t * 128:(t + 1) * 128], rhs=wv3,
                                                        start=True, stop=True), R=[dwukv, dckvT], W=[PD[0]])
                            op("dve", lambda e: e.tensor_scalar(out=V[:, i, 0:3, 0:64], in0=PB[0][:, 0:192].rearrange("p (h d) -> p h d", h=3),
                                                                scalar1=rtok[:, t:t + 1], scalar2=None, op0=ALU.mult),
                               R=[PD[0], drtok], W=[dV[i]])
                            for k in range(8):
                                op("pe", lambda e: e.matmul(PB[1][:, 32 * t:32 * t + 32], lhsT=hT[:, k, i * 128:(i + 1) * 128], rhs=wkv[:, k, 128:160],
                                                            start=(k == 0), stop=(k == 7)),
                                   R=[dwkv] + HD[i:i + 1], W=[PD[1]], inc=(k == 7))
                        krv = PB[1][:, 0:128].rearrange("p (t d) -> p t d", t=4)
                        cos4, sin4 = cs[:, 4 * c:4 * c + 4, 0:16], cs[:, 4 * c:4 * c + 4, 16:32]
                        op("dve", lambda e: e.tensor_tensor(out=rk[0][:, :, :], in0=krv[:, :, 0:16], in1=cos4, op=ALU.mult), R=[PD[1], dCS], W=[drt])
                        op("dve", lambda e: e.tensor_tensor(out=rk[1][:, :, :], in0=krv[:, :, 16:32], in1=sin4, op=ALU.mult), R=[PD[1], dCS], W=[drt])
                        op("dve", lambda e: e.tensor_tensor(out=rk[2][:, :, :], in0=krv[:, :, 0:16], in1=sin4, op=ALU.mult), R=[PD[1], dCS], W=[drt])
                        op("dve", lambda e: e.tensor_tensor(out=rk[3][:, :, :], in0=krv[:, :, 16:32], in1=cos4, op=ALU.mult), R=[PD[1], dCS], W=[drt])
                        op("dve", lambda e: e.tensor_tensor(out=krp4[:, :, 64:80], in0=rk[0][:, :, :], in1=rk[1][:, :, :], op=ALU.subtract), R=[drt], W=[dkrp])
                        op("dve", lambda e: e.tensor_tensor(out=krp4[:, :, 80:96], in0=rk[2][:, :, :], in1=rk[3][:, :, :], op=ALU.add), R=[drt], W=[dkrp])
                        pv = PB[7][:, :].bitcast(BF16)
                        for t in range(4):
                            op("pe", lambda e: e.transpose(out=pv[0:96, t * 128:(t + 1) * 128], in_=krp4[:, t, :], identity=identb[:]),
                               R=[dkrp, dCONST], W=[PD[7]], inc=(t == 3))
                        pv = PB[7][:, :].bitcast(BF16)
                        for hl in range(3):
                            evac("act" if hl % 2 == 0 else "dve", KT[64:96, hl, c * 512:(c + 1) * 512], pv[64:96, 0:512], [PD[7]], [dKT[hl][c]])
                    def qprep(c):
                        for r_ in range(2):
                            proj_fm(r_, wq, dwq, 128 * r_, 128, c)
                            evac("dve", cqT[:, r_, :], PB[r_][:, :], [PD[r_]], [dcqT])
                            op("act", lambda e: e.activation(out=fa[r_][:, :], in_=PB[r_][:, :], func=AF.Square), R=[PD[r_]], W=[dfa[r_]])
                        for t in range(4):
                            for r_ in range(2):
                                op("pe", lambda e: e.matmul(PB[1][:, t:t + 1], lhsT=fa[r_][:, t * 128:(t + 1) * 128], rhs=onesf[:, 0:1],
                                                            start=(r_ == 0), stop=(r_ == 1)), R=[dCONST, dfa[r_]], W=[PD[1]], inc=(t == 3 and r_ == 1))
                        op("act", lambda e: e.activation(out=rtok[:, :], in_=PB[1][:, 0:4], func=AF.Ln, bias=epsc[:, 0:1], scale=1.0 / 256),
                           R=[PD[1], dCONST], W=[drtok])
                        op("act", lambda e: e.activation(out=rtok[:, :], in_=rtok[:, :], func=AF.Exp, scale=-0.5), R=[drtok], W=[drtok])
                        pq = [PB[0][:, :].bitcast(BF16), PB[1][:, :].bitcast(BF16), PB[7][:, :].bitcast(BF16)]
                        pqd = [PD[0], PD[1], PD[7]]
                        for t in range(4):
                            sb_ = nxt("s", 3, 2)
                            for r_ in range(2):
                                op("pe", lambda e: e.matmul(PB[sb_][:, 0:288], lhsT=cqT[:, r_, t * 128:(t + 1) * 128], rhs=wuq_b[:, r_, h0 * 96:(h0 + 3) * 96],
                                                            start=(r_ == 0), stop=(r_ == 1)), R=[dcqT, dwuq], W=[PD[sb_]], inc=(r_ == 1))
                            op("act", lambda e: e.activation(out=qsb4[:, t, :, :], in_=PB[sb_][:, 0:288].rearrange("p (h d) -> p h d", h=3), func=AF.Identity,
                                                             scale=rtok[:, t:t + 1]), R=[PD[sb_], drtok], W=[dqsb])

                        def b43(a_):
                            l_ = [list(v) for v in a_.ap]
                            return bass.AP(tensor=a_.tensor, offset=a_.offset, ap=[l_[0], l_[1], [0, 3], l_[2]])
                        cos43, sin43 = b43(cs[:, 4 * c:4 * c + 4, 0:16]), b43(cs[:, 4 * c:4 * c + 4, 16:32])
                        op("pool", lambda e: e.tensor_copy(out=qrot4[:, :, :, 0:64], in_=qsb4[:, :, :, 0:64]), R=[dqsb], W=[dqrot])
                        op("dve", lambda e: e.tensor_tensor(out=rq[0][:], in0=qsb4[:, :, :, 64:80], in1=cos43, op=ALU.mult), R=[dqsb, dCS], W=[drt])
                        op("dve", lambda e: e.tensor_tensor(out=rq[1][:], in0=qsb4[:, :, :, 80:96], in1=sin43, op=ALU.mult), R=[dqsb, dCS], W=[drt])
                        op("dve", lambda e: e.tensor_tensor(out=rq[2][:], in0=qsb4[:, :, :, 64:80], in1=sin43, op=ALU.mult), R=[dqsb, dCS], W=[drt])
                        op("dve", lambda e: e.tensor_tensor(out=rq[3][:], in0=qsb4[:, :, :, 80:96], in1=cos43, op=ALU.mult), R=[dqsb, dCS], W=[drt])
                        op("dve", lambda e: e.tensor_tensor(out=qrot4[:, :, :, 64:80], in0=rq[0][:], in1=rq[1][:], op=ALU.subtract), R=[drt], W=[dqrot])
                        op("dve", lambda e: e.tensor_tensor(out=qrot4[:, :, :, 80:96], in0=rq[2][:], in1=rq[3][:], op=ALU.add), R=[drt], W=[dqrot])
                        for t in range(4):
                            for hl in range(3):
                                op("pe", lambda e: e.transpose(out=pq[hl][0:96, t * 128:(t + 1) * 128], in_=qrot4[:, t, hl, :], identity=identb[:]),
                                   R=[dqrot, dCONST], W=[pqd[hl]])
                        for hl in range(3):
                            evac("act" if hl % 2 == 0 else "dve", QT[0:96, hl, c % 2, :], pq[hl][0:96, 0:512], [pqd[hl]], [dQT[hl][c % 2]])
                        pop_pending()
                    qprep(0)
                    for c in range(4):
                        maps = []
                        for hl in range(3):
                            def act_fn(j, p, sb_, lo):
                                op("act", lambda e: e.activation(out=PT[p][:, lo:512], in_=PB[sb_][:, lo:512], func=AF.Exp, scale=96 ** -0.5),
                                   R=[PD[sb_]], W=[dPT[p]])

                            def dest(acc_pb, f2, hl=hl):
                                op("dve", lambda e: e.tensor_tensor(out=OT[0:64, hl, :], in0=PB[acc_pb][0:64, :], in1=fa[f2][0:64, :],
                                                                    op=ALU.mult), R=[PD[acc_pb], dfa[f2]], W=[dOT[hl]])
                            maps.append(dict(kfn=lambda j, hl=hl: KT[0:96, hl, j * 128:(j + 1) * 128], kdeps=dKT[hl],
                                             qfn=lambda lo, hl=hl, c=c: QT[0:96, hl, c % 2, lo:512], qdep=dQT[hl][c % 2],
                                             hl=hl, act_fn=act_fn, fix=causal_fix, done=lambda acc, last, dest=dest: normalize_to(acc, dest, last)))
                        attn_chunk(c, maps)
                        if c < 3:
                            qprep(c + 1)
                        pop_pending(all_=True)
                        wout_phase(c, 3)
                S.barrier()

        def ffn(l):
            moe = (l % 2 == 1)
            with ExitStack() as s1:
                if moe:
                    rws = sbt(s1, "rws", (128, 8, NEXP), F32); drws = Dep()
                    logits = sbt(s1, "logits", (128, NT, NEXP), F32); dlog = Dep()
                    comb = sbt(s1, "comb", (128, NT, NEXP), F32); dcomb = Dep()
                    t8 = sbt(s1, "t8", (128, 8), F32); dt8 = Dep()
                    sel = sbt(s1, "sel", (128, 8), F32); ex = sbt(s1, "ex", (128, 8), F32); den = sbt(s1, "den", (128, 2), F32)
                    dma("sp", rws[:], router_w[0].rearrange("(k p) e -> p k e", p=128), W=[drws])
                    norm_phase(s1, 1, fp32_router=(rws, drws, logits, dlog))
                    for i in range(NT):
                        op("dve", lambda e: e.max(out=t8[:], in_=logits[:, i, :]), R=[dlog], W=[dt8])
                        op("dve", lambda e: e.tensor_scalar(out=sel[:], in0=logits[:, i, :], scalar1=t8[:, 1:2], scalar2=None, op0=ALU.is_ge),
                           R=[dlog, dt8], W=[dt8])
                        op("dve", lambda e: e.tensor_scalar(out=den[:, 0:1], in0=t8[:, 0:1], scalar1=-1.0, scalar2=None, op0=ALU.mult), R=[dt8], W=[dt8])
                        op("act", lambda e: e.activation(out=ex[:], in_=logits[:, i, :], func=AF.Exp, bias=den[:, 0:1], scale=1.0), R=[dlog, dt8], W=[dt8])
                        op("dve", lambda e: e.tensor_tensor(out=ex[:], in0=ex[:], in1=sel[:], op=ALU.mult), R=[dt8], W=[dt8])
                        op("dve", lambda e: e.reduce_sum(out=den[:, 1:2], in_=ex[:], axis=AX.X), R=[dt8], W=[dt8])
                        op("dve", lambda e: e.reciprocal(out=den[:, 1:2], in_=den[:, 1:2]), R=[dt8], W=[dt8])
                        op("dve", lambda e: e.tensor_scalar(out=comb[:, i, :], in0=ex[:], scalar1=den[:, 1:2], scalar2=None, op0=ALU.mult),
                           R=[dt8], W=[dcomb])
                else:
                    norm_phase(s1, 1)
                G = 4 if moe else 2
                GW = 128 * G
                wg = [sbt(s1, "wg%d" % i, (128, 8, GW), BF16) for i in range(2)]
                wu = [sbt(s1, "wu%d" % i, (128, 8, GW), BF16) for i in range(2)]
                wdf = [sbt(s1, "wdf%d" % i, (128, G, D), F32) for i in range(2)]
                wd = [sbt(s1, "wd%d" % i, (128, G, D), BF16) for i in range(2)]
                dwg = [Dep(), Dep()]; dwu = [Dep(), Dep()]; dwdf = [Dep(), Dep()]; dwd = [Dep(), Dep()]
                sg_ = [sbt(s1, "sg%d" % i, (128, 256), F32) for i in range(2)]; dsg = [Dep(), Dep()]
                aT = [sbt(s1, "aT%d" % i, (128, G, 256), BF16) for i in range(2)]; daT = [Dep(), Dep()]
                if moe:
                    groups = [(e_, g_) for e_ in range(NEXP) for g_ in range(DFE // GW)]
                else:
                    groups = [(None, g_) for g_ in range(DFF // GW)]
                cnt = 0
                prev_down = [None]
                for gi, (e_, g_) in enumerate(groups):
                    b = gi % 2
                    if moe:
                        gsrc = moe_w_gate[0, e_].rearrange("(k p) n -> p k n", p=128)[:, :, g_ * GW:(g_ + 1) * GW]
                        usrc = moe_w_up[0, e_].rearrange("(k p) n -> p k n", p=128)[:, :, g_ * GW:(g_ + 1) * GW]
                        dsrc = moe_w_down[0, e_, g_ * GW:(g_ + 1) * GW, :].rearrange("(j p) n -> p j n", p=128)
                    else:
                        gsrc = ffn_w_gate[0].rearrange("(k p) n -> p k n", p=128)[:, :, g_ * GW:(g_ + 1) * GW]
                        usrc = ffn_w_up[0].rearrange("(k p) n -> p k n", p=128)[:, :, g_ * GW:(g_ + 1) * GW]
                        dsrc = ffn_w_down[0, g_ * GW:(g_ + 1) * GW, :].rearrange("(j p) n -> p j n", p=128)
                    dma("pool", wg[b][:], gsrc, W=[dwg[b]])
                    dma("pool", wu[b][:], usrc, W=[dwu[b]])
                    dma("sp", wdf[b][:], dsrc, W=[dwdf[b]])
                    for j in range(G):
                        op("pool", lambda e: e.tensor_tensor(out=wd[b][:, j, :], in0=wdf[b][:, j, :], in1=gfb[:, :], op=ALU.mult),
                           R=[dwdf[b], dGFB], W=[dwd[b]])
                    for T in range(8):
                        ab = cnt % 2
                        cnt += 1
                        for j in range(G):
                            pg, pu = 2 * (j % 2), 2 * (j % 2) + 1
                            for k in range(8):
                                op("pe", lambda e: e.matmul(PB[pg][:, 0:256], lhsT=wg[b][:, k, j * 128:(j + 1) * 128], rhs=hT[:, k, T * 256:(T + 1) * 256],
                                                            start=(k == 0), stop=(k == 7)), R=[dwg[b]] + HD[2 * T:2 * T + 2], W=[PD[pg]], inc=(k == 7))
                            for k in range(8):
                                op("pe", lambda e: e.matmul(PB[pu][:, 0:256], lhsT=wu[b][:, k, j * 128:(j + 1) * 128], rhs=hT[:, k, T * 256:(T + 1) * 256],
                                                            start=(k == 0), stop=(k == 7)), R=[dwu[b]] + HD[2 * T:2 * T + 2], W=[PD[pu]], inc=(k == 7))
                            op("act", lambda e: e.activation(out=sg_[j % 2][:, :], in_=PB[pg][:, 0:256], func=AF.Silu), R=[PD[pg]], W=[dsg[j % 2]])
                            op("dve", lambda e: e.tensor_tensor(out=aT[ab][:, j, :], in0=PB[pu][:, 0:256], in1=sg_[j % 2][:, :], op=ALU.mult),
                               R=[PD[pu], dsg[j % 2]], W=[daT[ab]])
                            if j == 0 and prev_down[0] is not None:
                                prev_down[0]()
                                prev_down[0] = None

                        def down(T=T, ab=ab, b=b, e_=e_):
                            for t in range(2):
                                for half in range(2):
                                    pa = 4 + 2 * t + half
                                    i = 2 * T + t
                                    for j in range(G):
                                        op("pe", lambda e: e.matmul(PB[pa][:, :], lhsT=aT[ab][:, j, t * 128:(t + 1) * 128], rhs=wd[b][:, j, half * 512:(half + 1) * 512],
                                                                    start=(j == 0), stop=(j == G - 1)), R=[daT[ab], dwd[b]], W=[PD[pa]], inc=(j == G - 1))
                                    xsl = xs[:, i, half * 512:(half + 1) * 512]
                                    if moe:
                                        op("dve", lambda e: e.scalar_tensor_tensor(out=xsl, in0=PB[pa][:, :], scalar=comb[:, i, e_:e_ + 1], in1=xsl,
                                                                                   op0=ALU.mult, op1=ALU.add), R=[PD[pa], dcomb], W=[XD[i]])
                                    else:
                                        op("dve", lambda e: e.tensor_tensor(out=xsl, in0=PB[pa][:, :], in1=xsl, op=ALU.add), R=[PD[pa]], W=[XD[i]])
                        prev_down[0] = down
                if prev_down[0] is not None:
                    prev_down[0]()
                    prev_down[0] = None
                S.barrier()

        def final():
            with ExitStack() as s1:
                fnb = sbt(s1, "fnb", (128, D), F32); dfnb = Dep()
                junk = sbt(s1, "junkf", (128, D), BF16); djunk = Dep()
                dY = Dep()
                if stage == "full":
                    dma("sp", fnb[:], final_norm.partition_broadcast(128), W=[dfnb])
                    op("dve", lambda e: e.memset(ssq[:], 0.0), W=[dSSQ])
                    for i in range(NT):
                        op("act", lambda e: e.activation(out=junk[:], in_=xs[:, i, :], func=AF.Square, accum_out=ssq[:, i:i + 1]),
                           R=[XD[i]], W=[djunk, dSSQ])
                    op("act", lambda e: e.activation(out=rstd[:], in_=ssq[:], func=AF.Ln, bias=epsc[:, 0:1], scale=1.0 / D),
                       R=[dSSQ, dCONST], W=[dRSTD])
                    op("act", lambda e: e.activation(out=rstd[:], in_=rstd[:], func=AF.Exp, scale=-0.5), R=[dRSTD], W=[dRSTD])
                    for i in range(NT):
                        op("dve", lambda e: e.scalar_tensor_tensor(out=xs[:, i, :], in0=xs[:, i, :], scalar=rstd[:, i:i + 1], in1=fnb[:, :],
                                                                   op0=ALU.mult, op1=ALU.mult), R=[XD[i], dRSTD, dfnb], W=[XD[i]])
                yv = y_d.rearrange("(i p) f -> p i f", p=128)
                for q in range(4):
                    dma("sp", yv[:, 4 * q:4 * q + 4, :], xs[:, 4 * q:4 * q + 4, :], R=XD[4 * q:4 * q + 4], W=[dY])
                S.barrier()

        stop = False
        for l in range(DEPTH):
            if stage == "setup":
                break
            adaln(l)
            if stage == "adaln":
                break
            with ExitStack() as sn:
                norm_phase(sn, 0)
            if stage == "norm":
                break
            attention(l)
            if stage in ("diff", "fox", "attn0"):
                break
            ffn(l)
            if stage == "layer0":
                break
        final()
        print("instructions:", S.ninst)
    return nc


_CACHE = {}


def kernel(**inputs):
    stage = inputs.pop("_stage", "full")
    ncores = inputs.pop("_ncores", 8)
    if stage not in _CACHE:
        _CACHE[stage] = build(stage)
    nc = _CACHE[stage]
    consts = host_consts()
    shared = {k: np.ascontiguousarray(np.asarray(v, dtype=np.float32)) for k, v in inputs.items() if k not in ("x", "c")}
    shared.update(consts)
    x = np.asarray(inputs["x"], dtype=np.float32)
    c = np.asarray(inputs["c"], dtype=np.float32)
    in_maps = []
    for b in range(ncores):
        m = dict(shared)
        m["x"] = np.ascontiguousarray(x[b])
        m["c"] = np.ascontiguousarray(c[b])
        in_maps.append(m)
    res = run_bass_kernel_spmd(nc, in_maps, core_ids=list(range(ncores)))
    out = np.stack([np.asarray(r["y"], dtype=np.float32) for r in res.results], axis=0)
    return out
```

```python
import math
import types
import numpy as np
from contextlib import ExitStack
import concourse.bass as bass
import concourse.mybir as mybir
from concourse.bass_utils import run_bass_kernel_spmd

F32 = mybir.dt.float32
BF16 = mybir.dt.bfloat16
ALU = mybir.AluOpType
AF = mybir.ActivationFunctionType
AX = mybir.AxisListType

D = 1024
S_LEN = 2048
NT = 16
DEPTH = 2
IN_W = 2342
EPS = 1e-6
DFF = 2816
NEXP = 8
DFE = 3584
O_DQ, O_DK, O_DV, O_FQ, O_FK, O_FV, O_FF, O_CQ, O_CKV, O_KR = 0, 256, 512, 768, 1152, 1536, 1920, 1926, 2182, 2310


class SemRef:
    def __init__(self, sem, name):
        self.sem = sem
        self.name = name
        self.count = 0


class Eng:
    def __init__(self, name, obj, semref):
        self.name = name
        self.obj = obj
        self.sr = semref
        self.seen = {}


class Dep:
    __slots__ = ("w", "r", "excl")

    def __init__(self, excl=False):
        self.w = None
        self.r = {}
        self.excl = excl


class Sched:
    def __init__(self, nc, es, n_dma_sems=32):
        self.nc = nc
        self.E = {}
        for name, obj in [("pe", nc.tensor), ("act", nc.scalar), ("dve", nc.vector),
                          ("pool", nc.gpsimd), ("sp", nc.sync)]:
            sem = es.enter_context(nc.semaphore("s_" + name))
            self.E[name] = Eng(name, obj, SemRef(sem, name))
        self.dma_sems = {q: [SemRef(es.enter_context(nc.semaphore("d%s%d" % (q, i))), "d%s%d" % (q, i))
                             for i in range(n_dma_sems if q != "act" else 8)] for q in ("sp", "pool", "act")}
        self.dma_rr = {"sp": 0, "pool": 0, "act": 0}
        self.ninst = 0

    defer = None

    def _emit(self, eng, thunk):
        if self.defer is not None:
            self.defer[eng.name].append(thunk)
        else:
            thunk()

    @staticmethod
    def _snap(fn):
        if fn.__closure__ is None:
            return fn
        cells = tuple(types.CellType(c.cell_contents) for c in fn.__closure__)
        return types.FunctionType(fn.__code__, fn.__globals__, fn.__name__, fn.__defaults__, cells)

    def _wait(self, eng, needs):
        for sr, c in needs.items():
            if c <= 0:
                continue
            if sr is eng.sr and eng.name == "pe":
                continue
            if eng.seen.get(sr, 0) >= c:
                continue
            self._emit(eng, lambda o=eng.obj, s_=sr.sem, c_=c: o.wait_ge(s_, c_))
            eng.seen[sr] = c

    @staticmethod
    def _needs(R, W):
        needs = {}
        for d in R:
            if d.w is not None:
                sr, c = d.w
                needs[sr] = max(needs.get(sr, 0), c)
        for d in W:
            if d.w is not None:
                sr, c = d.w
                needs[sr] = max(needs.get(sr, 0), c)
            for sr, c in d.r.items():
                needs[sr] = max(needs.get(sr, 0), c)
        return needs

    def op(self, ename, fn, R=(), W=(), inc=True):
        if any(d.excl for d in R):
            W = list(W) + [d for d in R if d.excl]
            R = [d for d in R if not d.excl]
        eng = self.E[ename]
        self._wait(eng, self._needs(R, W))
        self.ninst += 1
        sr = eng.sr
        if self.defer is not None:
            fn = self._snap(fn)

        def thunk(fn=fn, o=eng.obj, s_=sr.sem, inc=inc):
            ins = fn(o)
            if inc:
                ins.then_inc(s_, 1)
        self._emit(eng, thunk)
        if inc:
            sr.count += 1
            tok = sr.count
        else:
            tok = sr.count + 1
        for d in R:
            d.r[sr] = max(d.r.get(sr, 0), tok)
        for d in W:
            d.w = (sr, tok)
            d.r = {}

    def dma(self, ename, out, in_, R=(), W=(), **kw):
        eng = self.E[ename]
        pool_ = self.dma_sems[ename]
        ds = pool_[self.dma_rr[ename]]
        self.dma_rr[ename] = (self.dma_rr[ename] + 1) % len(pool_)
        needs = self._needs(R, W)
        if ds.count > 0:
            needs[ds] = max(needs.get(ds, 0), ds.count)
        self._wait(eng, needs)

        def thunk(o=eng.obj, out=out, in_=in_, kw=kw, s_=ds.sem):
            o.dma_start(out=out, in_=in_, **kw).then_inc(s_, 16)
        self._emit(eng, thunk)
        self.ninst += 1
        ds.count += 16
        tok = ds.count
        for d in R:
            d.r[ds] = tok
        for d in W:
            d.w = (ds, tok)
            d.r = {}

    def barrier(self):
        srs = [e.sr for e in self.E.values()] + self.dma_sems["sp"] + self.dma_sems["pool"] + self.dma_sems["act"]
        for eng in self.E.values():
            needs = {sr: sr.count for sr in srs if sr is not eng.sr}
            if eng.name != "pe":
                needs[eng.sr] = eng.sr.count
            self._wait(eng, needs)


def bmid(a, n):
    l = [list(v) for v in a.ap]
    return bass.AP(tensor=a.tensor, offset=a.offset, ap=[l[0], [0, n]] + l[1:])


def host_consts():
    half = 16
    freqs = (10000.0 ** (-np.arange(half, dtype=np.float32) / half)).astype(np.float32)
    t = np.arange(S_LEN, dtype=np.float32)
    ang = (t[:, None] * freqs[None, :]).astype(np.float32)
    cs = np.concatenate([np.cos(ang), np.sin(ang)], axis=1).astype(np.float32)
    cs = np.ascontiguousarray(cs.reshape(NT, 128, 32).transpose(1, 0, 2))
    n = np.arange(-127, 256)
    nn = np.maximum(n, 0)
    nf = np.maximum(nn, 1).astype(np.float32)
    large = 16 + (np.log(nf / np.float32(16)) / np.float32(math.log(128 / 16)) * np.float32(16)).astype(np.int32)
    large = np.minimum(large, 31)
    bucket = np.where(nn < 16, nn, large)
    oh = np.zeros((32, 383), np.float32)
    oh[bucket, np.arange(383)] = 1.0
    step = np.broadcast_to((n >= 0).astype(np.float32)[None, :], (4, 383)).copy()
    neg = ((step - 1.0) * 30000.0).astype(np.float32)
    return {"cs_tab": cs, "t5_oh": oh, "t5_step": step, "t5_neg": neg}


def build(stage="full"):
    nc = bass.Bass("TRN2", target_bir_lowering=False)
    dt_in = lambda name, shape: nc.dram_tensor(name, list(shape), F32, kind="ExternalInput").ap()
    x_d = dt_in("x", (S_LEN, D))
    c_d = dt_in("c", (D,))
    w_ada = dt_in("w_ada", (DEPTH, D, 6 * D))
    b_ada = dt_in("b_ada", (DEPTH, 6 * D))
    attn_norm = dt_in("attn_norm", (DEPTH, D))
    ffn_norm = dt_in("ffn_norm", (DEPTH, D))
    w_in = dt_in("w_in", (DEPTH, D, IN_W))
    b_forget = dt_in("b_forget", (DEPTH, 6))
    diff_lambda = dt_in("diff_lambda", (DEPTH, 4, 32))
    diff_subln = dt_in("diff_subln", (DEPTH, 64))
    rel_bias = dt_in("rel_bias", (32, 4))
    mla_q_norm = dt_in("mla_q_norm", (DEPTH, 256))
    mla_kv_norm = dt_in("mla_kv_norm", (DEPTH, 128))
    w_uq = dt_in("w_uq", (DEPTH, 256, 576))
    w_ukv = dt_in("w_ukv", (DEPTH, 128, 768))
    w_out = dt_in("w_out", (DEPTH, D, D))
    ffn_w_gate = dt_in("ffn_w_gate", (1, D, DFF))
    ffn_w_up = dt_in("ffn_w_up", (1, D, DFF))
    ffn_w_down = dt_in("ffn_w_down", (1, DFF, D))
    router_w = dt_in("router_w", (1, D, NEXP))
    moe_w_gate = dt_in("moe_w_gate", (1, NEXP, D, DFE))
    moe_w_up = dt_in("moe_w_up", (1, NEXP, D, DFE))
    moe_w_down = dt_in("moe_w_down", (1, NEXP, DFE, D))
    final_norm = dt_in("final_norm", (D,))
    cs_d = dt_in("cs_tab", (128, NT, 32))
    oh_d = dt_in("t5_oh", (32, 383))
    step_d = dt_in("t5_step", (4, 383))
    neg_d = dt_in("t5_neg", (4, 383))
    y_d = nc.dram_tensor("y", [S_LEN, D], F32, kind="ExternalOutput").ap()
    mod_scr = nc.dram_tensor("mod_scr", [DEPTH, 6 * D], F32, kind="Internal")
    g_scr = nc.dram_tensor("g_scr", [4, 383], F32, kind="Internal")

    es = ExitStack()
    with es:
        S = Sched(nc, es)
        op, dma = S.op, S.dma

        uniq = [0]

        def sbt(stack, name, shape, dt):
            uniq[0] += 1
            return stack.enter_context(nc.sbuf_tensor("%s_%d" % (name, uniq[0]), list(shape), dt))

        PB = [es.enter_context(nc.psum_tensor("pb%d" % i, [128, 512], F32)) for i in range(8)]
        PD = [Dep(excl=True) for _ in range(8)]

        xs = sbt(es, "xs", (128, NT, D), F32)
        XD = [Dep() for _ in range(NT)]
        hT = sbt(es, "hT", (128, 8, S_LEN), BF16)
        HDa = [Dep() for _ in range(NT)]; HDb = [Dep() for _ in range(NT)]
        class _HD:
            def __getitem__(self, k):
                if isinstance(k, slice):
                    return HDa[k] + HDb[k]
                return HDa[k]
        HD = _HD()
        identb = sbt(es, "identb", (128, 128), BF16)
        identf = sbt(es, "identf", (128, 128), F32)
        onesf = sbt(es, "onesf", (128, 128), F32)
        cs = sbt(es, "cs", (128, NT, 32), F32)
        mt5 = sbt(es, "mt5", (128, 4, 2, 128), BF16)
        negmask = sbt(es, "negmask", (128, 128), BF16)
        b31 = sbt(es, "b31", (128, 4), F32)
        cact = sbt(es, "cact", (128, 8), F32)
        cols = sbt(es, "cols", (128, 64), F32)
        amod = sbt(es, "amod", (128, 4, 8), F32)
        gab = sbt(es, "gab", (128, D), F32)
        gfb = sbt(es, "gfb", (128, D), F32)
        rstd = sbt(es, "rstd", (128, NT), F32)
        ssq = sbt(es, "ssq", (128, NT), F32)
        epsc = sbt(es, "epsc", (128, 1), F32)
        neglam = sbt(es, "neglam", (128, 1), F32)
        gsub = sbt(es, "gsub", (64, 1), F32)
        dCONST = Dep(); dCS = Dep(); dMT5 = Dep(); dB31 = Dep(); dCACT = Dep(); dCOLS = Dep()
        dAMOD = Dep(); dGAB = Dep(); dGFB = Dep(); dRSTD = Dep(); dSSQ = Dep(); dLAM = Dep(); dGSUB = Dep()
        dMODSCR = Dep(); dGSCR = Dep()

        op("pool", lambda e: e.memset(onesf[:], 1.0), W=[dCONST])
        op("pool", lambda e: e.memset(epsc[:], EPS), W=[dCONST])
        op("pool", lambda e: e.affine_select(out=identf[:], in_=onesf[:], pattern=[[-1, 128]], compare_op=ALU.is_equal,
                                             fill=0.0, base=0, channel_multiplier=1), R=[dCONST], W=[dCONST])
        op("pool", lambda e: e.affine_select(out=identb[:], in_=onesf[:], pattern=[[-1, 128]], compare_op=ALU.is_equal,
                                             fill=0.0, base=0, channel_multiplier=1), R=[dCONST], W=[dCONST])
        zf = sbt(es, "zf", (128, 128), F32)
        op("pool", lambda e: e.memset(zf[:], 0.0), W=[dCONST])
        op("pool", lambda e: e.affine_select(out=negmask[:], in_=zf[:], pattern=[[1, 128]], compare_op=ALU.is_ge,
                                             fill=-30000.0, base=0, channel_multiplier=-1), R=[dCONST], W=[dCONST])
        dma("sp", cs[:], cs_d, W=[dCS])
        dma("sp", b31[:], rel_bias[31, :].partition_broadcast(128), W=[dB31])
        xv = x_d.rearrange("(i p) f -> p i f", p=128)
        for q in range(4):
            dma("sp", xs[:, 4 * q:4 * q + 4, :], xv[:, 4 * q:4 * q + 4, :], W=XD[4 * q:4 * q + 4])
        with ExitStack() as s0:
            crow = sbt(s0, "crow", (8, 128), F32); dcrow = Dep()
            jmat = sbt(s0, "jmat", (128, 128), F32)
            rbs = sbt(s0, "rbs", (32, 4), F32)
            ohs = sbt(s0, "ohs", (32, 383), F32)
            stp = sbt(s0, "stp", (4, 383), F32)
            nb31c = sbt(s0, "nb31c", (4, 1), F32)
            gsb = sbt(s0, "gsb", (4, 383), F32)
            hank = sbt(s0, "hank", (128, 2, 128), F32)
            dT5 = Dep(); dHK = [Dep(), Dep()]
            dma("sp", crow[:], c_d.rearrange("(k p) -> k p", p=128), W=[dcrow])
            op("pe", lambda e: e.transpose(out=PB[0][:, 0:8], in_=crow[:], identity=identf[0:8, 0:8]), R=[dcrow, dCONST], W=[PD[0]])
            op("act", lambda e: e.activation(out=cact[:], in_=PB[0][:, 0:8], func=AF.Silu), R=[PD[0]], W=[dCACT])
            op("pool", lambda e: e.affine_select(out=jmat[:], in_=onesf[:], pattern=[[1, 128]], compare_op=ALU.is_equal,
                                                 fill=0.0, base=-127, channel_multiplier=1), R=[dCONST], W=[dT5])
            dma("sp", rbs[:], rel_bias, W=[dT5])
            dma("sp", ohs[:], oh_d, W=[dT5])
            dma("sp", stp[:], step_d, W=[dT5])
            dma("sp", nb31c[:], rel_bias[31:32, :].rearrange("a h -> h a"), W=[dT5], allow_slow_non_contiguous=True)
            S32 = 32 ** 0.5
            ngt = sbt(s0, "ngt", (4, 383), F32)
            dma("sp", ngt[:], neg_d, W=[dT5])
            op("dve", lambda e: e.tensor_scalar(out=nb31c[:], in0=nb31c[:], scalar1=-S32, scalar2=None, op0=ALU.mult), R=[dT5], W=[dT5])
            op("pe", lambda e: e.matmul(PB[1][0:4, 0:383], lhsT=rbs[:, :], rhs=ohs[:, :], start=True, stop=True), R=[dT5], W=[PD[1]])
            op("act", lambda e: e.activation(out=gsb[:], in_=PB[1][0:4, 0:383], func=AF.Identity, bias=nb31c[:, 0:1], scale=S32), R=[PD[1], dT5], W=[dT5])
            op("dve", lambda e: e.tensor_tensor(out=gsb[:], in0=gsb[:], in1=stp[:], op=ALU.mult), R=[dT5], W=[dT5])
            op("dve", lambda e: e.tensor_tensor(out=gsb[:], in0=gsb[:], in1=ngt[:], op=ALU.add), R=[dT5], W=[dT5])
            dma("sp", g_scr.ap(), gsb[:], R=[dT5], W=[dGSCR])
            for h in range(4):
                for dl in range(2):
                    hk = dHK[dl]
                    dma("sp", hank[:, dl, :], bass.AP(tensor=g_scr, offset=h * 383 + 128 * dl, ap=[[1, 128], [1, 128]]),
                        R=[dGSCR], W=[hk])
                    pb = 2 + dl
                    op("pe", lambda e: e.matmul(PB[pb][:, 0:128], lhsT=jmat[:, :], rhs=hank[:, dl, :], start=True, stop=True),
                       R=[dT5, hk], W=[PD[pb]])
                    op("act", lambda e: e.activation(out=mt5[:, h, dl, :], in_=PB[pb][:, 0:128], func=AF.Identity), R=[PD[pb]], W=[dMT5])
            S.barrier()

        def adaln(l):
            with ExitStack() as s1:
                wb = [sbt(s1, "adaw%d" % i, (128, 3072), F32) for i in range(5)]
                dwb = [Dep() for _ in range(5)]
                brow = sbt(s1, "brow", (1, 6 * D), F32); dbrow = Dep()
                mrow = [sbt(s1, "mrow%d" % i, (1, 512), F32) for i in range(2)]
                dmrow = [Dep(), Dep()]
                rows = sbt(s1, "rows", (64, 128), F32); drows = Dep()
                dma("sp", brow[:], b_ada[l:l + 1, :], W=[dbrow])
                nb = 0
                for half in range(2):
                    for k in range(8):
                        b = nb % 5
                        nb += 1
                        dma(("sp", "pool", "act")[nb % 3], wb[b][:], w_ada[l, k * 128:(k + 1) * 128, half * 3072:(half + 1) * 3072], W=[dwb[b]])
                        for cb in range(6):
                            op("pe", lambda e: e.matmul(PB[cb][0:1, :], lhsT=cact[:, k:k + 1], rhs=wb[b][:, cb * 512:(cb + 1) * 512],
                                                        start=(k == 0), stop=(k == 7)),
                               R=[dCACT, dwb[b]], W=[PD[cb]], inc=(k == 7 or cb == 5))
                    for cb in range(6):
                        cc = half * 6 + cb
                        m_ = cb % 2
                        op("dve", lambda e: e.tensor_tensor(out=mrow[m_][:], in0=PB[cb][0:1, :], in1=brow[0:1, cc * 512:(cc + 1) * 512],
                                                            op=ALU.add), R=[PD[cb], dbrow], W=[dmrow[m_]])
                        dma("sp", mod_scr.ap()[l:l + 1, cc * 512:(cc + 1) * 512], mrow[m_][:], R=[dmrow[m_]], W=[dMODSCR])
                dma("sp", rows[0:48, :], mod_scr.ap()[l, :].rearrange("(j p) -> j p", p=128), R=[dMODSCR], W=[drows])
                dma("sp", rows[48:56, :], attn_norm[l, :].rearrange("(j p) -> j p", p=128), W=[drows])
                dma("sp", rows[56:64, :], ffn_norm[l, :].rearrange("(j p) -> j p", p=128), W=[drows])
                op("pe", lambda e: e.transpose(out=PB[2][:, 0:64], in_=rows[:, :], identity=identf[0:64, 0:64]),
                   R=[drows, dCONST], W=[PD[2]])
                op("act", lambda e: e.activation(out=cols[:], in_=PB[2][:, 0:64], func=AF.Identity), R=[PD[2]], W=[dCOLS])
                op("dve", lambda e: e.scalar_tensor_tensor(out=amod[:, 0, :], in0=cols[:, 8:16], scalar=1.0, in1=cols[:, 48:56],
                                                           op0=ALU.add, op1=ALU.mult), R=[dCOLS], W=[dAMOD])
                op("dve", lambda e: e.tensor_copy(out=amod[:, 1, :], in_=cols[:, 0:8]), R=[dCOLS], W=[dAMOD])
                op("dve", lambda e: e.scalar_tensor_tensor(out=amod[:, 2, :], in0=cols[:, 32:40], scalar=1.0, in1=cols[:, 56:64],
                                                           op0=ALU.add, op1=ALU.mult), R=[dCOLS], W=[dAMOD])
                op("dve", lambda e: e.tensor_copy(out=amod[:, 3, :], in_=cols[:, 24:32]), R=[dCOLS], W=[dAMOD])
                dma("sp", gab[:], mod_scr.ap()[l, 2 * D:3 * D].partition_broadcast(128), R=[dMODSCR], W=[dGAB])
                dma("sp", gfb[:], mod_scr.ap()[l, 5 * D:6 * D].partition_broadcast(128), R=[dMODSCR], W=[dGFB])
                S.barrier()

        def norm_phase(stack, which, fp32_router=None):
            ia, ish = 2 * which, 2 * which + 1
            with ExitStack() as s1:
                junk = sbt(s1, "junk", (128, D), BF16); djunk = Dep()
                if fp32_router is None:
                    xn = [sbt(s1, "xn%d" % i, (128, D), BF16) for i in range(2)]
                else:
                    xn = [sbt(s1, "xn%d" % i, (128, D), F32) for i in range(2)]
                    h2f = [sbt(s1, "h2f%d" % i, (128, 8, 128), F32) for i in range(2)]
                    dh2f = [Dep(), Dep()]
                    rws, drws, logits, dlog = fp32_router
                dxn = [Dep(), Dep()]
                op("dve", lambda e: e.memset(ssq[:], 0.0), W=[dSSQ])
                for i in range(NT):
                    op("act", lambda e: e.activation(out=junk[:], in_=xs[:, i, :], func=AF.Square, accum_out=ssq[:, i:i + 1]),
                       R=[XD[i]], W=[djunk, dSSQ])
                op("act", lambda e: e.activation(out=rstd[:], in_=ssq[:], func=AF.Ln, bias=epsc[:, 0:1], scale=1.0 / D),
                   R=[dSSQ, dCONST], W=[dRSTD])
                op("act", lambda e: e.activation(out=rstd[:], in_=rstd[:], func=AF.Exp, scale=-0.5), R=[dRSTD], W=[dRSTD])
                for i in range(NT):
                    b = i % 2
                    op("dve", lambda e: e.tensor_scalar(out=xn[b][:], in0=xs[:, i, :], scalar1=rstd[:, i:i + 1], scalar2=None,
                                                        op0=ALU.mult), R=[XD[i], dRSTD], W=[dxn[b]])
                    if fp32_router is None:
                        pb = i % 2
                        pv = PB[pb][:, :].bitcast(BF16)
                        for k in range(8):
                            op("pe", lambda e: e.transpose(out=pv[:, k * 128:(k + 1) * 128], in_=xn[b][:, k * 128:(k + 1) * 128],
                                                           identity=identb[:]), R=[dxn[b], dCONST], W=[PD[pb]], inc=(k == 7))
                        for k in range(8):
                            if i % 2 == 0:
                                op("act", lambda e: e.activation(out=hT[:, k, i * 128:(i + 1) * 128], in_=pv[:, k * 128:(k + 1) * 128],
                                                                 func=AF.Identity, scale=amod[:, ia, k:k + 1], bias=amod[:, ish, k:k + 1]),
                                   R=[PD[pb], dAMOD], W=[HDa[i]])
                            else:
                                op("dve", lambda e: e.tensor_scalar(out=hT[:, k, i * 128:(i + 1) * 128], in0=pv[:, k * 128:(k + 1) * 128],
                                                                    scalar1=amod[:, ia, k:k + 1], scalar2=amod[:, ish, k:k + 1],
                                                                    op0=ALU.mult, op1=ALU.add), R=[PD[pb], dAMOD], W=[HDb[i]])
                    else:
                        for half in range(2):
                            pb = half
                            for kk in range(4):
                                k = half * 4 + kk
                                op("pe", lambda e: e.transpose(out=PB[pb][:, kk * 128:(kk + 1) * 128], in_=xn[b][:, k * 128:(k + 1) * 128],
                                                               identity=identf[:]), R=[dxn[b], dCONST], W=[PD[pb]], inc=(kk == 3))
                            for kk in range(4):
                                k = half * 4 + kk
                                op("act", lambda e: e.activation(out=h2f[b][:, k, :], in_=PB[pb][:, kk * 128:(kk + 1) * 128],
                                                                 func=AF.Identity, scale=amod[:, ia, k:k + 1], bias=amod[:, ish, k:k + 1]),
                                   R=[PD[pb], dAMOD], W=[dh2f[b]])
                        op("dve", lambda e: e.tensor_copy(out=hT[:, :, i * 128:(i + 1) * 128], in_=h2f[b][:, :, :]), R=[dh2f[b]], W=[HDa[i], HDb[i]])
                        for k in range(8):
                            op("pe", lambda e: e.matmul(PB[2][:, 0:8], lhsT=h2f[b][:, k, :], rhs=rws[:, k, :], start=(k == 0), stop=(k == 7)),
                               R=[dh2f[b], drws], W=[PD[2]], inc=(k == 7))
                        op("dve", lambda e: e.tensor_copy(out=logits[:, i, :], in_=PB[2][:, 0:8]), R=[PD[2]], W=[dlog])
                S.barrier()

        def attention(l):
            lam_init = 0.8 - 0.6 * math.exp(-0.3 * l)
            win_v = w_in[l].rearrange("(k p) n -> p k n", p=128)
            with ExitStack() as s1:
                KT = sbt(s1, "KT", (128, 3, S_LEN), BF16); dKT = [[Dep() for _ in range(4)] for _ in range(3)]
                QT = sbt(s1, "QT", (128, 3, 2, 512), BF16); dQT = [[Dep(), Dep()] for _ in range(3)]
                V = sbt(s1, "V", (128, NT, 3, 65), BF16); dV = [Dep() for _ in range(NT)]
                OT = sbt(s1, "OT", (64, 3, 512), BF16); dOT = [Dep() for _ in range(3)]
                PT = [sbt(s1, "PT%d" % i, (128, 512), BF16) for i in range(4)]; dPT = [Dep() for _ in range(4)]
                wkv = sbt(s1, "wkv", (128, 8, 387), BF16); dwkv = Dep()
                wq = sbt(s1, "wq", (128, 8, 256), BF16); dwq = Dep()
                wo = sbt(s1, "wo", (64, 3, D), BF16); dwo = Dep()
                fa = [sbt(s1, "fa%d" % i, (128, 512), F32) for i in range(6)]; dfa = [Dep() for _ in range(6)]
                dl_b = sbt(s1, "dl_b", (128, 128), F32); ddl = Dep()
                lt = sbt(s1, "lt", (128, 8), F32)
                negb = sbt(s1, "negb", (3, 1), F32); dnegb = Dep()
                ftok = sbt(s1, "ftok", (128, NT, 3), F32); dftok = Dep()
                cb = sbt(s1, "cb", (128, NT, 3), F32); dcb = Dep()
                biask = sbt(s1, "biask", (128, 3, 4, NT), F32); dbk = Dep()
                carry = sbt(s1, "carry", (3, 1), F32); dcarry = Dep()
                ones3 = sbt(s1, "ones3", (3, 512), F32)
                gq_f = sbt(s1, "gq_f", (3, S_LEN), BF16); dgqf = Dep()
                selg = sbt(s1, "selg", (3, 3, 65), BF16)
                wukv_f = sbt(s1, "wukv_f", (128, 768), F32); wukv_b = sbt(s1, "wukv_b", (128, 768), BF16); dwukv = Dep()
                wuq_f = sbt(s1, "wuq_f", (128, 2, 576), F32); wuq_b = sbt(s1, "wuq_b", (128, 2, 576), BF16); dwuq = Dep()
                gq = sbt(s1, "gq", (128, 2), F32); gkv = sbt(s1, "gkv", (128, 1), F32); dgn = Dep()
                ckvT = sbt(s1, "ckvT", (128, 512), BF16); dckvT = Dep()
                cqT = sbt(s1, "cqT", (128, 2, 512), BF16); dcqT = Dep()
                rtok = sbt(s1, "rtok", (128, 4), F32); drtok = Dep()
                krp4 = sbt(s1, "krp4", (128, 4, 96), BF16); dkrp = Dep()
                rk = [sbt(s1, "rk%d" % i, (128, 4, 16), F32) for i in range(4)]
                qsb4 = sbt(s1, "qsb4", (128, 4, 3, 96), F32)
                qrot4 = sbt(s1, "qrot4", (128, 4, 3, 96), BF16)
                rq = [sbt(s1, "rq%d" % i, (128, 4, 3, 16), F32) for i in range(4)]
                dqsb = Dep(); dqrot = Dep(); drt = Dep()

                op("pool", lambda e: e.memset(V[:, :, :, 64:65], 1.0), W=dV)
                op("pool", lambda e: e.memset(krp4[:], 0.0), W=[dkrp])
                op("pool", lambda e: e.memset(ones3[:], 1.0), W=[dcarry])
                op("pool", lambda e: e.memset(selg[:], 0.0), W=[dgqf])
                for hl_ in range(3):
                    op("pool", lambda e: e.tensor_copy(out=selg[0:3, hl_, 64:65], in_=identf[0:3, hl_:hl_ + 1]), R=[dCONST], W=[dgqf])
                dma("sp", dl_b[:], diff_lambda[l].rearrange("a b -> (a b)").partition_broadcast(128), W=[ddl])
                op("dve", lambda e: e.tensor_tensor(out=dl_b[:, 0:32], in0=dl_b[:, 0:32], in1=dl_b[:, 32:64], op=ALU.mult), R=[ddl], W=[ddl])
                op("dve", lambda e: e.tensor_tensor(out=dl_b[:, 64:96], in0=dl_b[:, 64:96], in1=dl_b[:, 96:128], op=ALU.mult), R=[ddl], W=[ddl])
                op("dve", lambda e: e.reduce_sum(out=lt[:, 0:1], in_=dl_b[:, 0:32], axis=AX.X), R=[ddl], W=[ddl])
                op("dve", lambda e: e.reduce_sum(out=lt[:, 1:2], in_=dl_b[:, 64:96], axis=AX.X), R=[ddl], W=[ddl])
                op("act", lambda e: e.activation(out=lt[:, 2:4], in_=lt[:, 0:2], func=AF.Exp), R=[ddl], W=[ddl])
                op("dve", lambda e: e.tensor_tensor(out=lt[:, 4:5], in0=lt[:, 3:4], in1=lt[:, 2:3], op=ALU.subtract), R=[ddl], W=[ddl])
                op("dve", lambda e: e.tensor_scalar(out=neglam[:], in0=lt[:, 4:5], scalar1=-lam_init, scalar2=None, op0=ALU.add), R=[ddl], W=[dLAM])
                dma("sp", gsub[:], diff_subln[l:l + 1, :].rearrange("a d -> d a"), W=[dGSUB], allow_slow_non_contiguous=True)
                op("dve", lambda e: e.tensor_scalar(out=gsub[:], in0=gsub[:], scalar1=1.0 - lam_init, scalar2=None, op0=ALU.mult), R=[dGSUB], W=[dGSUB])
                dma("sp", gq[:], mla_q_norm[l, :].rearrange("(r p) -> p r", p=128), W=[dgn], allow_slow_non_contiguous=True)
                dma("sp", gkv[:], mla_kv_norm[l:l + 1, :].rearrange("a p -> p a"), W=[dgn], allow_slow_non_contiguous=True)

                state = {"s": 0, "p": 0, "a": 0, "f": 0, "x": 0}

                fring = [0, 6]

                def nxt(key, n, base):
                    if key == "f":
                        base, n = fring
                    v = base + state[key] % n
                    state[key] += 1
                    if key == "f":
                        ensure_free("f%d" % v)
                    elif key == "a":
                        ensure_free("a%d" % v)
                    return v

                pending = []

                def pop_pending(all_=False):
                    if all_:
                        while pending:
                            pending.pop(0)[1]()
                        return
                    if pending:
                        pending[0][0] -= 1
                        if pending[0][0] <= 0:
                            pending.pop(0)[1]()

                def ensure_free(res):
                    while any(res in p_[2] for p_ in pending if len(p_) > 2):
                        pending.pop(0)[1]()

                def normalize_to(acc_pb, dest_fn, last=False):
                    f1 = nxt("f", 6, 0)
                    if last:
                        op("act", lambda e: e.activation(out=fa[f1][64:65, :], in_=PB[acc_pb][64:65, :], func=AF.Ln), R=[PD[acc_pb]], W=[dfa[f1]])
                        op("act", lambda e: e.activation(out=fa[f1][64:65, :], in_=fa[f1][64:65, :], func=AF.Exp, scale=-1.0), R=[dfa[f1]], W=[dfa[f1]])
                    else:
                        op("dve", lambda e: e.reciprocal(out=fa[f1][64:65, :], in_=PB[acc_pb][64:65, :]), R=[PD[acc_pb]], W=[dfa[f1]])
                    f2 = nxt("f", 6, 0)

                    def stage_b():
                        op("pe", lambda e: e.matmul(PB[7][0:64, :], lhsT=onesf[64:65, 0:64], rhs=fa[f1][64:65, :], start=True, stop=True),
                           R=[dCONST, dfa[f1]], W=[PD[7]])
                        op("dve", lambda e: e.tensor_copy(out=fa[f2][0:64, :], in_=PB[7][0:64, :]), R=[PD[7]], W=[dfa[f2]])
                        dest_fn(acc_pb, f2)
                    pending.append([2 if last else 4, stage_b, {"a%d" % acc_pb, "f%d" % f1, "f%d" % f2}])

                def attn_chunk(c, maps):
                    nj = 4 * c + 4
                    seq = [(mi, j) for mi in range(len(maps)) for j in range(nj)]
                    LA = 2
                    sbank = {}
                    accs = {}

                    def qk(idx):
                        mi, j = seq[idx]
                        M = maps[mi]
                        r = j - 4 * c
                        lo = max(0, r) * 128
                        sb_ = nxt("s", 3, 2)
                        sbank[idx] = sb_
                        fx = M["fix"](r)
                        op("pe", lambda e: e.matmul(PB[sb_][:, lo:512], lhsT=M["kfn"](j), rhs=M["qfn"](lo), start=True, stop=(len(fx) == 0)),
                           R=[M["kdeps"][j // 4], M["qdep"]], W=[PD[sb_]], inc=(len(fx) == 0))
                        for n_, (rr, tile_ap) in enumerate(fx):
                            op("pe", lambda e: e.matmul(PB[sb_][:, rr * 128:(rr + 1) * 128], lhsT=identb[:, :], rhs=tile_ap,
                                                        start=False, stop=(n_ == len(fx) - 1)),
                               R=[dCONST, dMT5], W=[PD[sb_]], inc=(n_ == len(fx) - 1))
                    for idx in range(min(LA, len(seq))):
                        qk(idx)
                    for idx, (mi, j) in enumerate(seq):
                        M = maps[mi]
                        if j == 0:
                            accs[mi] = nxt("a", 2, 5)
                        acc = accs[mi]
                        r = j - 4 * c
                        lo = max(0, r) * 128
                        p = nxt("p", 4, 0)
                        M["act_fn"](j, p, sbank[idx], lo)
                        if idx + LA < len(seq):
                            qk(idx + LA)
                        op("pe", lambda e: e.matmul(PB[acc][0:65, lo:512], lhsT=V[:, j, M["hl"], :], rhs=PT[p][:, lo:512],
                                                    start=(j == 0), stop=(j == nj - 1)),
                           R=[dV[j], dPT[p]], W=[PD[acc]])
                        if j == nj - 1:
                            M["done"](acc, mi == len(maps) - 1)
                        pop_pending()

                def wout_group(c, nh, t, half):
                    pb = nxt("x", 2, 0)
                    for hl in range(nh):
                        op("pe", lambda e: e.matmul(PB[pb][:, :], lhsT=OT[0:64, hl, t * 128:(t + 1) * 128],
                                                    rhs=wo[0:64, hl, half * 512:(half + 1) * 512], start=(hl == 0), stop=(hl == nh - 1)),
                           R=[dOT[hl], dwo], W=[PD[pb]], inc=(hl == nh - 1))
                    i = 4 * c + t
                    op("dve", lambda e: e.tensor_tensor(out=xs[:, i, half * 512:(half + 1) * 512], in0=PB[pb][:, :],
                                                        in1=xs[:, i, half * 512:(half + 1) * 512], op=ALU.add),
                       R=[PD[pb]], W=[XD[i]])

                def wout_phase(c, nh):
                    for t in range(4):
                        for half in range(2):
                            if c < 3:
                                pending.append([1, lambda c=c, nh=nh, t=t, half=half: wout_group(c, nh, t, half)])
                            else:
                                wout_group(c, nh, t, half)

                def load_wo(row0, nh):
                    dma("pool", wo[0:64, 0:nh, :], w_out[l, row0:row0 + 64 * nh, :].rearrange("(h d) n -> d h n", d=64), W=[dwo])
                    for hl in range(nh):
                        op("pool", lambda e: e.tensor_tensor(out=wo[0:64, hl, :], in0=wo[0:64, hl, :], in1=gab[0:64, :], op=ALU.mult),
                           R=[dGAB], W=[dwo])

                def proj_fm(pb, wt, dw, col0, m, c):
                    for k in range(8):
                        op("pe", lambda e: e.matmul(PB[pb][0:m, :], lhsT=wt[:, k, col0:col0 + m], rhs=hT[:, k, c * 512:(c + 1) * 512],
                                                    start=(k == 0), stop=(k == 7)),
                           R=[dw] + HD[4 * c:4 * c + 4], W=[PD[pb]], inc=(k == 7))

                def proj_tm(pb, wt, dw, col0, n, i):
                    for k in range(8):
                        op("pe", lambda e: e.matmul(PB[pb][:, 0:n], lhsT=hT[:, k, i * 128:(i + 1) * 128], rhs=wt[:, k, col0:col0 + n],
                                                    start=(k == 0), stop=(k == 7)),
                           R=[dw] + HD[i:i + 1], W=[PD[pb]], inc=(k == 7))

                def evac(eng, out, in_, R, W):
                    if eng == "act":
                        op("act", lambda e: e.activation(out=out, in_=in_, func=AF.Identity), R=R, W=W)
                    else:
                        op(eng, lambda e: e.tensor_copy(out=out, in_=in_), R=R, W=W)

                def causal_fix(r):
                    return [(r, negmask[:, :])] if r >= 0 else []

                for sg in range(2):
                    heads = [2 * sg, 2 * sg + 1]
                    dma("pool", wkv[:, :, 0:128], win_v[:, :, O_DK + 128 * sg:O_DK + 128 * sg + 128], W=[dwkv])
                    dma("pool", wkv[:, :, 128:256], win_v[:, :, O_DV + 128 * sg:O_DV + 128 * sg + 128], W=[dwkv])
                    dma("pool", wq[:, :, 0:128], win_v[:, :, O_DQ + 128 * sg:O_DQ + 128 * sg + 128], W=[dwq])
                    load_wo(128 * sg, 2)
                    for c in range(4):
                        for hl in range(2):
                            pbx = nxt("x", 2, 0)
                            proj_fm(pbx, wkv, dwkv, 64 * hl, 64, c)
                            evac("dve", KT[0:64, hl, c * 512:(c + 1) * 512], PB[pbx][0:64, :], [PD[pbx]], [dKT[hl][c]])
                        for t in range(4):
                            i = 4 * c + t
                            pby = nxt("s", 3, 2)
                            proj_tm(pby, wkv, dwkv, 128, 128, i)
                            op("act", lambda e: e.activation(out=V[:, i, 0:2, 0:64], in_=PB[pby][:, 0:128].rearrange("p (h d) -> p h d", h=2), func=AF.Identity),
                               R=[PD[pby]], W=[dV[i]])
                    def qprep(c):
                        for hl in range(2):
                            pbx = nxt("x", 2, 0)
                            proj_fm(pbx, wq, dwq, 64 * hl, 64, c)
                            evac("dve", QT[0:64, hl, c % 2, :], PB[pbx][0:64, :], [PD[pbx]], [dQT[hl][c % 2]])
                            pop_pending()
                    qprep(0)
                    for c in range(4):
                        maps = []
                        for hl in range(2):
                            h = heads[hl]
                            om = []
                            for m in range(2):
                                def act_fn(j, p, sb_, lo, h=h):
                                    op("act", lambda e: e.activation(out=PT[p][:, lo:512], in_=PB[sb_][:, lo:512], func=AF.Exp,
                                                                     bias=b31[:, h:h + 1], scale=32 ** -0.5),
                                       R=[PD[sb_], dB31], W=[dPT[p]])

                                def fix_fn(r, h=h):
                                    return [(r + dlt, mt5[:, h, dlt, :]) for dlt in range(2) if 0 <= r + dlt <= 3]

                                def done(acc, last, m=m, hl=hl, om=om):
                                    fo = nxt("f", 6, 0)

                                    def dest(acc_pb, f2, fo=fo):
                                        op("dve", lambda e: e.tensor_tensor(out=fa[fo][0:64, :], in0=PB[acc_pb][0:64, :], in1=fa[f2][0:64, :],
                                                                            op=ALU.mult), R=[PD[acc_pb], dfa[f2]], W=[dfa[fo]])
                                    normalize_to(acc, dest, last)
                                    om.append(fo)
                                    if m == 0:
                                        return
                                    fo = nxt("f", 6, 0)
                                    while fo in om:
                                        fo = nxt("f", 6, 0)
                                    fs = nxt("f", 6, 0)
                                    while fs in om or fs == fo:
                                        fs = nxt("f", 6, 0)

                                    def stage_d(om=om, fo=fo, fs=fs):
                                        op("dve", lambda e: e.scalar_tensor_tensor(out=fa[fo][0:64, :], in0=fa[om[1]][0:64, :], scalar=neglam[0:64, 0:1],
                                                                                   in1=fa[om[0]][0:64, :], op0=ALU.mult, op1=ALU.add),
                                           R=[dfa[om[0]], dfa[om[1]], dLAM], W=[dfa[fo]])
                                        op("act", lambda e: e.activation(out=fa[fs][0:64, :], in_=fa[fo][0:64, :], func=AF.Square), R=[dfa[fo]], W=[dfa[fs]])

                                    def stage_e(fo=fo, fs=fs, hl=hl):
                                        op("pe", lambda e: e.matmul(PB[7][0:64, :], lhsT=onesf[0:64, 0:64], rhs=fa[fs][0:64, :], start=True, stop=True),
                                           R=[dCONST, dfa[fs]], W=[PD[7]])
                                        op("act", lambda e: e.activation(out=fa[fs][0:64, :], in_=PB[7][0:64, :], func=AF.Ln, bias=epsc[0:64, 0:1],
                                                                         scale=1.0 / 64), R=[PD[7], dCONST], W=[dfa[fs]])
                                        op("act", lambda e: e.activation(out=fa[fs][0:64, :], in_=fa[fs][0:64, :], func=AF.Exp, scale=-0.5), R=[dfa[fs]], W=[dfa[fs]])
                                        op("dve", lambda e: e.scalar_tensor_tensor(out=OT[0:64, hl, :], in0=fa[fo][0:64, :], scalar=gsub[0:64, 0:1],
                                                                                   in1=fa[fs][0:64, :], op0=ALU.mult, op1=ALU.mult),
                                           R=[dfa[fo], dfa[fs], dGSUB], W=[dOT[hl]])
                                    pending.append([1, stage_d, {"f%d" % om[0], "f%d" % om[1], "f%d" % fo, "f%d" % fs}])
                                    pending.append([2, stage_e, {"f%d" % fo, "f%d" % fs}])
                                maps.append(dict(kfn=lambda j, m=m, hl=hl: KT[32 * m:32 * m + 32, hl, j * 128:(j + 1) * 128], kdeps=dKT[hl],
                                                 qfn=lambda lo, m=m, hl=hl, c=c: QT[32 * m:32 * m + 32, hl, c % 2, lo:512], qdep=dQT[hl][c % 2],
                                                 hl=hl, act_fn=act_fn, fix=fix_fn, done=done))
                        attn_chunk(c, maps)
                        if c < 3:
                            qprep(c + 1)
                        else:
                            pop_pending(all_=True)
                        wout_phase(c, 2)
                if stage == "diff":
                    S.barrier()
                    return

                for sg in range(2):
                    h0 = 3 * sg
                    dma("pool", wkv[:, :, 0:192], win_v[:, :, O_FK + 64 * h0:O_FK + 64 * h0 + 192], W=[dwkv])
                    dma("pool", wkv[:, :, 192:384], win_v[:, :, O_FV + 64 * h0:O_FV + 64 * h0 + 192], W=[dwkv])
                    dma("pool", wkv[:, :, 384:387], win_v[:, :, O_FF + h0:O_FF + h0 + 3], W=[dwkv])
                    op("pool", lambda e: e.memset(wq[:, :, :], 0.0), W=[dwq])
                    for hl in range(3):
                        dma("pool", wq[:, :, 65 * hl:65 * hl + 64], win_v[:, :, O_FQ + 64 * (h0 + hl):O_FQ + 64 * (h0 + hl) + 64], W=[dwq])
                    load_wo(256 + 64 * h0, 3)
                    dma("sp", negb[:], b_forget[l:l + 1, h0:h0 + 3].rearrange("a h -> h a"), W=[dnegb], allow_slow_non_contiguous=True)
                    op("dve", lambda e: e.tensor_scalar(out=negb[:], in0=negb[:], scalar1=-1.0, scalar2=None, op0=ALU.mult), R=[dnegb], W=[dnegb])
                    for hl in range(3):
                        op("pool", lambda e: e.memset(KT[64:65, hl, :], 1.0), W=dKT[hl])
                    for c in range(4):
                        for hl in range(3):
                            pbx = nxt("x", 2, 0)
                            proj_fm(pbx, wkv, dwkv, 64 * hl, 64, c)
                            evac("dve", KT[0:64, hl, c * 512:(c + 1) * 512], PB[pbx][0:64, :], [PD[pbx]], [dKT[hl][c]])
                        for t in range(4):
                            i = 4 * c + t
                            pby = nxt("s", 3, 2)
                            proj_tm(pby, wkv, dwkv, 192, 192, i)
                            op("act", lambda e: e.activation(out=V[:, i, 0:3, 0:64], in_=PB[pby][:, 0:192].rearrange("p (h d) -> p h d", h=3), func=AF.Identity),
                               R=[PD[pby]], W=[dV[i]])
                        proj_fm(0, wkv, dwkv, 384, 3, c)
                        op("act", lambda e: e.activation(out=fa[0][0:3, :], in_=PB[0][0:3, :], func=AF.Exp, bias=negb[:, 0:1], scale=-1.0),
                           R=[PD[0], dnegb], W=[dfa[0]])
                        op("act", lambda e: e.activation(out=fa[1][0:3, :], in_=fa[0][0:3, :], func=AF.Ln, bias=1.0), R=[dfa[0]], W=[dfa[1]])
                        if c == 0:
                            op("dve", lambda e: e.tensor_tensor_scan(out=fa[2][0:3, :], data0=ones3[:, :], data1=fa[1][0:3, :], initial=0.0,
                                                                     op0=ALU.mult, op1=ALU.add), R=[dfa[1], dcarry], W=[dfa[2]])
                        else:
                            op("dve", lambda e: e.tensor_tensor_scan(out=fa[2][0:3, :], data0=ones3[:, :], data1=fa[1][0:3, :],
                                                                     initial=carry[:, 0:1], op0=ALU.mult, op1=ALU.add),
                               R=[dfa[1], dcarry], W=[dfa[2]])
                        op("dve", lambda e: e.tensor_copy(out=carry[:, 0:1], in_=fa[2][0:3, 511:512]), R=[dfa[2]], W=[dcarry])
                        op("dve", lambda e: e.tensor_scalar(out=gq_f[0:3, c * 512:(c + 1) * 512], in0=fa[2][0:3, :], scalar1=fa[2][0:3, 0:1], scalar2=-8.0,
                                                            op0=ALU.subtract, op1=ALU.mult), R=[dfa[2]], W=[dgqf])
                        for t in range(4):
                            op("pe", lambda e: e.transpose(out=PB[1][:, 3 * t:3 * t + 3], in_=fa[2][0:3, t * 128:(t + 1) * 128],
                                                           identity=identf[0:3, 0:3]), R=[dfa[2], dCONST], W=[PD[1]], inc=(t == 3))
                        op("dve", lambda e: e.tensor_copy(out=ftok[:, 4 * c:4 * c + 4, :], in_=PB[1][:, 0:12].rearrange("p (t h) -> p t h", h=3)),
                           R=[PD[1]], W=[dftok])
                    op("pe", lambda e: e.matmul(PB[0][:, 0:48], lhsT=onesf[0:1, 0:128], rhs=ftok[0:1, :, :].rearrange("p t h -> p (t h)"),
                                                start=True, stop=True), R=[dCONST, dftok], W=[PD[0]])
                    op("dve", lambda e: e.tensor_copy(out=cb[:, :, :], in_=PB[0][:, 0:48].rearrange("p (t h) -> p t h", h=3)), R=[PD[0]], W=[dcb])
                    for hl in range(3):
                        for c in range(4):
                            op("dve", lambda e: e.tensor_scalar(out=biask[:, hl, c, 0:4 * c + 4], in0=ftok[:, 0:4 * c + 4, hl], scalar1=cb[:, 4 * c, hl:hl + 1],
                                                                scalar2=None, op0=ALU.subtract), R=[dftok, dcb], W=[dbk])
                    fring[:] = [2, 4]

                    def qprep(c):
                        for hl in range(3):
                            pbx = nxt("x", 2, 0)
                            for k in range(8):
                                op("pe", lambda e: e.matmul(PB[pbx][0:65, :], lhsT=wq[:, k, 65 * hl:65 * hl + 65], rhs=hT[:, k, c * 512:(c + 1) * 512],
                                                            start=(k == 0), stop=False),
                                   R=[dwq] + HD[4 * c:4 * c + 4], W=[PD[pbx]], inc=False)
                            op("pe", lambda e: e.matmul(PB[pbx][0:65, :], lhsT=selg[0:3, hl, :], rhs=gq_f[0:3, c * 512:(c + 1) * 512], start=False, stop=True),
                               R=[dgqf], W=[PD[pbx]])
                            evac("dve", QT[0:65, hl, c % 2, :], PB[pbx][0:65, :], [PD[pbx]], [dQT[hl][c % 2]])
                            pop_pending()
                    qprep(0)
                    for c in range(4):
                        maps = []
                        for hl in range(3):
                            def act_fn(j, p, sb_, lo, hl=hl, c=c):
                                op("act", lambda e: e.activation(out=PT[p][:, lo:512], in_=PB[sb_][:, lo:512],
                                                                 func=AF.Exp, bias=biask[:, hl, c, j:j + 1], scale=64 ** -0.5),
                                   R=[PD[sb_], dbk], W=[dPT[p]])

                            def dest(acc_pb, f2, hl=hl):
                                op("dve", lambda e: e.tensor_tensor(out=OT[0:64, hl, :], in0=PB[acc_pb][0:64, :], in1=fa[f2][0:64, :],
                                                                    op=ALU.mult), R=[PD[acc_pb], dfa[f2]], W=[dOT[hl]])
                            maps.append(dict(kfn=lambda j, hl=hl: KT[0:65, hl, j * 128:(j + 1) * 128], kdeps=dKT[hl],
                                             qfn=lambda lo, hl=hl, c=c: QT[0:65, hl, c % 2, lo:512], qdep=dQT[hl][c % 2],
                                             hl=hl, act_fn=act_fn, fix=causal_fix, done=lambda acc, last, dest=dest: normalize_to(acc, dest, last)))
                        attn_chunk(c, maps)
                        if c < 3:
                            qprep(c + 1)
                        else:
                            pop_pending(all_=True)
                        wout_phase(c, 3)
                if stage == "fox":
                    S.barrier()
                    return

                dma("sp", wukv_f[:], w_ukv[l], W=[dwukv])
                op("pool", lambda e: e.tensor_scalar(out=wukv_b[:], in0=wukv_f[:], scalar1=gkv[:, 0:1], scalar2=None, op0=ALU.mult),
                   R=[dgn], W=[dwukv])
                dma("sp", wuq_f[:], w_uq[l].rearrange("(r p) n -> p r n", p=128), W=[dwuq])
                for r_ in range(2):
                    op("pool", lambda e: e.tensor_scalar(out=wuq_b[:, r_, :], in0=wuq_f[:, r_, :], scalar1=gq[:, r_:r_ + 1], scalar2=None,
                                                         op0=ALU.mult), R=[dgn], W=[dwuq])
                for sg in range(2):
                    h0 = 3 * sg
                    dma("pool", wkv[:, :, 0:160], win_v[:, :, O_CKV:O_CKV + 160], W=[dwkv])
                    dma("pool", wq[:, :, 0:256], win_v[:, :, O_CQ:O_CQ + 256], W=[dwq])
                    load_wo(640 + 64 * h0, 3)
                    for c in range(4):
                        proj_fm(0, wkv, dwkv, 0, 128, c)
                        evac("dve", ckvT[:, :], PB[0][:, :], [PD[0]], [dckvT])
                        op("act", lambda e: e.activation(out=fa[0][:, :], in_=PB[0][:, :], func=AF.Square), R=[PD[0]], W=[dfa[0]])
                        op("pe", lambda e: e.matmul(PB[1][:, :], lhsT=onesf[:, :], rhs=fa[0][:, :], start=True, stop=True),
                           R=[dCONST, dfa[0]], W=[PD[1]])
                        op("act", lambda e: e.activation(out=fa[1][:, :], in_=PB[1][:, :], func=AF.Ln, bias=epsc[:, 0:1], scale=1.0 / 128),
                           R=[PD[1], dCONST], W=[dfa[1]])
                        op("act", lambda e: e.activation(out=fa[1][:, :], in_=fa[1][:, :], func=AF.Exp, scale=-0.5), R=[dfa[1]], W=[dfa[1]])
                        for t in range(4):
                            op("pe", lambda e: e.matmul(PB[1][:, t:t + 1], lhsT=fa[0][:, t * 128:(t + 1) * 128], rhs=onesf[:, 0:1],
                                                        start=True, stop=True), R=[dCONST, dfa[0]], W=[PD[1]], inc=(t == 3))
                        op("act", lambda e: e.activation(out=rtok[:, :], in_=PB[1][:, 0:4], func=AF.Ln, bias=epsc[:, 0:1], scale=1.0 / 128),
                           R=[PD[1], dCONST], W=[drtok])
                        op("act", lambda e: e.activation(out=rtok[:, :], in_=rtok[:, :], func=AF.Exp, scale=-0.5), R=[drtok], W=[drtok])
                        for hl in range(3):
                            h = h0 + hl
                            op("pe", lambda e: e.matmul(PB[0][0:64, :], lhsT=wukv_b[:, h * 128:h * 128 + 64], rhs=ckvT[:, :], start=True, stop=True),
                               R=[dwukv, dckvT], W=[PD[0]])
                            op("dve", lambda e: e.tensor_tensor(out=KT[0:64, hl, c * 512:(c + 1) * 512], in0=PB[0][0:64, :], in1=fa[1][0:64, :],
                                                                op=ALU.mult), R=[PD[0], dfa[1]], W=[dKT[hl][c]])
                        for t in range(4):
                            i = 4 * c + t
                            wv3 = wukv_b[:, h0 * 128:(h0 + 3) * 128].rearrange("p (h d) -> p h d", h=3)[:, :, 64:128]
                            op("pe", lambda e: e.matmul(PB[0][:, 0:192].rearrange("p (h d) -> p h d", h=3), lhsT=ckvT[:, t * 128:(t + 1) * 128], rhs=wv3,
                                                        start=True, stop=True), R=[dwukv, dckvT], W=[PD[0]])
                            op("dve", lambda e: e.tensor_scalar(out=V[:, i, 0:3, 0:64], in0=PB[0][:, 0:192].rearrange("p (h d) -> p h d", h=3),
                                                                scalar1=rtok[:, t:t + 1], scalar2=None, op0=ALU.mult),
                               R=[PD[0], drtok], W=[dV[i]])
                            for k in range(8):
                                op("pe", lambda e: e.matmul(PB[1][:, 32 * t:32 * t + 32], lhsT=hT[:, k, i * 128:(i + 1) * 128], rhs=wkv[:, k, 128:160],
                                                            start=(k == 0), stop=(k == 7)),
                                   R=[dwkv] + HD[i:i + 1], W=[PD[1]], inc=(k == 7))
                        krv = PB[1][:, 0:128].rearrange("p (t d) -> p t d", t=4)
                        cos4, sin4 = cs[:, 4 * c:4 * c + 4, 0:16], cs[:, 4 * c:4 * c + 4, 16:32]
                        op("dve", lambda e: e.tensor_tensor(out=rk[0][:, :, :], in0=krv[:, :, 0:16], in1=cos4, op=ALU.mult), R=[PD[1], dCS], W=[drt])
                        op("dve", lambda e: e.tensor_tensor(out=rk[1][:, :, :], in0=krv[:, :, 16:32], in1=sin4, op=ALU.mult), R=[PD[1], dCS], W=[drt])
                        op("dve", lambda e: e.tensor_tensor(out=rk[2][:, :, :], in0=krv[:, :, 0:16], in1=sin4, op=ALU.mult), R=[PD[1], dCS], W=[drt])
                        op("dve", lambda e: e.tensor_tensor(out=rk[3][:, :, :], in0=krv[:, :, 16:32], in1=cos4, op=ALU.mult), R=[PD[1], dCS], W=[drt])
                        op("dve", lambda e: e.tensor_tensor(out=krp4[:, :, 64:80], in0=rk[0][:, :, :], in1=rk[1][:, :, :], op=ALU.subtract), R=[drt], W=[dkrp])
                        op("dve", lambda e: e.tensor_tensor(out=krp4[:, :, 80:96], in0=rk[2][:, :, :], in1=rk[3][:, :, :], op=ALU.add), R=[drt], W=[dkrp])
                        pv = PB[7][:, :].bitcast(BF16)
                        for t in range(4):
                            op("pe", lambda e: e.transpose(out=pv[0:96, t * 128:(t + 1) * 128], in_=krp4[:, t, :], identity=identb[:]),
                               R=[dkrp, dCONST], W=[PD[7]], inc=(t == 3))
                        pv = PB[7][:, :].bitcast(BF16)
                        for hl in range(3):
                            evac("act" if hl % 2 == 0 else "dve", KT[64:96, hl, c * 512:(c + 1) * 512], pv[64:96, 0:512], [PD[7]], [dKT[hl][c]])
                    def qprep(c):
                        for r_ in range(2):
                            proj_fm(r_, wq, dwq, 128 * r_, 128, c)
                            evac("dve", cqT[:, r_, :], PB[r_][:, :], [PD[r_]], [dcqT])
                            op("act", lambda e: e.activation(out=fa[r_][:, :], in_=PB[r_][:, :], func=AF.Square), R=[PD[r_]], W=[dfa[r_]])
                        for t in range(4):
                            for r_ in range(2):
                                op("pe", lambda e: e.matmul(PB[1][:, t:t + 1], lhsT=fa[r_][:, t * 128:(t + 1) * 128], rhs=onesf[:, 0:1],
                                                            start=(r_ == 0), stop=(r_ == 1)), R=[dCONST, dfa[r_]], W=[PD[1]], inc=(t == 3 and r_ == 1))
                        op("act", lambda e: e.activation(out=rtok[:, :], in_=PB[1][:, 0:4], func=AF.Ln, bias=epsc[:, 0:1], scale=1.0 / 256),
                           R=[PD[1], dCONST], W=[drtok])
                        op("act", lambda e: e.activation(out=rtok[:, :], in_=rtok[:, :], func=AF.Exp, scale=-0.5), R=[drtok], W=[drtok])
                        pq = [PB[0][:, :].bitcast(BF16), PB[1][:, :].bitcast(BF16), PB[7][:, :].bitcast(BF16)]
                        pqd = [PD[0], PD[1], PD[7]]
                        for t in range(4):
                            sb_ = nxt("s", 3, 2)
                            for r_ in range(2):
                                op("pe", lambda e: e.matmul(PB[sb_][:, 0:288], lhsT=cqT[:, r_, t * 128:(t + 1) * 128], rhs=wuq_b[:, r_, h0 * 96:(h0 + 3) * 96],
                                                            start=(r_ == 0), stop=(r_ == 1)), R=[dcqT, dwuq], W=[PD[sb_]], inc=(r_ == 1))
                            op("act", lambda e: e.activation(out=qsb4[:, t, :, :], in_=PB[sb_][:, 0:288].rearrange("p (h d) -> p h d", h=3), func=AF.Identity,
                                                             scale=rtok[:, t:t + 1]), R=[PD[sb_], drtok], W=[dqsb])

                        def b43(a_):
                            l_ = [list(v) for v in a_.ap]
                            return bass.AP(tensor=a_.tensor, offset=a_.offset, ap=[l_[0], l_[1], [0, 3], l_[2]])
                        cos43, sin43 = b43(cs[:, 4 * c:4 * c + 4, 0:16]), b43(cs[:, 4 * c:4 * c + 4, 16:32])
                        op("pool", lambda e: e.tensor_copy(out=qrot4[:, :, :, 0:64], in_=qsb4[:, :, :, 0:64]), R=[dqsb], W=[dqrot])
                        op("dve", lambda e: e.tensor_tensor(out=rq[0][:], in0=qsb4[:, :, :, 64:80], in1=cos43, op=ALU.mult), R=[dqsb, dCS], W=[drt])
                        op("dve", lambda e: e.tensor_tensor(out=rq[1][:], in0=qsb4[:, :, :, 80:96], in1=sin43, op=ALU.mult), R=[dqsb, dCS], W=[drt])
                        op("dve", lambda e: e.tensor_tensor(out=rq[2][:], in0=qsb4[:, :, :, 64:80], in1=sin43, op=ALU.mult), R=[dqsb, dCS], W=[drt])
                        op("dve", lambda e: e.tensor_tensor(out=rq[3][:], in0=qsb4[:, :, :, 80:96], in1=cos43, op=ALU.mult), R=[dqsb, dCS], W=[drt])
                        op("dve", lambda e: e.tensor_tensor(out=qrot4[:, :, :, 64:80], in0=rq[0][:], in1=rq[1][:], op=ALU.subtract), R=[drt], W=[dqrot])
                        op("dve", lambda e: e.tensor_tensor(out=qrot4[:, :, :, 80:96], in0=rq[2][:], in1=rq[3][:], op=ALU.add), R=[drt], W=[dqrot])
                        for t in range(4):
                            for hl in range(3):
                                op("pe", lambda e: e.transpose(out=pq[hl][0:96, t * 128:(t + 1) * 128], in_=qrot4[:, t, hl, :], identity=identb[:]),
                                   R=[dqrot, dCONST], W=[pqd[hl]])
                        for hl in range(3):
                            evac("act" if hl % 2 == 0 else "dve", QT[0:96, hl, c % 2, :], pq[hl][0:96, 0:512], [pqd[hl]], [dQT[hl][c % 2]])
                        pop_pending()
                    qprep(0)
                    for c in range(4):
                        maps = []
                        for hl in range(3):
                            def act_fn(j, p, sb_, lo):
                                op("act", lambda e: e.activation(out=PT[p][:, lo:512], in_=PB[sb_][:, lo:512], func=AF.Exp, scale=96 ** -0.5),
                                   R=[PD[sb_]], W=[dPT[p]])

                            def dest(acc_pb, f2, hl=hl):
                                op("dve", lambda e: e.tensor_tensor(out=OT[0:64, hl, :], in0=PB[acc_pb][0:64, :], in1=fa[f2][0:64, :],
                                                                    op=ALU.mult), R=[PD[acc_pb], dfa[f2]], W=[dOT[hl]])
                            maps.append(dict(kfn=lambda j, hl=hl: KT[0:96, hl, j * 128:(j + 1) * 128], kdeps=dKT[hl],
                                             qfn=lambda lo, hl=hl, c=c: QT[0:96, hl, c % 2, lo:512], qdep=dQT[hl][c % 2],
                                             hl=hl, act_fn=act_fn, fix=causal_fix, done=lambda acc, last, dest=dest: normalize_to(acc, dest, last)))
                        attn_chunk(c, maps)
                        if c < 3:
                            qprep(c + 1)
                        else:
                            pop_pending(all_=True)
                        wout_phase(c, 3)
                S.barrier()

        def ffn(l):
            moe = (l % 2 == 1)
            with ExitStack() as s1:
                if moe:
                    rws = sbt(s1, "rws", (128, 8, NEXP), F32); drws = Dep()
                    logits = sbt(s1, "logits", (128, NT, NEXP), F32); dlog = Dep()
                    comb = sbt(s1, "comb", (128, NT, NEXP), F32); dcomb = Dep()
                    t8 = sbt(s1, "t8", (128, 8), F32); dt8 = Dep()
                    sel = sbt(s1, "sel", (128, 8), F32); ex = sbt(s1, "ex", (128, 8), F32); den = sbt(s1, "den", (128, 2), F32)
                    dma("sp", rws[:], router_w[0].rearrange("(k p) e -> p k e", p=128), W=[drws])
                    norm_phase(s1, 1, fp32_router=(rws, drws, logits, dlog))
                    for i in range(NT):
                        op("dve", lambda e: e.max(out=t8[:], in_=logits[:, i, :]), R=[dlog], W=[dt8])
                        op("dve", lambda e: e.tensor_scalar(out=sel[:], in0=logits[:, i, :], scalar1=t8[:, 1:2], scalar2=None, op0=ALU.is_ge),
                           R=[dlog, dt8], W=[dt8])
                        op("dve", lambda e: e.tensor_scalar(out=den[:, 0:1], in0=t8[:, 0:1], scalar1=-1.0, scalar2=None, op0=ALU.mult), R=[dt8], W=[dt8])
                        op("act", lambda e: e.activation(out=ex[:], in_=logits[:, i, :], func=AF.Exp, bias=den[:, 0:1], scale=1.0), R=[dlog, dt8], W=[dt8])
                        op("dve", lambda e: e.tensor_tensor(out=ex[:], in0=ex[:], in1=sel[:], op=ALU.mult), R=[dt8], W=[dt8])
                        op("dve", lambda e: e.reduce_sum(out=den[:, 1:2], in_=ex[:], axis=AX.X), R=[dt8], W=[dt8])
                        op("dve", lambda e: e.reciprocal(out=den[:, 1:2], in_=den[:, 1:2]), R=[dt8], W=[dt8])
                        op("dve", lambda e: e.tensor_scalar(out=comb[:, i, :], in0=ex[:], scalar1=den[:, 1:2], scalar2=None, op0=ALU.mult),
                           R=[dt8], W=[dcomb])
                else:
                    norm_phase(s1, 1)
                G = 4 if moe else 2
                GW = 128 * G
                wg = [sbt(s1, "wg%d" % i, (128, 8, GW), BF16) for i in range(2)]
                wu = [sbt(s1, "wu%d" % i, (128, 8, GW), BF16) for i in range(2)]
                wdf = [sbt(s1, "wdf%d" % i, (128, G, D), F32) for i in range(2)]
                wd = [sbt(s1, "wd%d" % i, (128, G, D), BF16) for i in range(2)]
                dwg = [Dep(), Dep()]; dwu = [Dep(), Dep()]; dwdf = [Dep(), Dep()]; dwd = [Dep(), Dep()]
                sg_ = [sbt(s1, "sg%d" % i, (128, 256), F32) for i in range(2)]; dsg = [Dep(), Dep()]
                aT = [sbt(s1, "aT%d" % i, (128, G, 256), BF16) for i in range(2)]; daT = [Dep(), Dep()]
                if moe:
                    groups = [(e_, g_) for e_ in range(NEXP) for g_ in range(DFE // GW)]
                else:
                    groups = [(None, g_) for g_ in range(DFF // GW)]
                cnt = 0
                prev_down = [None]
                for gi, (e_, g_) in enumerate(groups):
                    b = gi % 2
                    if moe:
                        gsrc = moe_w_gate[0, e_].rearrange("(k p) n -> p k n", p=128)[:, :, g_ * GW:(g_ + 1) * GW]
                        usrc = moe_w_up[0, e_].rearrange("(k p) n -> p k n", p=128)[:, :, g_ * GW:(g_ + 1) * GW]
                        dsrc = moe_w_down[0, e_, g_ * GW:(g_ + 1) * GW, :].rearrange("(j p) n -> p j n", p=128)
                    else:
                        gsrc = ffn_w_gate[0].rearrange("(k p) n -> p k n", p=128)[:, :, g_ * GW:(g_ + 1) * GW]
                        usrc = ffn_w_up[0].rearrange("(k p) n -> p k n", p=128)[:, :, g_ * GW:(g_ + 1) * GW]
                        dsrc = ffn_w_down[0, g_ * GW:(g_ + 1) * GW, :].rearrange("(j p) n -> p j n", p=128)
                    dma("pool", wg[b][:], gsrc, W=[dwg[b]])
                    dma("pool", wu[b][:], usrc, W=[dwu[b]])
                    dma("sp", wdf[b][:], dsrc, W=[dwdf[b]])
                    for j in range(G):
                        op("pool", lambda e: e.tensor_tensor(out=wd[b][:, j, :], in0=wdf[b][:, j, :], in1=gfb[:, :], op=ALU.mult),
                           R=[dwdf[b], dGFB], W=[dwd[b]])
                    for T in range(8):
                        ab = cnt % 2
                        cnt += 1
                        for j in range(G):
                            pg, pu = 2 * (j % 2), 2 * (j % 2) + 1
                            for k in range(8):
                                op("pe", lambda e: e.matmul(PB[pg][:, 0:256], lhsT=wg[b][:, k, j * 128:(j + 1) * 128], rhs=hT[:, k, T * 256:(T + 1) * 256],
                                                            start=(k == 0), stop=(k == 7)), R=[dwg[b]] + HD[2 * T:2 * T + 2], W=[PD[pg]], inc=(k == 7))
                            for k in range(8):
                                op("pe", lambda e: e.matmul(PB[pu][:, 0:256], lhsT=wu[b][:, k, j * 128:(j + 1) * 128], rhs=hT[:, k, T * 256:(T + 1) * 256],
                                                            start=(k == 0), stop=(k == 7)), R=[dwu[b]] + HD[2 * T:2 * T + 2], W=[PD[pu]], inc=(k == 7))
                            op("act", lambda e: e.activation(out=sg_[j % 2][:, :], in_=PB[pg][:, 0:256], func=AF.Silu), R=[PD[pg]], W=[dsg[j % 2]])
                            op("dve", lambda e: e.tensor_tensor(out=aT[ab][:, j, :], in0=PB[pu][:, 0:256], in1=sg_[j % 2][:, :], op=ALU.mult),
                               R=[PD[pu], dsg[j % 2]], W=[daT[ab]])
                            if j == 0 and prev_down[0] is not None:
                                prev_down[0]()
                                prev_down[0] = None

                        def down(T=T, ab=ab, b=b, e_=e_):
                            for t in range(2):
                                for half in range(2):
                                    pa = 4 + 2 * t + half
                                    i = 2 * T + t
                                    for j in range(G):
                                        op("pe", lambda e: e.matmul(PB[pa][:, :], lhsT=aT[ab][:, j, t * 128:(t + 1) * 128], rhs=wd[b][:, j, half * 512:(half + 1) * 512],
                                                                    start=(j == 0), stop=(j == G - 1)), R=[daT[ab], dwd[b]], W=[PD[pa]], inc=(j == G - 1))
                                    xsl = xs[:, i, half * 512:(half + 1) * 512]
                                    if moe:
                                        op("dve", lambda e: e.scalar_tensor_tensor(out=xsl, in0=PB[pa][:, :], scalar=comb[:, i, e_:e_ + 1], in1=xsl,
                                                                                   op0=ALU.mult, op1=ALU.add), R=[PD[pa], dcomb], W=[XD[i]])
                                    else:
                                        op("dve", lambda e: e.tensor_tensor(out=xsl, in0=PB[pa][:, :], in1=xsl, op=ALU.add), R=[PD[pa]], W=[XD[i]])
                        prev_down[0] = down
                if prev_down[0] is not None:
                    prev_down[0]()
                    prev_down[0] = None
                S.barrier()

        def final():
            with ExitStack() as s1:
                fnb = sbt(s1, "fnb", (128, D), F32); dfnb = Dep()
                junk = sbt(s1, "junkf", (128, D), BF16); djunk = Dep()
                dY = Dep()
                if stage == "full":
                    dma("sp", fnb[:], final_norm.partition_broadcast(128), W=[dfnb])
                    op("dve", lambda e: e.memset(ssq[:], 0.0), W=[dSSQ])
                    for i in range(NT):
                        op("act", lambda e: e.activation(out=junk[:], in_=xs[:, i, :], func=AF.Square, accum_out=ssq[:, i:i + 1]),
                           R=[XD[i]], W=[djunk, dSSQ])
                    op("act", lambda e: e.activation(out=rstd[:], in_=ssq[:], func=AF.Ln, bias=epsc[:, 0:1], scale=1.0 / D),
                       R=[dSSQ, dCONST], W=[dRSTD])
                    op("act", lambda e: e.activation(out=rstd[:], in_=rstd[:], func=AF.Exp, scale=-0.5), R=[dRSTD], W=[dRSTD])
                    for i in range(NT):
                        op("dve", lambda e: e.scalar_tensor_tensor(out=xs[:, i, :], in0=xs[:, i, :], scalar=rstd[:, i:i + 1], in1=fnb[:, :],
                                                                   op0=ALU.mult, op1=ALU.mult), R=[XD[i], dRSTD, dfnb], W=[XD[i]])
                yv = y_d.rearrange("(i p) f -> p i f", p=128)
                for q in range(4):
                    dma("sp", yv[:, 4 * q:4 * q + 4, :], xs[:, 4 * q:4 * q + 4, :], R=XD[4 * q:4 * q + 4], W=[dY])
                S.barrier()

        stop = False
        for l in range(DEPTH):
            if stage == "setup":
                break
            adaln(l)
            if stage == "adaln":
                break
            with ExitStack() as sn:
                norm_phase(sn, 0)
            if stage == "norm":
                break
            attention(l)
            if stage in ("diff", "fox", "attn0"):
                break
            ffn(l)
            if stage == "layer0":
                break
        final()
        print("instructions:", S.ninst)
    return nc


_CACHE = {}


def kernel(**inputs):
    stage = inputs.pop("_stage", "full")
    ncores = inputs.pop("_ncores", 8)
    if stage not in _CACHE:
        _CACHE[stage] = build(stage)
    nc = _CACHE[stage]
    consts = host_consts()
    shared = {k: np.ascontiguousarray(np.asarray(v, dtype=np.float32)) for k, v in inputs.items() if k not in ("x", "c")}
    shared.update(consts)
    x = np.asarray(inputs["x"], dtype=np.float32)
    c = np.asarray(inputs["c"], dtype=np.float32)
    in_maps = []
    for b in range(ncores):
        m = dict(shared)
        m["x"] = np.ascontiguousarray(x[b])
        m["c"] = np.ascontiguousarray(c[b])
        in_maps.append(m)
    res = run_bass_kernel_spmd(nc, in_maps, core_ids=list(range(ncores)))
    out = np.stack([np.asarray(r["y"], dtype=np.float32) for r in res.results], axis=0)
    return out
```

```python
import math
import types
import numpy as np
from contextlib import ExitStack
import concourse.bass as bass
import concourse.mybir as mybir
from concourse.bass_utils import run_bass_kernel_spmd

F32 = mybir.dt.float32
BF16 = mybir.dt.bfloat16
ALU = mybir.AluOpType
AF = mybir.ActivationFunctionType
AX = mybir.AxisListType

D = 1024
S_LEN = 2048
NT = 16
DEPTH = 2
IN_W = 2342
EPS = 1e-6
DFF = 2816
NEXP = 8
DFE = 3584
O_DQ, O_DK, O_DV, O_FQ, O_FK, O_FV, O_FF, O_CQ, O_CKV, O_KR = 0, 256, 512, 768, 1152, 1536, 1920, 1926, 2182, 2310


class SemRef:
    def __init__(self, sem, name):
        self.sem = sem
        self.name = name
        self.count = 0


class Eng:
    def __init__(self, name, obj, semref):
        self.name = name
        self.obj = obj
        self.sr = semref
        self.seen = {}


class Dep:
    __slots__ = ("w", "r", "excl")

    def __init__(self, excl=False):
        self.w = None
        self.r = {}
        self.excl = excl


class Sched:
    def __init__(self, nc, es, n_dma_sems=32):
        self.nc = nc
        self.E = {}
        for name, obj in [("pe", nc.tensor), ("act", nc.scalar), ("dve", nc.vector),
                          ("pool", nc.gpsimd), ("sp", nc.sync)]:
            sem = es.enter_context(nc.semaphore("s_" + name))
            self.E[name] = Eng(name, obj, SemRef(sem, name))
        self.dma_sems = {q: [SemRef(es.enter_context(nc.semaphore("d%s%d" % (q, i))), "d%s%d" % (q, i))
                             for i in range(n_dma_sems if q != "act" else 8)] for q in ("sp", "pool", "act")}
        self.dma_rr = {"sp": 0, "pool": 0, "act": 0}
        self.ninst = 0

    defer = None

    def _emit(self, eng, thunk):
        if self.defer is not None:
            self.defer[eng.name].append(thunk)
        else:
            thunk()

    @staticmethod
    def _snap(fn):
        if fn.__closure__ is None:
            return fn
        cells = tuple(types.CellType(c.cell_contents) for c in fn.__closure__)
        return types.FunctionType(fn.__code__, fn.__globals__, fn.__name__, fn.__defaults__, cells)

    def _wait(self, eng, needs):
        for sr, c in needs.items():
            if c <= 0:
                continue
            if sr is eng.sr and eng.name == "pe":
                continue
            if eng.seen.get(sr, 0) >= c:
                continue
            self._emit(eng, lambda o=eng.obj, s_=sr.sem, c_=c: o.wait_ge(s_, c_))
            eng.seen[sr] = c

    @staticmethod
    def _needs(R, W):
        needs = {}
        for d in R:
            if d.w is not None:
                sr, c = d.w
                needs[sr] = max(needs.get(sr, 0), c)
        for d in W:
            if d.w is not None:
                sr, c = d.w
                needs[sr] = max(needs.get(sr, 0), c)
            for sr, c in d.r.items():
                needs[sr] = max(needs.get(sr, 0), c)
        return needs

    def op(self, ename, fn, R=(), W=(), inc=True):
        if any(d.excl for d in R):
            W = list(W) + [d for d in R if d.excl]
            R = [d for d in R if not d.excl]
        eng = self.E[ename]
        self._wait(eng, self._needs(R, W))
        self.ninst += 1
        sr = eng.sr
        if self.defer is not None:
            fn = self._snap(fn)

        def thunk(fn=fn, o=eng.obj, s_=sr.sem, inc=inc):
            ins = fn(o)
            if inc:
                ins.then_inc(s_, 1)
        self._emit(eng, thunk)
        if inc:
            sr.count += 1
            tok = sr.count
        else:
            tok = sr.count + 1
        for d in R:
            d.r[sr] = max(d.r.get(sr, 0), tok)
        for d in W:
            d.w = (sr, tok)
            d.r = {}

    def dma(self, ename, out, in_, R=(), W=(), **kw):
        eng = self.E[ename]
        pool_ = self.dma_sems[ename]
        ds = pool_[self.dma_rr[ename]]
        self.dma_rr[ename] = (self.dma_rr[ename] + 1) % len(pool_)
        needs = self._needs(R, W)
        if ds.count > 0:
            needs[ds] = max(needs.get(ds, 0), ds.count)
        self._wait(eng, needs)

        def thunk(o=eng.obj, out=out, in_=in_, kw=kw, s_=ds.sem):
            o.dma_start(out=out, in_=in_, **kw).then_inc(s_, 16)
        self._emit(eng, thunk)
        self.ninst += 1
        ds.count += 16
        tok = ds.count
        for d in R:
            d.r[ds] = tok
        for d in W:
            d.w = (ds, tok)
            d.r = {}

    def barrier(self):
        srs = [e.sr for e in self.E.values()] + self.dma_sems["sp"] + self.dma_sems["pool"] + self.dma_sems["act"]
        for eng in self.E.values():
            needs = {sr: sr.count for sr in srs if sr is not eng.sr}
            if eng.name != "pe":
                needs[eng.sr] = eng.sr.count
            self._wait(eng, needs)


def bmid(a, n):
    l = [list(v) for v in a.ap]
    return bass.AP(tensor=a.tensor, offset=a.offset, ap=[l[0], [0, n]] + l[1:])


def host_consts():
    half = 16
    freqs = (10000.0 ** (-np.arange(half, dtype=np.float32) / half)).astype(np.float32)
    t = np.arange(S_LEN, dtype=np.float32)
    ang = (t[:, None] * freqs[None, :]).astype(np.float32)
    cs = np.concatenate([np.cos(ang), np.sin(ang)], axis=1).astype(np.float32)
    cs = np.ascontiguousarray(cs.reshape(NT, 128, 32).transpose(1, 0, 2))
    n = np.arange(-127, 256)
    nn = np.maximum(n, 0)
    nf = np.maximum(nn, 1).astype(np.float32)
    large = 16 + (np.log(nf / np.float32(16)) / np.float32(math.log(128 / 16)) * np.float32(16)).astype(np.int32)
    large = np.minimum(large, 31)
    bucket = np.where(nn < 16, nn, large)
    oh = np.zeros((32, 383), np.float32)
    oh[bucket, np.arange(383)] = 1.0
    step = np.broadcast_to((n >= 0).astype(np.float32)[None, :], (4, 383)).copy()
    neg = ((step - 1.0) * 30000.0).astype(np.float32)
    return {"cs_tab": cs, "t5_oh": oh, "t5_step": step, "t5_neg": neg}


def build(stage="full"):
    nc = bass.Bass("TRN2", target_bir_lowering=False)
    dt_in = lambda name, shape: nc.dram_tensor(name, list(shape), F32, kind="ExternalInput").ap()
    x_d = dt_in("x", (S_LEN, D))
    c_d = dt_in("c", (D,))
    w_ada = dt_in("w_ada", (DEPTH, D, 6 * D))
    b_ada = dt_in("b_ada", (DEPTH, 6 * D))
    attn_norm = dt_in("attn_norm", (DEPTH, D))
    ffn_norm = dt_in("ffn_norm", (DEPTH, D))
    w_in = dt_in("w_in", (DEPTH, D, IN_W))
    b_forget = dt_in("b_forget", (DEPTH, 6))
    diff_lambda = dt_in("diff_lambda", (DEPTH, 4, 32))
    diff_subln = dt_in("diff_subln", (DEPTH, 64))
    rel_bias = dt_in("rel_bias", (32, 4))
    mla_q_norm = dt_in("mla_q_norm", (DEPTH, 256))
    mla_kv_norm = dt_in("mla_kv_norm", (DEPTH, 128))
    w_uq = dt_in("w_uq", (DEPTH, 256, 576))
    w_ukv = dt_in("w_ukv", (DEPTH, 128, 768))
    w_out = dt_in("w_out", (DEPTH, D, D))
    ffn_w_gate = dt_in("ffn_w_gate", (1, D, DFF))
    ffn_w_up = dt_in("ffn_w_up", (1, D, DFF))
    ffn_w_down = dt_in("ffn_w_down", (1, DFF, D))
    router_w = dt_in("router_w", (1, D, NEXP))
    moe_w_gate = dt_in("moe_w_gate", (1, NEXP, D, DFE))
    moe_w_up = dt_in("moe_w_up", (1, NEXP, D, DFE))
    moe_w_down = dt_in("moe_w_down", (1, NEXP, DFE, D))
    final_norm = dt_in("final_norm", (D,))
    cs_d = dt_in("cs_tab", (128, NT, 32))
    oh_d = dt_in("t5_oh", (32, 383))
    step_d = dt_in("t5_step", (4, 383))
    neg_d = dt_in("t5_neg", (4, 383))
    y_d = nc.dram_tensor("y", [S_LEN, D], F32, kind="ExternalOutput").ap()
    mod_scr = nc.dram_tensor("mod_scr", [DEPTH, 6 * D], F32, kind="Internal")
    g_scr = nc.dram_tensor("g_scr", [4, 383], F32, kind="Internal")

    es = ExitStack()
    with es:
        S = Sched(nc, es)
        op, dma = S.op, S.dma

        uniq = [0]

        def sbt(stack, name, shape, dt):
            uniq[0] += 1
            return stack.enter_context(nc.sbuf_tensor("%s_%d" % (name, uniq[0]), list(shape), dt))

        PB = [es.enter_context(nc.psum_tensor("pb%d" % i, [128, 512], F32)) for i in range(8)]
        PD = [Dep(excl=True) for _ in range(8)]

        xs = sbt(es, "xs", (128, NT, D), F32)
        XD = [Dep() for _ in range(NT)]
        hT = sbt(es, "hT", (128, 8, S_LEN), BF16)
        HDa = [Dep() for _ in range(NT)]; HDb = [Dep() for _ in range(NT)]
        class _HD:
            def __getitem__(self, k):
                if isinstance(k, slice):
                    return HDa[k] + HDb[k]
                return HDa[k]
        HD = _HD()
        identb = sbt(es, "identb", (128, 128), BF16)
        identf = sbt(es, "identf", (128, 128), F32)
        onesf = sbt(es, "onesf", (128, 128), F32)
        cs = sbt(es, "cs", (128, NT, 32), F32)
        mt5 = sbt(es, "mt5", (128, 4, 2, 128), BF16)
        negmask = sbt(es, "negmask", (128, 128), BF16)
        b31 = sbt(es, "b31", (128, 4), F32)
        cact = sbt(es, "cact", (128, 8), F32)
        cols = sbt(es, "cols", (128, 64), F32)
        amod = sbt(es, "amod", (128, 4, 8), F32)
        gab = sbt(es, "gab", (128, D), F32)
        gfb = sbt(es, "gfb", (128, D), F32)
        rstd = sbt(es, "rstd", (128, NT), F32)
        ssq = sbt(es, "ssq", (128, NT), F32)
        epsc = sbt(es, "epsc", (128, 1), F32)
        neglam = sbt(es, "neglam", (128, 1), F32)
        gsub = sbt(es, "gsub", (64, 1), F32)
        dCONST = Dep(); dCS = Dep(); dMT5 = Dep(); dB31 = Dep(); dCACT = Dep(); dCOLS = Dep()
        dAMOD = Dep(); dGAB = Dep(); dGFB = Dep(); dRSTD = Dep(); dSSQ = Dep(); dLAM = Dep(); dGSUB = Dep()
        dMODSCR = Dep(); dGSCR = Dep()

        op("pool", lambda e: e.memset(onesf[:], 1.0), W=[dCONST])
        op("pool", lambda e: e.memset(epsc[:], EPS), W=[dCONST])
        op("pool", lambda e: e.affine_select(out=identf[:], in_=onesf[:], pattern=[[-1, 128]], compare_op=ALU.is_equal,
                                             fill=0.0, base=0, channel_multiplier=1), R=[dCONST], W=[dCONST])
        op("pool", lambda e: e.affine_select(out=identb[:], in_=onesf[:], pattern=[[-1, 128]], compare_op=ALU.is_equal,
                                             fill=0.0, base=0, channel_multiplier=1), R=[dCONST], W=[dCONST])
        zf = sbt(es, "zf", (128, 128), F32)
        op("pool", lambda e: e.memset(zf[:], 0.0), W=[dCONST])
        op("pool", lambda e: e.affine_select(out=negmask[:], in_=zf[:], pattern=[[1, 128]], compare_op=ALU.is_ge,
                                             fill=-30000.0, base=0, channel_multiplier=-1), R=[dCONST], W=[dCONST])
        dma("sp", cs[:], cs_d, W=[dCS])
        dma("sp", b31[:], rel_bias[31, :].partition_broadcast(128), W=[dB31])
        xv = x_d.rearrange("(i p) f -> p i f", p=128)
        for q in range(4):
            dma("sp", xs[:, 4 * q:4 * q + 4, :], xv[:, 4 * q:4 * q + 4, :], W=XD[4 * q:4 * q + 4])
        with ExitStack() as s0:
            crow = sbt(s0, "crow", (8, 128), F32); dcrow = Dep()
            jmat = sbt(s0, "jmat", (128, 128), F32)
            rbs = sbt(s0, "rbs", (32, 4), F32)
            ohs = sbt(s0, "ohs", (32, 383), F32)
            stp = sbt(s0, "stp", (4, 383), F32)
            nb31c = sbt(s0, "nb31c", (4, 1), F32)
            gsb = sbt(s0, "gsb", (4, 383), F32)
            hank = sbt(s0, "hank", (128, 2, 128), F32)
            dT5 = Dep(); dHK = [Dep(), Dep()]
            dma("sp", crow[:], c_d.rearrange("(k p) -> k p", p=128), W=[dcrow])
            op("pe", lambda e: e.transpose(out=PB[0][:, 0:8], in_=crow[:], identity=identf[0:8, 0:8]), R=[dcrow, dCONST], W=[PD[0]])
            op("act", lambda e: e.activation(out=cact[:], in_=PB[0][:, 0:8], func=AF.Silu), R=[PD[0]], W=[dCACT])
            op("pool", lambda e: e.affine_select(out=jmat[:], in_=onesf[:], pattern=[[1, 128]], compare_op=ALU.is_equal,
                                                 fill=0.0, base=-127, channel_multiplier=1), R=[dCONST], W=[dT5])
            dma("sp", rbs[:], rel_bias, W=[dT5])
            dma("sp", ohs[:], oh_d, W=[dT5])
            dma("sp", stp[:], step_d, W=[dT5])
            dma("sp", nb31c[:], rel_bias[31:32, :].rearrange("a h -> h a"), W=[dT5], allow_slow_non_contiguous=True)
            S32 = 32 ** 0.5
            ngt = sbt(s0, "ngt", (4, 383), F32)
            dma("sp", ngt[:], neg_d, W=[dT5])
            op("dve", lambda e: e.tensor_scalar(out=nb31c[:], in0=nb31c[:], scalar1=-S32, scalar2=None, op0=ALU.mult), R=[dT5], W=[dT5])
            op("pe", lambda e: e.matmul(PB[1][0:4, 0:383], lhsT=rbs[:, :], rhs=ohs[:, :], start=True, stop=True), R=[dT5], W=[PD[1]])
            op("act", lambda e: e.activation(out=gsb[:], in_=PB[1][0:4, 0:383], func=AF.Identity, bias=nb31c[:, 0:1], scale=S32), R=[PD[1], dT5], W=[dT5])
            op("dve", lambda e: e.tensor_tensor(out=gsb[:], in0=gsb[:], in1=stp[:], op=ALU.mult), R=[dT5], W=[dT5])
            op("dve", lambda e: e.tensor_tensor(out=gsb[:], in0=gsb[:], in1=ngt[:], op=ALU.add), R=[dT5], W=[dT5])
            dma("sp", g_scr.ap(), gsb[:], R=[dT5], W=[dGSCR])
            for h in range(4):
                for dl in range(2):
                    hk = dHK[dl]
                    dma("sp", hank[:, dl, :], bass.AP(tensor=g_scr, offset=h * 383 + 128 * dl, ap=[[1, 128], [1, 128]]),
                        R=[dGSCR], W=[hk])
                    pb = 2 + dl
                    op("pe", lambda e: e.matmul(PB[pb][:, 0:128], lhsT=jmat[:, :], rhs=hank[:, dl, :], start=True, stop=True),
                       R=[dT5, hk], W=[PD[pb]])
                    op("act", lambda e: e.activation(out=mt5[:, h, dl, :], in_=PB[pb][:, 0:128], func=AF.Identity), R=[PD[pb]], W=[dMT5])
            S.barrier()

        def adaln(l):
            with ExitStack() as s1:
                wb = [sbt(s1, "adaw%d" % i, (128, 3072), F32) for i in range(5)]
                dwb = [Dep() for _ in range(5)]
                brow = sbt(s1, "brow", (1, 6 * D), F32); dbrow = Dep()
                mrow = [sbt(s1, "mrow%d" % i, (1, 512), F32) for i in range(2)]
                dmrow = [Dep(), Dep()]
                rows = sbt(s1, "rows", (64, 128), F32); drows = Dep()
                dma("sp", brow[:], b_ada[l:l + 1, :], W=[dbrow])
                nb = 0
                for half in range(2):
                    for k in range(8):
                        b = nb % 5
                        nb += 1
                        dma(("sp", "pool", "act")[nb % 3], wb[b][:], w_ada[l, k * 128:(k + 1) * 128, half * 3072:(half + 1) * 3072], W=[dwb[b]])
                        for cb in range(6):
                            op("pe", lambda e: e.matmul(PB[cb][0:1, :], lhsT=cact[:, k:k + 1], rhs=wb[b][:, cb * 512:(cb + 1) * 512],
                                                        start=(k == 0), stop=(k == 7)),
                               R=[dCACT, dwb[b]], W=[PD[cb]], inc=(k == 7 or cb == 5))
                    for cb in range(6):
                        cc = half * 6 + cb
                        m_ = cb % 2
                        op("dve", lambda e: e.tensor_tensor(out=mrow[m_][:], in0=PB[cb][0:1, :], in1=brow[0:1, cc * 512:(cc + 1) * 512],
                                                            op=ALU.add), R=[PD[cb], dbrow], W=[dmrow[m_]])
                        dma("sp", mod_scr.ap()[l:l + 1, cc * 512:(cc + 1) * 512], mrow[m_][:], R=[dmrow[m_]], W=[dMODSCR])
                dma("sp", rows[0:48, :], mod_scr.ap()[l, :].rearrange("(j p) -> j p", p=128), R=[dMODSCR], W=[drows])
                dma("sp", rows[48:56, :], attn_norm[l, :].rearrange("(j p) -> j p", p=128), W=[drows])
                dma("sp", rows[56:64, :], ffn_norm[l, :].rearrange("(j p) -> j p", p=128), W=[drows])
                op("pe", lambda e: e.transpose(out=PB[2][:, 0:64], in_=rows[:, :], identity=identf[0:64, 0:64]),
                   R=[drows, dCONST], W=[PD[2]])
                op("act", lambda e: e.activation(out=cols[:], in_=PB[2][:, 0:64], func=AF.Identity), R=[PD[2]], W=[dCOLS])
                op("dve", lambda e: e.scalar_tensor_tensor(out=amod[:, 0, :], in0=cols[:, 8:16], scalar=1.0, in1=cols[:, 48:56],
                                                           op0=ALU.add, op1=ALU.mult), R=[dCOLS], W=[dAMOD])
                op("dve", lambda e: e.tensor_copy(out=amod[:, 1, :], in_=cols[:, 0:8]), R=[dCOLS], W=[dAMOD])
                op("dve", lambda e: e.scalar_tensor_tensor(out=amod[:, 2, :], in0=cols[:, 32:40], scalar=1.0, in1=cols[:, 56:64],
                                                           op0=ALU.add, op1=ALU.mult), R=[dCOLS], W=[dAMOD])
                op("dve", lambda e: e.tensor_copy(out=amod[:, 3, :], in_=cols[:, 24:32]), R=[dCOLS], W=[dAMOD])
                dma("sp", gab[:], mod_scr.ap()[l, 2 * D:3 * D].partition_broadcast(128), R=[dMODSCR], W=[dGAB])
                dma("sp", gfb[:], mod_scr.ap()[l, 5 * D:6 * D].partition_broadcast(128), R=[dMODSCR], W=[dGFB])
                S.barrier()

        def norm_phase(stack, which, fp32_router=None):
            ia, ish = 2 * which, 2 * which + 1
            with ExitStack() as s1:
                junk = sbt(s1, "junk", (128, D), BF16); djunk = Dep()
                if fp32_router is None:
                    xn = [sbt(s1, "xn%d" % i, (128, D), BF16) for i in range(2)]
                else:
                    xn = [sbt(s1, "xn%d" % i, (128, D), F32) for i in range(2)]
                    h2f = [sbt(s1, "h2f%d" % i, (128, 8, 128), F32) for i in range(2)]
                    dh2f = [Dep(), Dep()]
                    rws, drws, logits, dlog = fp32_router
                dxn = [Dep(), Dep()]
                op("dve", lambda e: e.memset(ssq[:], 0.0), W=[dSSQ])
                for i in range(NT):
                    op("act", lambda e: e.activation(out=junk[:], in_=xs[:, i, :], func=AF.Square, accum_out=ssq[:, i:i + 1]),
                       R=[XD[i]], W=[djunk, dSSQ])
                op("act", lambda e: e.activation(out=rstd[:], in_=ssq[:], func=AF.Ln, bias=epsc[:, 0:1], scale=1.0 / D),
                   R=[dSSQ, dCONST], W=[dRSTD])
                op("act", lambda e: e.activation(out=rstd[:], in_=rstd[:], func=AF.Exp, scale=-0.5), R=[dRSTD], W=[dRSTD])
                for i in range(NT):
                    b = i % 2
                    op("dve", lambda e: e.tensor_scalar(out=xn[b][:], in0=xs[:, i, :], scalar1=rstd[:, i:i + 1], scalar2=None,
                                                        op0=ALU.mult), R=[XD[i], dRSTD], W=[dxn[b]])
                    if fp32_router is None:
                        pb = i % 2
                        pv = PB[pb][:, :].bitcast(BF16)
                        for k in range(8):
                            op("pe", lambda e: e.transpose(out=pv[:, k * 128:(k + 1) * 128], in_=xn[b][:, k * 128:(k + 1) * 128],
                                                           identity=identb[:]), R=[dxn[b], dCONST], W=[PD[pb]], inc=(k == 7))
                        for k in range(8):
                            if i % 2 == 0:
                                op("act", lambda e: e.activation(out=hT[:, k, i * 128:(i + 1) * 128], in_=pv[:, k * 128:(k + 1) * 128],
                                                                 func=AF.Identity, scale=amod[:, ia, k:k + 1], bias=amod[:, ish, k:k + 1]),
                                   R=[PD[pb], dAMOD], W=[HDa[i]])
                            else:
                                op("dve", lambda e: e.tensor_scalar(out=hT[:, k, i * 128:(i + 1) * 128], in0=pv[:, k * 128:(k + 1) * 128],
                                                                    scalar1=amod[:, ia, k:k + 1], scalar2=amod[:, ish, k:k + 1],
                                                                    op0=ALU.mult, op1=ALU.add), R=[PD[pb], dAMOD], W=[HDb[i]])
                    else:
                        for half in range(2):
                            pb = half
                            for kk in range(4):
                                k = half * 4 + kk
                                op("pe", lambda e: e.transpose(out=PB[pb][:, kk * 128:(kk + 1) * 128], in_=xn[b][:, k * 128:(k + 1) * 128],
                                                               identity=identf[:]), R=[dxn[b], dCONST], W=[PD[pb]], inc=(kk == 3))
                            for kk in range(4):
                                k = half * 4 + kk
                                op("act", lambda e: e.activation(out=h2f[b][:, k, :], in_=PB[pb][:, kk * 128:(kk + 1) * 128],
                                                                 func=AF.Identity, scale=amod[:, ia, k:k + 1], bias=amod[:, ish, k:k + 1]),
                                   R=[PD[pb], dAMOD], W=[dh2f[b]])
                        op("dve", lambda e: e.tensor_copy(out=hT[:, :, i * 128:(i + 1) * 128], in_=h2f[b][:, :, :]), R=[dh2f[b]], W=[HDa[i], HDb[i]])
                        for k in range(8):
                            op("pe", lambda e: e.matmul(PB[2][:, 0:8], lhsT=h2f[b][:, k, :], rhs=rws[:, k, :], start=(k == 0), stop=(k == 7)),
                               R=[dh2f[b], drws], W=[PD[2]], inc=(k == 7))
                        op("dve", lambda e: e.tensor_copy(out=logits[:, i, :], in_=PB[2][:, 0:8]), R=[PD[2]], W=[dlog])
                S.barrier()

        def attention(l):
            lam_init = 0.8 - 0.6 * math.exp(-0.3 * l)
            win_v = w_in[l].rearrange("(k p) n -> p k n", p=128)
            with ExitStack() as s1:
                KT = sbt(s1, "KT", (128, 3, S_LEN), BF16); dKT = [[Dep() for _ in range(4)] for _ in range(3)]
                QT = sbt(s1, "QT", (128, 3, 2, 512), BF16); dQT = [[Dep(), Dep()] for _ in range(3)]
                V = sbt(s1, "V", (128, NT, 3, 65), BF16); dV = [Dep() for _ in range(NT)]
                OT = sbt(s1, "OT", (64, 3, 512), BF16); dOT = [Dep() for _ in range(3)]
                PT = [sbt(s1, "PT%d" % i, (128, 512), BF16) for i in range(4)]; dPT = [Dep() for _ in range(4)]
                wkv = sbt(s1, "wkv", (128, 8, 387), BF16); dwkv = Dep()
                wq = sbt(s1, "wq", (128, 8, 256), BF16); dwq = Dep()
                wo = sbt(s1, "wo", (64, 3, D), BF16); dwo = Dep()
                fa = [sbt(s1, "fa%d" % i, (128, 512), F32) for i in range(6)]; dfa = [Dep() for _ in range(6)]
                dl_b = sbt(s1, "dl_b", (128, 128), F32); ddl = Dep()
                lt = sbt(s1, "lt", (128, 8), F32)
                negb = sbt(s1, "negb", (3, 1), F32); dnegb = Dep()
                ftok = sbt(s1, "ftok", (128, NT, 3), F32); dftok = Dep()
                cb = sbt(s1, "cb", (128, NT, 3), F32); dcb = Dep()
                biask = sbt(s1, "biask", (128, 3, 4, NT), F32); dbk = Dep()
                carry = sbt(s1, "carry", (3, 1), F32); dcarry = Dep()
                ones3 = sbt(s1, "ones3", (3, 512), F32)
                gq_f = sbt(s1, "gq_f", (3, S_LEN), BF16); dgqf = Dep()
                selg = sbt(s1, "selg", (3, 3, 65), BF16)
                wukv_f = sbt(s1, "wukv_f", (128, 768), F32); wukv_b = sbt(s1, "wukv_b", (128, 768), BF16); dwukv = Dep()
                wuq_f = sbt(s1, "wuq_f", (128, 2, 576), F32); wuq_b = sbt(s1, "wuq_b", (128, 2, 576), BF16); dwuq = Dep()
                gq = sbt(s1, "gq", (128, 2), F32); gkv = sbt(s1, "gkv", (128, 1), F32); dgn = Dep()
                ckvT = sbt(s1, "ckvT", (128, 512), BF16); dckvT = Dep()
                cqT = sbt(s1, "cqT", (128, 2, 512), BF16); dcqT = Dep()
                rtok = sbt(s1, "rtok", (128, 4), F32); drtok = Dep()
                krp4 = sbt(s1, "krp4", (128, 4, 96), BF16); dkrp = Dep()
                rk = [sbt(s1, "rk%d" % i, (128, 4, 16), F32) for i in range(4)]
                qsb4 = sbt(s1, "qsb4", (128, 4, 3, 96), F32)
                qrot4 = sbt(s1, "qrot4", (128, 4, 3, 96), BF16)
                rq = [sbt(s1, "rq%d" % i, (128, 4, 3, 16), F32) for i in range(4)]
                dqsb = Dep(); dqrot = Dep(); drt = Dep()

                op("pool", lambda e: e.memset(V[:, :, :, 64:65], 1.0), W=dV)
                op("pool", lambda e: e.memset(krp4[:], 0.0), W=[dkrp])
                op("pool", lambda e: e.memset(ones3[:], 1.0), W=[dcarry])
                op("pool", lambda e: e.memset(selg[:], 0.0), W=[dgqf])
                for hl_ in range(3):
                    op("pool", lambda e: e.tensor_copy(out=selg[0:3, hl_, 64:65], in_=identf[0:3, hl_:hl_ + 1]), R=[dCONST], W=[dgqf])
                dma("sp", dl_b[:], diff_lambda[l].rearrange("a b -> (a b)").partition_broadcast(128), W=[ddl])
                op("dve", lambda e: e.tensor_tensor(out=dl_b[:, 0:32], in0=dl_b[:, 0:32], in1=dl_b[:, 32:64], op=ALU.mult), R=[ddl], W=[ddl])
                op("dve", lambda e: e.tensor_tensor(out=dl_b[:, 64:96], in0=dl_b[:, 64:96], in1=dl_b[:, 96:128], op=ALU.mult), R=[ddl], W=[ddl])
                op("dve", lambda e: e.reduce_sum(out=lt[:, 0:1], in_=dl_b[:, 0:32], axis=AX.X), R=[ddl], W=[ddl])
                op("dve", lambda e: e.reduce_sum(out=lt[:, 1:2], in_=dl_b[:, 64:96], axis=AX.X), R=[ddl], W=[ddl])
                op("act", lambda e: e.activation(out=lt[:, 2:4], in_=lt[:, 0:2], func=AF.Exp), R=[ddl], W=[ddl])
                op("dve", lambda e: e.tensor_tensor(out=lt[:, 4:5], in0=lt[:, 3:4], in1=lt[:, 2:3], op=ALU.subtract), R=[ddl], W=[ddl])
                op("dve", lambda e: e.tensor_scalar(out=neglam[:], in0=lt[:, 4:5], scalar1=-lam_init, scalar2=None, op0=ALU.add), R=[ddl], W=[dLAM])
                dma("sp", gsub[:], diff_subln[l:l + 1, :].rearrange("a d -> d a"), W=[dGSUB], allow_slow_non_contiguous=True)
                op("dve", lambda e: e.tensor_scalar(out=gsub[:], in0=gsub[:], scalar1=1.0 - lam_init, scalar2=None, op0=ALU.mult), R=[dGSUB], W=[dGSUB])
                dma("sp", gq[:], mla_q_norm[l, :].rearrange("(r p) -> p r", p=128), W=[dgn], allow_slow_non_contiguous=True)
                dma("sp", gkv[:], mla_kv_norm[l:l + 1, :].rearrange("a p -> p a"), W=[dgn], allow_slow_non_contiguous=True)

                state = {"s": 0, "p": 0, "a": 0, "f": 0, "x": 0}

                fring = [0, 6]

                def nxt(key, n, base):
                    if key == "f":
                        base, n = fring
                    v = base + state[key] % n
                    state[key] += 1
                    if key == "f":
                        ensure_free("f%d" % v)
                    elif key == "a":
                        ensure_free("a%d" % v)
                    return v

                pending = []

                def pop_pending(all_=False):
                    if all_:
                        while pending:
                            pending.pop(0)[1]()
                        return
                    if pending:
                        pending[0][0] -= 1
                        if pending[0][0] <= 0:
                            pending.pop(0)[1]()

                def ensure_free(res):
                    while any(res in p_[2] for p_ in pending if len(p_) > 2):
                        pending.pop(0)[1]()

                def normalize_to(acc_pb, dest_fn, last=False):
                    f1 = nxt("f", 6, 0)
                    if last:
                        op("act", lambda e: e.activation(out=fa[f1][64:65, :], in_=PB[acc_pb][64:65, :], func=AF.Ln), R=[PD[acc_pb]], W=[dfa[f1]])
                        op("act", lambda e: e.activation(out=fa[f1][64:65, :], in_=fa[f1][64:65, :], func=AF.Exp, scale=-1.0), R=[dfa[f1]], W=[dfa[f1]])
                    else:
                        op("dve", lambda e: e.reciprocal(out=fa[f1][64:65, :], in_=PB[acc_pb][64:65, :]), R=[PD[acc_pb]], W=[dfa[f1]])
                    f2 = nxt("f", 6, 0)

                    def stage_b():
                        op("pe", lambda e: e.matmul(PB[7][0:64, :], lhsT=onesf[64:65, 0:64], rhs=fa[f1][64:65, :], start=True, stop=True),
                           R=[dCONST, dfa[f1]], W=[PD[7]])
                        op("dve", lambda e: e.tensor_copy(out=fa[f2][0:64, :], in_=PB[7][0:64, :]), R=[PD[7]], W=[dfa[f2]])
                        dest_fn(acc_pb, f2)
                    pending.append([2 if last else 6, stage_b, {"a%d" % acc_pb, "f%d" % f1, "f%d" % f2}])

                def attn_chunk(c, maps):
                    nj = 4 * c + 4
                    seq = [(mi, j) for mi in range(len(maps)) for j in range(nj)]
                    LA = 2
                    sbank = {}
                    accs = {}

                    def qk(idx):
                        mi, j = seq[idx]
                        M = maps[mi]
                        r = j - 4 * c
                        lo = max(0, r) * 128
                        sb_ = nxt("s", 3, 2)
                        sbank[idx] = sb_
                        fx = M["fix"](r)
                        op("pe", lambda e: e.matmul(PB[sb_][:, lo:512], lhsT=M["kfn"](j), rhs=M["qfn"](lo), start=True, stop=(len(fx) == 0)),
                           R=[M["kdeps"][j // 4], M["qdep"]], W=[PD[sb_]], inc=(len(fx) == 0))
                        for n_, (rr, tile_ap) in enumerate(fx):
                            op("pe", lambda e: e.matmul(PB[sb_][:, rr * 128:(rr + 1) * 128], lhsT=identb[:, :], rhs=tile_ap,
                                                        start=False, stop=(n_ == len(fx) - 1)),
                               R=[dCONST, dMT5], W=[PD[sb_]], inc=(n_ == len(fx) - 1))
                    for idx in range(min(LA, len(seq))):
                        qk(idx)
                    for idx, (mi, j) in enumerate(seq):
                        M = maps[mi]
                        if j == 0:
                            accs[mi] = nxt("a", 2, 5)
                        acc = accs[mi]
                        r = j - 4 * c
                        lo = max(0, r) * 128
                        p = nxt("p", 4, 0)
                        M["act_fn"](j, p, sbank[idx], lo)
                        if idx + LA < len(seq):
                            qk(idx + LA)
                        op("pe", lambda e: e.matmul(PB[acc][0:65, lo:512], lhsT=V[:, j, M["hl"], :], rhs=PT[p][:, lo:512],
                                                    start=(j == 0), stop=(j == nj - 1)),
                           R=[dV[j], dPT[p]], W=[PD[acc]])
                        if j == nj - 1:
                            M["done"](acc, mi == len(maps) - 1)
                        pop_pending()

                def wout_group(c, nh, t, half):
                    pb = nxt("x", 2, 0)
                    for hl in range(nh):
                        op("pe", lambda e: e.matmul(PB[pb][:, :], lhsT=OT[0:64, hl, t * 128:(t + 1) * 128],
                                                    rhs=wo[0:64, hl, half * 512:(half + 1) * 512], start=(hl == 0), stop=(hl == nh - 1)),
                           R=[dOT[hl], dwo], W=[PD[pb]], inc=(hl == nh - 1))
                    i = 4 * c + t
                    op("dve", lambda e: e.tensor_tensor(out=xs[:, i, half * 512:(half + 1) * 512], in0=PB[pb][:, :],
                                                        in1=xs[:, i, half * 512:(half + 1) * 512], op=ALU.add),
                       R=[PD[pb]], W=[XD[i]])

                def wout_phase(c, nh):
                    for t in range(4):
                        for half in range(2):
                            if c < 3:
                                pending.append([1, lambda c=c, nh=nh, t=t, half=half: wout_group(c, nh, t, half)])
                            else:
                                wout_group(c, nh, t, half)

                def load_wo(row0, nh):
                    dma("pool", wo[0:64, 0:nh, :], w_out[l, row0:row0 + 64 * nh, :].rearrange("(h d) n -> d h n", d=64), W=[dwo])
                    for hl in range(nh):
                        op("pool", lambda e: e.tensor_tensor(out=wo[0:64, hl, :], in0=wo[0:64, hl, :], in1=gab[0:64, :], op=ALU.mult),
                           R=[dGAB], W=[dwo])

                def proj_fm(pb, wt, dw, col0, m, c):
                    for k in range(8):
                        op("pe", lambda e: e.matmul(PB[pb][0:m, :], lhsT=wt[:, k, col0:col0 + m], rhs=hT[:, k, c * 512:(c + 1) * 512],
                                                    start=(k == 0), stop=(k == 7)),
                           R=[dw] + HD[4 * c:4 * c + 4], W=[PD[pb]], inc=(k == 7))

                def proj_tm(pb, wt, dw, col0, n, i):
                    for k in range(8):
                        op("pe", lambda e: e.matmul(PB[pb][:, 0:n], lhsT=hT[:, k, i * 128:(i + 1) * 128], rhs=wt[:, k, col0:col0 + n],
                                                    start=(k == 0), stop=(k == 7)),
                           R=[dw] + HD[i:i + 1], W=[PD[pb]], inc=(k == 7))

                def evac(eng, out, in_, R, W):
                    if eng == "act":
                        op("act", lambda e: e.activation(out=out, in_=in_, func=AF.Identity), R=R, W=W)
                    else:
                        op(eng, lambda e: e.tensor_copy(out=out, in_=in_), R=R, W=W)

                def causal_fix(r):
                    return [(r, negmask[:, :])] if r >= 0 else []

                for sg in range(2):
                    heads = [2 * sg, 2 * sg + 1]
                    dma("pool", wkv[:, :, 0:128], win_v[:, :, O_DK + 128 * sg:O_DK + 128 * sg + 128], W=[dwkv])
                    dma("pool", wkv[:, :, 128:256], win_v[:, :, O_DV + 128 * sg:O_DV + 128 * sg + 128], W=[dwkv])
                    dma("pool", wq[:, :, 0:128], win_v[:, :, O_DQ + 128 * sg:O_DQ + 128 * sg + 128], W=[dwq])
                    load_wo(128 * sg, 2)
                    for c in range(4):
                        for hl in range(2):
                            pbx = nxt("x", 2, 0)
                            proj_fm(pbx, wkv, dwkv, 64 * hl, 64, c)
                            evac("dve", KT[0:64, hl, c * 512:(c + 1) * 512], PB[pbx][0:64, :], [PD[pbx]], [dKT[hl][c]])
                        for t in range(4):
                            i = 4 * c + t
                            pby = nxt("s", 3, 2)
                            proj_tm(pby, wkv, dwkv, 128, 128, i)
                            op("act", lambda e: e.activation(out=V[:, i, 0:2, 0:64], in_=PB[pby][:, 0:128].rearrange("p (h d) -> p h d", h=2), func=AF.Identity),
                               R=[PD[pby]], W=[dV[i]])
                    def qprep(c):
                        for hl in range(2):
                            pbx = nxt("x", 2, 0)
                            proj_fm(pbx, wq, dwq, 64 * hl, 64, c)
                            evac("dve", QT[0:64, hl, c % 2, :], PB[pbx][0:64, :], [PD[pbx]], [dQT[hl][c % 2]])
                            pop_pending()
                    qprep(0)
                    for c in range(4):
                        maps = []
                        for hl in range(2):
                            h = heads[hl]
                            om = []
                            for m in range(2):
                                def act_fn(j, p, sb_, lo, h=h):
                                    op("act", lambda e: e.activation(out=PT[p][:, lo:512], in_=PB[sb_][:, lo:512], func=AF.Exp,
                                                                     bias=b31[:, h:h + 1], scale=32 ** -0.5),
                                       R=[PD[sb_], dB31], W=[dPT[p]])

                                def fix_fn(r, h=h):
                                    return [(r + dlt, mt5[:, h, dlt, :]) for dlt in range(2) if 0 <= r + dlt <= 3]

                                def done(acc, last, m=m, hl=hl, om=om):
                                    fo = nxt("f", 6, 0)

                                    def dest(acc_pb, f2, fo=fo):
                                        op("dve", lambda e: e.tensor_tensor(out=fa[fo][0:64, :], in0=PB[acc_pb][0:64, :], in1=fa[f2][0:64, :],
                                                                            op=ALU.mult), R=[PD[acc_pb], dfa[f2]], W=[dfa[fo]])
                                    normalize_to(acc, dest, last)
                                    om.append(fo)
                                    if m == 0:
                                        return
                                    fo = nxt("f", 6, 0)
                                    while fo in om:
                                        fo = nxt("f", 6, 0)
                                    fs = nxt("f", 6, 0)
                                    while fs in om or fs == fo:
                                        fs = nxt("f", 6, 0)

                                    def stage_d(om=om, fo=fo, fs=fs):
                                        op("dve", lambda e: e.scalar_tensor_tensor(out=fa[fo][0:64, :], in0=fa[om[1]][0:64, :], scalar=neglam[0:64, 0:1],
                                                                                   in1=fa[om[0]][0:64, :], op0=ALU.mult, op1=ALU.add),
                                           R=[dfa[om[0]], dfa[om[1]], dLAM], W=[dfa[fo]])
                                        op("act", lambda e: e.activation(out=fa[fs][0:64, :], in_=fa[fo][0:64, :], func=AF.Square), R=[dfa[fo]], W=[dfa[fs]])

                                    def stage_e(fo=fo, fs=fs, hl=hl):
                                        op("pe", lambda e: e.matmul(PB[7][0:64, :], lhsT=onesf[0:64, 0:64], rhs=fa[fs][0:64, :], start=True, stop=True),
                                           R=[dCONST, dfa[fs]], W=[PD[7]])
                                        op("act", lambda e: e.activation(out=fa[fs][0:64, :], in_=PB[7][0:64, :], func=AF.Ln, bias=epsc[0:64, 0:1],
                                                                         scale=1.0 / 64), R=[PD[7], dCONST], W=[dfa[fs]])
                                        op("act", lambda e: e.activation(out=fa[fs][0:64, :], in_=fa[fs][0:64, :], func=AF.Exp, scale=-0.5), R=[dfa[fs]], W=[dfa[fs]])
                                        op("dve", lambda e: e.scalar_tensor_tensor(out=OT[0:64, hl, :], in0=fa[fo][0:64, :], scalar=gsub[0:64, 0:1],
                                                                                   in1=fa[fs][0:64, :], op0=ALU.mult, op1=ALU.mult),
                                           R=[dfa[fo], dfa[fs], dGSUB], W=[dOT[hl]])
                                    pending.append([1, stage_d, {"f%d" % om[0], "f%d" % om[1], "f%d" % fo, "f%d" % fs}])
                                    pending.append([2, stage_e, {"f%d" % fo, "f%d" % fs}])
                                maps.append(dict(kfn=lambda j, m=m, hl=hl: KT[32 * m:32 * m + 32, hl, j * 128:(j + 1) * 128], kdeps=dKT[hl],
                                                 qfn=lambda lo, m=m, hl=hl, c=c: QT[32 * m:32 * m + 32, hl, c % 2, lo:512], qdep=dQT[hl][c % 2],
                                                 hl=hl, act_fn=act_fn, fix=fix_fn, done=done))
                        attn_chunk(c, maps)
                        if c < 3:
                            qprep(c + 1)
                        else:
                            pop_pending(all_=True)
                        wout_phase(c, 2)
                if stage == "diff":
                    S.barrier()
                    return

                for sg in range(2):
                    h0 = 3 * sg
                    dma("pool", wkv[:, :, 0:192], win_v[:, :, O_FK + 64 * h0:O_FK + 64 * h0 + 192], W=[dwkv])
                    dma("pool", wkv[:, :, 192:384], win_v[:, :, O_FV + 64 * h0:O_FV + 64 * h0 + 192], W=[dwkv])
                    dma("pool", wkv[:, :, 384:387], win_v[:, :, O_FF + h0:O_FF + h0 + 3], W=[dwkv])
                    op("pool", lambda e: e.memset(wq[:, :, :], 0.0), W=[dwq])
                    for hl in range(3):
                        dma("pool", wq[:, :, 65 * hl:65 * hl + 64], win_v[:, :, O_FQ + 64 * (h0 + hl):O_FQ + 64 * (h0 + hl) + 64], W=[dwq])
                    load_wo(256 + 64 * h0, 3)
                    dma("sp", negb[:], b_forget[l:l + 1, h0:h0 + 3].rearrange("a h -> h a"), W=[dnegb], allow_slow_non_contiguous=True)
                    op("dve", lambda e: e.tensor_scalar(out=negb[:], in0=negb[:], scalar1=-1.0, scalar2=None, op0=ALU.mult), R=[dnegb], W=[dnegb])
                    for hl in range(3):
                        op("pool", lambda e: e.memset(KT[64:65, hl, :], 1.0), W=dKT[hl])
                    for c in range(4):
                        for hl in range(3):
                            pbx = nxt("x", 2, 0)
                            proj_fm(pbx, wkv, dwkv, 64 * hl, 64, c)
                            evac("dve", KT[0:64, hl, c * 512:(c + 1) * 512], PB[pbx][0:64, :], [PD[pbx]], [dKT[hl][c]])
                        for t in range(4):
                            i = 4 * c + t
                            pby = nxt("s", 3, 2)
                            proj_tm(pby, wkv, dwkv, 192, 192, i)
                            op("act", lambda e: e.activation(out=V[:, i, 0:3, 0:64], in_=PB[pby][:, 0:192].rearrange("p (h d) -> p h d", h=3), func=AF.Identity),
                               R=[PD[pby]], W=[dV[i]])
                        proj_fm(0, wkv, dwkv, 384, 3, c)
                        op("act", lambda e: e.activation(out=fa[0][0:3, :], in_=PB[0][0:3, :], func=AF.Exp, bias=negb[:, 0:1], scale=-1.0),
                           R=[PD[0], dnegb], W=[dfa[0]])
                        op("act", lambda e: e.activation(out=fa[1][0:3, :], in_=fa[0][0:3, :], func=AF.Ln, bias=1.0), R=[dfa[0]], W=[dfa[1]])
                        if c == 0:
                            op("dve", lambda e: e.tensor_tensor_scan(out=fa[2][0:3, :], data0=ones3[:, :], data1=fa[1][0:3, :], initial=0.0,
                                                                     op0=ALU.mult, op1=ALU.add), R=[dfa[1], dcarry], W=[dfa[2]])
                        else:
                            op("dve", lambda e: e.tensor_tensor_scan(out=fa[2][0:3, :], data0=ones3[:, :], data1=fa[1][0:3, :],
                                                                     initial=carry[:, 0:1], op0=ALU.mult, op1=ALU.add),
                               R=[dfa[1], dcarry], W=[dfa[2]])
                        op("dve", lambda e: e.tensor_copy(out=carry[:, 0:1], in_=fa[2][0:3, 511:512]), R=[dfa[2]], W=[dcarry])
                        op("dve", lambda e: e.tensor_scalar(out=gq_f[0:3, c * 512:(c + 1) * 512], in0=fa[2][0:3, :], scalar1=fa[2][0:3, 0:1], scalar2=-8.0,
                                                            op0=ALU.subtract, op1=ALU.mult), R=[dfa[2]], W=[dgqf])
                        for t in range(4):
                            op("pe", lambda e: e.transpose(out=PB[1][:, 3 * t:3 * t + 3], in_=fa[2][0:3, t * 128:(t + 1) * 128],
                                                           identity=identf[0:3, 0:3]), R=[dfa[2], dCONST], W=[PD[1]], inc=(t == 3))
                        op("dve", lambda e: e.tensor_copy(out=ftok[:, 4 * c:4 * c + 4, :], in_=PB[1][:, 0:12].rearrange("p (t h) -> p t h", h=3)),
                           R=[PD[1]], W=[dftok])
                    op("pe", lambda e: e.matmul(PB[0][:, 0:48], lhsT=onesf[0:1, 0:128], rhs=ftok[0:1, :, :].rearrange("p t h -> p (t h)"),
                                                start=True, stop=True), R=[dCONST, dftok], W=[PD[0]])
                    op("dve", lambda e: e.tensor_copy(out=cb[:, :, :], in_=PB[0][:, 0:48].rearrange("p (t h) -> p t h", h=3)), R=[PD[0]], W=[dcb])
                    for hl in range(3):
                        for c in range(4):
                            op("dve", lambda e: e.tensor_scalar(out=biask[:, hl, c, 0:4 * c + 4], in0=ftok[:, 0:4 * c + 4, hl], scalar1=cb[:, 4 * c, hl:hl + 1],
                                                                scalar2=None, op0=ALU.subtract), R=[dftok, dcb], W=[dbk])
                    fring[:] = [2, 4]

                    def qprep(c):
                        for hl in range(3):
                            pbx = nxt("x", 2, 0)
                            for k in range(8):
                                op("pe", lambda e: e.matmul(PB[pbx][0:65, :], lhsT=wq[:, k, 65 * hl:65 * hl + 65], rhs=hT[:, k, c * 512:(c + 1) * 512],
                                                            start=(k == 0), stop=False),
                                   R=[dwq] + HD[4 * c:4 * c + 4], W=[PD[pbx]], inc=False)
                            op("pe", lambda e: e.matmul(PB[pbx][0:65, :], lhsT=selg[0:3, hl, :], rhs=gq_f[0:3, c * 512:(c + 1) * 512], start=False, stop=True),
                               R=[dgqf], W=[PD[pbx]])
                            evac("dve", QT[0:65, hl, c % 2, :], PB[pbx][0:65, :], [PD[pbx]], [dQT[hl][c % 2]])
                            pop_pending()
                    qprep(0)
                    for c in range(4):
                        maps = []
                        for hl in range(3):
                            def act_fn(j, p, sb_, lo, hl=hl, c=c):
                                op("act", lambda e: e.activation(out=PT[p][:, lo:512], in_=PB[sb_][:, lo:512],
                                                                 func=AF.Exp, bias=biask[:, hl, c, j:j + 1], scale=64 ** -0.5),
                                   R=[PD[sb_], dbk], W=[dPT[p]])

                            def dest(acc_pb, f2, hl=hl):
                                op("dve", lambda e: e.tensor_tensor(out=OT[0:64, hl, :], in0=PB[acc_pb][0:64, :], in1=fa[f2][0:64, :],
                                                                    op=ALU.mult), R=[PD[acc_pb], dfa[f2]], W=[dOT[hl]])
                            maps.append(dict(kfn=lambda j, hl=hl: KT[0:65, hl, j * 128:(j + 1) * 128], kdeps=dKT[hl],
                                             qfn=lambda lo, hl=hl, c=c: QT[0:65, hl, c % 2, lo:512], qdep=dQT[hl][c % 2],
                                             hl=hl, act_fn=act_fn, fix=causal_fix, done=lambda acc, last, dest=dest: normalize_to(acc, dest, last)))
                        attn_chunk(c, maps)
                        if c < 3:
                            qprep(c + 1)
                        else:
                            pop_pending(all_=True)
                        wout_phase(c, 3)
                if stage == "fox":
                    S.barrier()
                    return

                dma("sp", wukv_f[:], w_ukv[l], W=[dwukv])
                op("pool", lambda e: e.tensor_scalar(out=wukv_b[:], in0=wukv_f[:], scalar1=gkv[:, 0:1], scalar2=None, op0=ALU.mult),
                   R=[dgn], W=[dwukv])
                dma("sp", wuq_f[:], w_uq[l].rearrange("(r p) n -> p r n", p=128), W=[dwuq])
                for r_ in range(2):
                    op("pool", lambda e: e.tensor_scalar(out=wuq_b[:, r_, :], in0=wuq_f[:, r_, :], scalar1=gq[:, r_:r_ + 1], scalar2=None,
                                                         op0=ALU.mult), R=[dgn], W=[dwuq])
                for sg in range(2):
                    h0 = 3 * sg
                    dma("pool", wkv[:, :, 0:160], win_v[:, :, O_CKV:O_CKV + 160], W=[dwkv])
                    dma("pool", wq[:, :, 0:256], win_v[:, :, O_CQ:O_CQ + 256], W=[dwq])
                    load_wo(640 + 64 * h0, 3)
                    for c in range(4):
                        proj_fm(0, wkv, dwkv, 0, 128, c)
                        evac("dve", ckvT[:, :], PB[0][:, :], [PD[0]], [dckvT])
                        op("act", lambda e: e.activation(out=fa[0][:, :], in_=PB[0][:, :], func=AF.Square), R=[PD[0]], W=[dfa[0]])
                        op("pe", lambda e: e.matmul(PB[1][:, :], lhsT=onesf[:, :], rhs=fa[0][:, :], start=True, stop=True),
                           R=[dCONST, dfa[0]], W=[PD[1]])
                        op("act", lambda e: e.activation(out=fa[1][:, :], in_=PB[1][:, :], func=AF.Ln, bias=epsc[:, 0:1], scale=1.0 / 128),
                           R=[PD[1], dCONST], W=[dfa[1]])
                        op("act", lambda e: e.activation(out=fa[1][:, :], in_=fa[1][:, :], func=AF.Exp, scale=-0.5), R=[dfa[1]], W=[dfa[1]])
                        for t in range(4):
                            op("pe", lambda e: e.matmul(PB[1][:, t:t + 1], lhsT=fa[0][:, t * 128:(t + 1) * 128], rhs=onesf[:, 0:1],
                                                        start=True, stop=True), R=[dCONST, dfa[0]], W=[PD[1]], inc=(t == 3))
                        op("act", lambda e: e.activation(out=rtok[:, :], in_=PB[1][:, 0:4], func=AF.Ln, bias=epsc[:, 0:1], scale=1.0 / 128),
                           R=[PD[1], dCONST], W=[drtok])
                        op("act", lambda e: e.activation(out=rtok[:, :], in_=rtok[:, :], func=AF.Exp, scale=-0.5), R=[drtok], W=[drtok])
                        for hl in range(3):
                            h = h0 + hl
                            op("pe", lambda e: e.matmul(PB[0][0:64, :], lhsT=wukv_b[:, h * 128:h * 128 + 64], rhs=ckvT[:, :], start=True, stop=True),
                               R=[dwukv, dckvT], W=[PD[0]])
                            op("dve", lambda e: e.tensor_tensor(out=KT[0:64, hl, c * 512:(c + 1) * 512], in0=PB[0][0:64, :], in1=fa[1][0:64, :],
                                                                op=ALU.mult), R=[PD[0], dfa[1]], W=[dKT[hl][c]])
                        for t in range(4):
                            i = 4 * c + t
                            wv3 = wukv_b[:, h0 * 128:(h0 + 3) * 128].rearrange("p (h d) -> p h d", h=3)[:, :, 64:128]
                            op("pe", lambda e: e.matmul(PB[0][:, 0:192].rearrange("p (h d) -> p h d", h=3), lhsT=ckvT[:, t * 128:(t + 1) * 128], rhs=wv3,
                                                        start=True, stop=True), R=[dwukv, dckvT], W=[PD[0]])
                            op("dve", lambda e: e.tensor_scalar(out=V[:, i, 0:3, 0:64], in0=PB[0][:, 0:192].rearrange("p (h d) -> p h d", h=3),
                                                                scalar1=rtok[:, t:t + 1], scalar2=None, op0=ALU.mult),
                               R=[PD[0], drtok], W=[dV[i]])
                            for k in range(8):
                                op("pe", lambda e: e.matmul(PB[1][:, 32 * t:32 * t + 32], lhsT=hT[:, k, i * 128:(i + 1) * 128], rhs=wkv[:, k, 128:160],
                                                            start=(k == 0), stop=(k == 7)),
                                   R=[dwkv] + HD[i:i + 1], W=[PD[1]], inc=(k == 7))
                        krv = PB[1][:, 0:128].rearrange("p (t d) -> p t d", t=4)
                        cos4, sin4 = cs[:, 4 * c:4 * c + 4, 0:16], cs[:, 4 * c:4 * c + 4, 16:32]
                        op("dve", lambda e: e.tensor_tensor(out=rk[0][:, :, :], in0=krv[:, :, 0:16], in1=cos4, op=ALU.mult), R=[PD[1], dCS], W=[drt])
                        op("dve", lambda e: e.tensor_tensor(out=rk[1][:, :, :], in0=krv[:, :, 16:32], in1=sin4, op=ALU.mult), R=[PD[1], dCS], W=[drt])
                        op("dve", lambda e: e.tensor_tensor(out=rk[2][:, :, :], in0=krv[:, :, 0:16], in1=sin4, op=ALU.mult), R=[PD[1], dCS], W=[drt])
                        op("dve", lambda e: e.tensor_tensor(out=rk[3][:, :, :], in0=krv[:, :, 16:32], in1=cos4, op=ALU.mult), R=[PD[1], dCS], W=[drt])
                        op("dve", lambda e: e.tensor_tensor(out=krp4[:, :, 64:80], in0=rk[0][:, :, :], in1=rk[1][:, :, :], op=ALU.subtract), R=[drt], W=[dkrp])
                        op("dve", lambda e: e.tensor_tensor(out=krp4[:, :, 80:96], in0=rk[2][:, :, :], in1=rk[3][:, :, :], op=ALU.add), R=[drt], W=[dkrp])
                        pv = PB[7][:, :].bitcast(BF16)
                        for t in range(4):
                            op("pe", lambda e: e.transpose(out=pv[0:96, t * 128:(t + 1) * 128], in_=krp4[:, t, :], identity=identb[:]),
                               R=[dkrp, dCONST], W=[PD[7]], inc=(t == 3))
                        pv = PB[7][:, :].bitcast(BF16)
                        for hl in range(3):
                            evac("act" if hl % 2 == 0 else "dve", KT[64:96, hl, c * 512:(c + 1) * 512], pv[64:96, 0:512], [PD[7]], [dKT[hl][c]])
                    def qprep(c):
                        for r_ in range(2):
                            proj_fm(r_, wq, dwq, 128 * r_, 128, c)
                            evac("dve", cqT[:, r_, :], PB[r_][:, :], [PD[r_]], [dcqT])
                            op("act", lambda e: e.activation(out=fa[r_][:, :], in_=PB[r_][:, :], func=AF.Square), R=[PD[r_]], W=[dfa[r_]])
                        for t in range(4):
                            for r_ in range(2):
                                op("pe", lambda e: e.matmul(PB[1][:, t:t + 1], lhsT=fa[r_][:, t * 128:(t + 1) * 128], rhs=onesf[:, 0:1],
                                                            start=(r_ == 0), stop=(r_ == 1)), R=[dCONST, dfa[r_]], W=[PD[1]], inc=(t == 3 and r_ == 1))
                        op("act", lambda e: e.activation(out=rtok[:, :], in_=PB[1][:, 0:4], func=AF.Ln, bias=epsc[:, 0:1], scale=1.0 / 256),
                           R=[PD[1], dCONST], W=[drtok])
                        op("act", lambda e: e.activation(out=rtok[:, :], in_=rtok[:, :], func=AF.Exp, scale=-0.5), R=[drtok], W=[drtok])
                        pq = [PB[0][:, :].bitcast(BF16), PB[1][:, :].bitcast(BF16), PB[7][:, :].bitcast(BF16)]
                        pqd = [PD[0], PD[1], PD[7]]
                        for t in range(4):
                            sb_ = nxt("s", 3, 2)
                            for r_ in range(2):
                                op("pe", lambda e: e.matmul(PB[sb_][:, 0:288], lhsT=cqT[:, r_, t * 128:(t + 1) * 128], rhs=wuq_b[:, r_, h0 * 96:(h0 + 3) * 96],
                                                            start=(r_ == 0), stop=(r_ == 1)), R=[dcqT, dwuq], W=[PD[sb_]], inc=(r_ == 1))
                            op("act", lambda e: e.activation(out=qsb4[:, t, :, :], in_=PB[sb_][:, 0:288].rearrange("p (h d) -> p h d", h=3), func=AF.Identity,
                                                             scale=rtok[:, t:t + 1]), R=[PD[sb_], drtok], W=[dqsb])

                        def b43(a_):
                            l_ = [list(v) for v in a_.ap]
                            return bass.AP(tensor=a_.tensor, offset=a_.offset, ap=[l_[0], l_[1], [0, 3], l_[2]])
                        cos43, sin43 = b43(cs[:, 4 * c:4 * c + 4, 0:16]), b43(cs[:, 4 * c:4 * c + 4, 16:32])
                        op("pool", lambda e: e.tensor_copy(out=qrot4[:, :, :, 0:64], in_=qsb4[:, :, :, 0:64]), R=[dqsb], W=[dqrot])
                        op("dve", lambda e: e.tensor_tensor(out=rq[0][:], in0=qsb4[:, :, :, 64:80], in1=cos43, op=ALU.mult), R=[dqsb, dCS], W=[drt])
                        op("dve", lambda e: e.tensor_tensor(out=rq[1][:], in0=qsb4[:, :, :, 80:96], in1=sin43, op=ALU.mult), R=[dqsb, dCS], W=[drt])
                        op("dve", lambda e: e.tensor_tensor(out=rq[2][:], in0=qsb4[:, :, :, 64:80], in1=sin43, op=ALU.mult), R=[dqsb, dCS], W=[drt])
                        op("dve", lambda e: e.tensor_tensor(out=rq[3][:], in0=qsb4[:, :, :, 80:96], in1=cos43, op=ALU.mult), R=[dqsb, dCS], W=[drt])
                        op("dve", lambda e: e.tensor_tensor(out=qrot4[:, :, :, 64:80], in0=rq[0][:], in1=rq[1][:], op=ALU.subtract), R=[drt], W=[dqrot])
                        op("dve", lambda e: e.tensor_tensor(out=qrot4[:, :, :, 80:96], in0=rq[2][:], in1=rq[3][:], op=ALU.add), R=[drt], W=[dqrot])
                        for t in range(4):
                            for hl in range(3):
                                op("pe", lambda e: e.transpose(out=pq[hl][0:96, t * 128:(t + 1) * 128], in_=qrot4[:, t, hl, :], identity=identb[:]),
                                   R=[dqrot, dCONST], W=[pqd[hl]])
                        for hl in range(3):
                            evac("act" if hl % 2 == 0 else "dve", QT[0:96, hl, c % 2, :], pq[hl][0:96, 0:512], [pqd[hl]], [dQT[hl][c % 2]])
                        pop_pending()
                    qprep(0)
                    for c in range(4):
                        maps = []
                        for hl in range(3):
                            def act_fn(j, p, sb_, lo):
                                op("act", lambda e: e.activation(out=PT[p][:, lo:512], in_=PB[sb_][:, lo:512], func=AF.Exp, scale=96 ** -0.5),
                                   R=[PD[sb_]], W=[dPT[p]])

                            def dest(acc_pb, f2, hl=hl):
                                op("dve", lambda e: e.tensor_tensor(out=OT[0:64, hl, :], in0=PB[acc_pb][0:64, :], in1=fa[f2][0:64, :],
                                                                    op=ALU.mult), R=[PD[acc_pb], dfa[f2]], W=[dOT[hl]])
                            maps.append(dict(kfn=lambda j, hl=hl: KT[0:96, hl, j * 128:(j + 1) * 128], kdeps=dKT[hl],
                                             qfn=lambda lo, hl=hl, c=c: QT[0:96, hl, c % 2, lo:512], qdep=dQT[hl][c % 2],
                                             hl=hl, act_fn=act_fn, fix=causal_fix, done=lambda acc, last, dest=dest: normalize_to(acc, dest, last)))
                        attn_chunk(c, maps)
                        if c < 3:
                            qprep(c + 1)
                        else:
                            pop_pending(all_=True)
                        wout_phase(c, 3)
                S.barrier()

        def ffn(l):
            moe = (l % 2 == 1)
            with ExitStack() as s1:
                if moe:
                    rws = sbt(s1, "rws", (128, 8, NEXP), F32); drws = Dep()
                    logits = sbt(s1, "logits", (128, NT, NEXP), F32); dlog = Dep()
                    comb = sbt(s1, "comb", (128, NT, NEXP), F32); dcomb = Dep()
                    t8 = sbt(s1, "t8", (128, 8), F32); dt8 = Dep()
                    sel = sbt(s1, "sel", (128, 8), F32); ex = sbt(s1, "ex", (128, 8), F32); den = sbt(s1, "den", (128, 2), F32)
                    dma("sp", rws[:], router_w[0].rearrange("(k p) e -> p k e", p=128), W=[drws])
                    norm_phase(s1, 1, fp32_router=(rws, drws, logits, dlog))
                    for i in range(NT):
                        op("dve", lambda e: e.max(out=t8[:], in_=logits[:, i, :]), R=[dlog], W=[dt8])
                        op("dve", lambda e: e.tensor_scalar(out=sel[:], in0=logits[:, i, :], scalar1=t8[:, 1:2], scalar2=None, op0=ALU.is_ge),
                           R=[dlog, dt8], W=[dt8])
                        op("dve", lambda e: e.tensor_scalar(out=den[:, 0:1], in0=t8[:, 0:1], scalar1=-1.0, scalar2=None, op0=ALU.mult), R=[dt8], W=[dt8])
                        op("act", lambda e: e.activation(out=ex[:], in_=logits[:, i, :], func=AF.Exp, bias=den[:, 0:1], scale=1.0), R=[dlog, dt8], W=[dt8])
                        op("dve", lambda e: e.tensor_tensor(out=ex[:], in0=ex[:], in1=sel[:], op=ALU.mult), R=[dt8], W=[dt8])
                        op("dve", lambda e: e.reduce_sum(out=den[:, 1:2], in_=ex[:], axis=AX.X), R=[dt8], W=[dt8])
                        op("dve", lambda e: e.reciprocal(out=den[:, 1:2], in_=den[:, 1:2]), R=[dt8], W=[dt8])
                        op("dve", lambda e: e.tensor_scalar(out=comb[:, i, :], in0=ex[:], scalar1=den[:, 1:2], scalar2=None, op0=ALU.mult),
                           R=[dt8], W=[dcomb])
                else:
                    norm_phase(s1, 1)
                G = 4 if moe else 2
                GW = 128 * G
                wg = [sbt(s1, "wg%d" % i, (128, 8, GW), BF16) for i in range(2)]
                wu = [sbt(s1, "wu%d" % i, (128, 8, GW), BF16) for i in range(2)]
                wdf = [sbt(s1, "wdf%d" % i, (128, G, D), F32) for i in range(2)]
                wd = [sbt(s1, "wd%d" % i, (128, G, D), BF16) for i in range(2)]
                dwg = [Dep(), Dep()]; dwu = [Dep(), Dep()]; dwdf = [Dep(), Dep()]; dwd = [Dep(), Dep()]
                sg_ = [sbt(s1, "sg%d" % i, (128, 256), F32) for i in range(2)]; dsg = [Dep(), Dep()]
                aT = [sbt(s1, "aT%d" % i, (128, G, 256), BF16) for i in range(2)]; daT = [Dep(), Dep()]
                if moe:
                    groups = [(e_, g_) for e_ in range(NEXP) for g_ in range(DFE // GW)]
                else:
                    groups = [(None, g_) for g_ in range(DFF // GW)]
                cnt = 0
                prev_down = [None]
                for gi, (e_, g_) in enumerate(groups):
                    b = gi % 2
                    if moe:
                        gsrc = moe_w_gate[0, e_].rearrange("(k p) n -> p k n", p=128)[:, :, g_ * GW:(g_ + 1) * GW]
                        usrc = moe_w_up[0, e_].rearrange("(k p) n -> p k n", p=128)[:, :, g_ * GW:(g_ + 1) * GW]
                        dsrc = moe_w_down[0, e_, g_ * GW:(g_ + 1) * GW, :].rearrange("(j p) n -> p j n", p=128)
                    else:
                        gsrc = ffn_w_gate[0].rearrange("(k p) n -> p k n", p=128)[:, :, g_ * GW:(g_ + 1) * GW]
                        usrc = ffn_w_up[0].rearrange("(k p) n -> p k n", p=128)[:, :, g_ * GW:(g_ + 1) * GW]
                        dsrc = ffn_w_down[0, g_ * GW:(g_ + 1) * GW, :].rearrange("(j p) n -> p j n", p=128)
                    dma("pool", wg[b][:], gsrc, W=[dwg[b]])
                    dma("pool", wu[b][:], usrc, W=[dwu[b]])
                    dma("sp", wdf[b][:], dsrc, W=[dwdf[b]])
                    for j in range(G):
                        op("pool", lambda e: e.tensor_tensor(out=wd[b][:, j, :], in0=wdf[b][:, j, :], in1=gfb[:, :], op=ALU.mult),
                           R=[dwdf[b], dGFB], W=[dwd[b]])
                    for T in range(8):
                        ab = cnt % 2
                        cnt += 1
                        for j in range(G):
                            pg, pu = 2 * (j % 2), 2 * (j % 2) + 1
                            for k in range(8):
                                op("pe", lambda e: e.matmul(PB[pg][:, 0:256], lhsT=wg[b][:, k, j * 128:(j + 1) * 128], rhs=hT[:, k, T * 256:(T + 1) * 256],
                                                            start=(k == 0), stop=(k == 7)), R=[dwg[b]] + HD[2 * T:2 * T + 2], W=[PD[pg]], inc=(k == 7))
                            for k in range(8):
                                op("pe", lambda e: e.matmul(PB[pu][:, 0:256], lhsT=wu[b][:, k, j * 128:(j + 1) * 128], rhs=hT[:, k, T * 256:(T + 1) * 256],
                                                            start=(k == 0), stop=(k == 7)), R=[dwu[b]] + HD[2 * T:2 * T + 2], W=[PD[pu]], inc=(k == 7))
                            op("act", lambda e: e.activation(out=sg_[j % 2][:, :], in_=PB[pg][:, 0:256], func=AF.Silu), R=[PD[pg]], W=[dsg[j % 2]])
                            op("dve", lambda e: e.tensor_tensor(out=aT[ab][:, j, :], in0=PB[pu][:, 0:256], in1=sg_[j % 2][:, :], op=ALU.mult),
                               R=[PD[pu], dsg[j % 2]], W=[daT[ab]])
                            if j == 0 and prev_down[0] is not None:
                                prev_down[0]()
                                prev_down[0] = None

                        def down(T=T, ab=ab, b=b, e_=e_):
                            for t in range(2):
                                for half in range(2):
                                    pa = 4 + 2 * t + half
                                    i = 2 * T + t
                                    for j in range(G):
                                        op("pe", lambda e: e.matmul(PB[pa][:, :], lhsT=aT[ab][:, j, t * 128:(t + 1) * 128], rhs=wd[b][:, j, half * 512:(half + 1) * 512],
                                                                    start=(j == 0), stop=(j == G - 1)), R=[daT[ab], dwd[b]], W=[PD[pa]], inc=(j == G - 1))
                                    xsl = xs[:, i, half * 512:(half + 1) * 512]
                                    if moe:
                                        op("dve", lambda e: e.scalar_tensor_tensor(out=xsl, in0=PB[pa][:, :], scalar=comb[:, i, e_:e_ + 1], in1=xsl,
                                                                                   op0=ALU.mult, op1=ALU.add), R=[PD[pa], dcomb], W=[XD[i]])
                                    else:
                                        op("dve", lambda e: e.tensor_tensor(out=xsl, in0=PB[pa][:, :], in1=xsl, op=ALU.add), R=[PD[pa]], W=[XD[i]])
                        prev_down[0] = down
                if prev_down[0] is not None:
                    prev_down[0]()
                    prev_down[0] = None
                S.barrier()

        def final():
            with ExitStack() as s1:
                fnb = sbt(s1, "fnb", (128, D), F32); dfnb = Dep()
                junk = sbt(s1, "junkf", (128, D), BF16); djunk = Dep()
                dY = Dep()
                if stage == "full":
                    dma("sp", fnb[:], final_norm.partition_broadcast(128), W=[dfnb])
                    op("dve", lambda e: e.memset(ssq[:], 0.0), W=[dSSQ])
                    for i in range(NT):
                        op("act", lambda e: e.activation(out=junk[:], in_=xs[:, i, :], func=AF.Square, accum_out=ssq[:, i:i + 1]),
                           R=[XD[i]], W=[djunk, dSSQ])
                    op("act", lambda e: e.activation(out=rstd[:], in_=ssq[:], func=AF.Ln, bias=epsc[:, 0:1], scale=1.0 / D),
                       R=[dSSQ, dCONST], W=[dRSTD])
                    op("act", lambda e: e.activation(out=rstd[:], in_=rstd[:], func=AF.Exp, scale=-0.5), R=[dRSTD], W=[dRSTD])
                    for i in range(NT):
                        op("dve", lambda e: e.scalar_tensor_tensor(out=xs[:, i, :], in0=xs[:, i, :], scalar=rstd[:, i:i + 1], in1=fnb[:, :],
                                                                   op0=ALU.mult, op1=ALU.mult), R=[XD[i], dRSTD, dfnb], W=[XD[i]])
                yv = y_d.rearrange("(i p) f -> p i f", p=128)
                for q in range(4):
                    dma("sp", yv[:, 4 * q:4 * q + 4, :], xs[:, 4 * q:4 * q + 4, :], R=XD[4 * q:4 * q + 4], W=[dY])
                S.barrier()

        stop = False
        for l in range(DEPTH):
            if stage == "setup":
                break
            adaln(l)
            if stage == "adaln":
                break
            with ExitStack() as sn:
                norm_phase(sn, 0)
            if stage == "norm":
                break
            attention(l)
            if stage in ("diff", "fox", "attn0"):
                break
            ffn(l)
            if stage == "layer0":
                break
        final()
        print("instructions:", S.ninst)
    return nc


_CACHE = {}


def kernel(**inputs):
    stage = inputs.pop("_stage", "full")
    ncores = inputs.pop("_ncores", 8)
    if stage not in _CACHE:
        _CACHE[stage] = build(stage)
    nc = _CACHE[stage]
    consts = host_consts()
    shared = {k: np.ascontiguousarray(np.asarray(v, dtype=np.float32)) for k, v in inputs.items() if k not in ("x", "c")}
    shared.update(consts)
    x = np.asarray(inputs["x"], dtype=np.float32)
    c = np.asarray(inputs["c"], dtype=np.float32)
    in_maps = []
    for b in range(ncores):
        m = dict(shared)
        m["x"] = np.ascontiguousarray(x[b])
        m["c"] = np.ascontiguousarray(c[b])
        in_maps.append(m)
    res = run_bass_kernel_spmd(nc, in_maps, core_ids=list(range(ncores)))
    out = np.stack([np.asarray(r["y"], dtype=np.float32) for r in res.results], axis=0)
    return out
```

```python
import math
import types
import numpy as np
from contextlib import ExitStack
import concourse.bass as bass
import concourse.mybir as mybir
from concourse.bass_utils import run_bass_kernel_spmd

F32 = mybir.dt.float32
BF16 = mybir.dt.bfloat16
ALU = mybir.AluOpType
AF = mybir.ActivationFunctionType
AX = mybir.AxisListType

D = 1024
S_LEN = 2048
NT = 16
DEPTH = 2
IN_W = 2342
EPS = 1e-6
DFF = 2816
NEXP = 8
DFE = 3584
O_DQ, O_DK, O_DV, O_FQ, O_FK, O_FV, O_FF, O_CQ, O_CKV, O_KR = 0, 256, 512, 768, 1152, 1536, 1920, 1926, 2182, 2310


class SemRef:
    def __init__(self, sem, name):
        self.sem = sem
        self.name = name
        self.count = 0


class Eng:
    def __init__(self, name, obj, semref):
        self.name = name
        self.obj = obj
        self.sr = semref
        self.seen = {}


class Dep:
    __slots__ = ("w", "r", "excl")

    def __init__(self, excl=False):
        self.w = None
        self.r = {}
        self.excl = excl


class Sched:
    def __init__(self, nc, es, n_dma_sems=32):
        self.nc = nc
        self.E = {}
        for name, obj in [("pe", nc.tensor), ("act", nc.scalar), ("dve", nc.vector),
                          ("pool", nc.gpsimd), ("sp", nc.sync)]:
            sem = es.enter_context(nc.semaphore("s_" + name))
            self.E[name] = Eng(name, obj, SemRef(sem, name))
        self.dma_sems = {q: [SemRef(es.enter_context(nc.semaphore("d%s%d" % (q, i))), "d%s%d" % (q, i))
                             for i in range(n_dma_sems if q != "act" else 8)] for q in ("sp", "pool", "act")}
        self.dma_rr = {"sp": 0, "pool": 0, "act": 0}
        self.ninst = 0

    defer = None

    def _emit(self, eng, thunk):
        if self.defer is not None:
            self.defer[eng.name].append(thunk)
        else:
            thunk()

    @staticmethod
    def _snap(fn):
        if fn.__closure__ is None:
            return fn
        cells = tuple(types.CellType(c.cell_contents) for c in fn.__closure__)
        return types.FunctionType(fn.__code__, fn.__globals__, fn.__name__, fn.__defaults__, cells)

    def _wait(self, eng, needs):
        for sr, c in needs.items():
            if c <= 0:
                continue
            if sr is eng.sr and eng.name == "pe":
                continue
            if eng.seen.get(sr, 0) >= c:
                continue
            self._emit(eng, lambda o=eng.obj, s_=sr.sem, c_=c: o.wait_ge(s_, c_))
            eng.seen[sr] = c

    @staticmethod
    def _needs(R, W):
        needs = {}
        for d in R:
            if d.w is not None:
                sr, c = d.w
                needs[sr] = max(needs.get(sr, 0), c)
        for d in W:
            if d.w is not None:
                sr, c = d.w
                needs[sr] = max(needs.get(sr, 0), c)
            for sr, c in d.r.items():
                needs[sr] = max(needs.get(sr, 0), c)
        return needs

    def op(self, ename, fn, R=(), W=(), inc=True):
        if any(d.excl for d in R):
            W = list(W) + [d for d in R if d.excl]
            R = [d for d in R if not d.excl]
        eng = self.E[ename]
        self._wait(eng, self._needs(R, W))
        self.ninst += 1
        sr = eng.sr
        if self.defer is not None:
            fn = self._snap(fn)

        def thunk(fn=fn, o=eng.obj, s_=sr.sem, inc=inc):
            ins = fn(o)
            if inc:
                ins.then_inc(s_, 1)
        self._emit(eng, thunk)
        if inc:
            sr.count += 1
            tok = sr.count
        else:
            tok = sr.count + 1
        for d in R:
            d.r[sr] = max(d.r.get(sr, 0), tok)
        for d in W:
            d.w = (sr, tok)
            d.r = {}

    def dma(self, ename, out, in_, R=(), W=(), **kw):
        eng = self.E[ename]
        pool_ = self.dma_sems[ename]
        ds = pool_[self.dma_rr[ename]]
        self.dma_rr[ename] = (self.dma_rr[ename] + 1) % len(pool_)
        needs = self._needs(R, W)
        if ds.count > 0:
            needs[ds] = max(needs.get(ds, 0), ds.count)
        self._wait(eng, needs)

        def thunk(o=eng.obj, out=out, in_=in_, kw=kw, s_=ds.sem):
            o.dma_start(out=out, in_=in_, **kw).then_inc(s_, 16)
        self._emit(eng, thunk)
        self.ninst += 1
        ds.count += 16
        tok = ds.count
        for d in R:
            d.r[ds] = tok
        for d in W:
            d.w = (ds, tok)
            d.r = {}

    def barrier(self):
        srs = [e.sr for e in self.E.values()] + self.dma_sems["sp"] + self.dma_sems["pool"] + self.dma_sems["act"]
        for eng in self.E.values():
            needs = {sr: sr.count for sr in srs if sr is not eng.sr}
            if eng.name != "pe":
                needs[eng.sr] = eng.sr.count
            self._wait(eng, needs)


def bmid(a, n):
    l = [list(v) for v in a.ap]
    return bass.AP(tensor=a.tensor, offset=a.offset, ap=[l[0], [0, n]] + l[1:])


def host_consts():
    half = 16
    freqs = (10000.0 ** (-np.arange(half, dtype=np.float32) / half)).astype(np.float32)
    t = np.arange(S_LEN, dtype=np.float32)
    ang = (t[:, None] * freqs[None, :]).astype(np.float32)
    cs = np.concatenate([np.cos(ang), np.sin(ang)], axis=1).astype(np.float32)
    cs = np.ascontiguousarray(cs.reshape(NT, 128, 32).transpose(1, 0, 2))
    n = np.arange(-127, 256)
    nn = np.maximum(n, 0)
    nf = np.maximum(nn, 1).astype(np.float32)
    large = 16 + (np.log(nf / np.float32(16)) / np.float32(math.log(128 / 16)) * np.float32(16)).astype(np.int32)
    large = np.minimum(large, 31)
    bucket = np.where(nn < 16, nn, large)
    oh = np.zeros((32, 383), np.float32)
    oh[bucket, np.arange(383)] = 1.0
    step = np.broadcast_to((n >= 0).astype(np.float32)[None, :], (4, 383)).copy()
    neg = ((step - 1.0) * 30000.0).astype(np.float32)
    return {"cs_tab": cs, "t5_oh": oh, "t5_step": step, "t5_neg": neg}


def build(stage="full"):
    nc = bass.Bass("TRN2", target_bir_lowering=False)
    dt_in = lambda name, shape: nc.dram_tensor(name, list(shape), F32, kind="ExternalInput").ap()
    x_d = dt_in("x", (S_LEN, D))
    c_d = dt_in("c", (D,))
    w_ada = dt_in("w_ada", (DEPTH, D, 6 * D))
    b_ada = dt_in("b_ada", (DEPTH, 6 * D))
    attn_norm = dt_in("attn_norm", (DEPTH, D))
    ffn_norm = dt_in("ffn_norm", (DEPTH, D))
    w_in = dt_in("w_in", (DEPTH, D, IN_W))
    b_forget = dt_in("b_forget", (DEPTH, 6))
    diff_lambda = dt_in("diff_lambda", (DEPTH, 4, 32))
    diff_subln = dt_in("diff_subln", (DEPTH, 64))
    rel_bias = dt_in("rel_bias", (32, 4))
    mla_q_norm = dt_in("mla_q_norm", (DEPTH, 256))
    mla_kv_norm = dt_in("mla_kv_norm", (DEPTH, 128))
    w_uq = dt_in("w_uq", (DEPTH, 256, 576))
    w_ukv = dt_in("w_ukv", (DEPTH, 128, 768))
    w_out = dt_in("w_out", (DEPTH, D, D))
    ffn_w_gate = dt_in("ffn_w_gate", (1, D, DFF))
    ffn_w_up = dt_in("ffn_w_up", (1, D, DFF))
    ffn_w_down = dt_in("ffn_w_down", (1, DFF, D))
    router_w = dt_in("router_w", (1, D, NEXP))
    moe_w_gate = dt_in("moe_w_gate", (1, NEXP, D, DFE))
    moe_w_up = dt_in("moe_w_up", (1, NEXP, D, DFE))
    moe_w_down = dt_in("moe_w_down", (1, NEXP, DFE, D))
    final_norm = dt_in("final_norm", (D,))
    cs_d = dt_in("cs_tab", (128, NT, 32))
    oh_d = dt_in("t5_oh", (32, 383))
    step_d = dt_in("t5_step", (4, 383))
    neg_d = dt_in("t5_neg", (4, 383))
    y_d = nc.dram_tensor("y", [S_LEN, D], F32, kind="ExternalOutput").ap()
    mod_scr = nc.dram_tensor("mod_scr", [DEPTH, 6 * D], F32, kind="Internal")
    g_scr = nc.dram_tensor("g_scr", [4, 383], F32, kind="Internal")

    es = ExitStack()
    with es:
        S = Sched(nc, es)
        op, dma = S.op, S.dma

        uniq = [0]

        def sbt(stack, name, shape, dt):
            uniq[0] += 1
            return stack.enter_context(nc.sbuf_tensor("%s_%d" % (name, uniq[0]), list(shape), dt))

        PB = [es.enter_context(nc.psum_tensor("pb%d" % i, [128, 512], F32)) for i in range(8)]
        PD = [Dep(excl=True) for _ in range(8)]

        xs = sbt(es, "xs", (128, NT, D), F32)
        XD = [Dep() for _ in range(NT)]
        hT = sbt(es, "hT", (128, 8, S_LEN), BF16)
        HDa = [Dep() for _ in range(NT)]; HDb = [Dep() for _ in range(NT)]
        class _HD:
            def __getitem__(self, k):
                if isinstance(k, slice):
                    return HDa[k] + HDb[k]
                return HDa[k]
        HD = _HD()
        identb = sbt(es, "identb", (128, 128), BF16)
        identf = sbt(es, "identf", (128, 128), F32)
        onesf = sbt(es, "onesf", (128, 128), F32)
        cs = sbt(es, "cs", (128, NT, 32), F32)
        mt5 = sbt(es, "mt5", (128, 4, 2, 128), BF16)
        negmask = sbt(es, "negmask", (128, 128), BF16)
        b31 = sbt(es, "b31", (128, 4), F32)
        cact = sbt(es, "cact", (128, 8), F32)
        cols = sbt(es, "cols", (128, 64), F32)
        amod = sbt(es, "amod", (128, 4, 8), F32)
        gab = sbt(es, "gab", (128, D), F32)
        gfb = sbt(es, "gfb", (128, D), F32)
        rstd = sbt(es, "rstd", (128, NT), F32)
        ssq = sbt(es, "ssq", (128, NT), F32)
        epsc = sbt(es, "epsc", (128, 1), F32)
        neglam = sbt(es, "neglam", (128, 1), F32)
        gsub = sbt(es, "gsub", (64, 1), F32)
        dCONST = Dep(); dCS = Dep(); dMT5 = Dep(); dB31 = Dep(); dCACT = Dep(); dCOLS = Dep()
        dAMOD = Dep(); dGAB = Dep(); dGFB = Dep(); dRSTD = Dep(); dSSQ = Dep(); dLAM = Dep(); dGSUB = Dep()
        dMODSCR = Dep(); dGSCR = Dep()

        op("pool", lambda e: e.memset(onesf[:], 1.0), W=[dCONST])
        op("pool", lambda e: e.memset(epsc[:], EPS), W=[dCONST])
        op("pool", lambda e: e.affine_select(out=identf[:], in_=onesf[:], pattern=[[-1, 128]], compare_op=ALU.is_equal,
                                             fill=0.0, base=0, channel_multiplier=1), R=[dCONST], W=[dCONST])
        op("pool", lambda e: e.affine_select(out=identb[:], in_=onesf[:], pattern=[[-1, 128]], compare_op=ALU.is_equal,
                                             fill=0.0, base=0, channel_multiplier=1), R=[dCONST], W=[dCONST])
        zf = sbt(es, "zf", (128, 128), F32)
        op("pool", lambda e: e.memset(zf[:], 0.0), W=[dCONST])
        op("pool", lambda e: e.affine_select(out=negmask[:], in_=zf[:], pattern=[[1, 128]], compare_op=ALU.is_ge,
                                             fill=-30000.0, base=0, channel_multiplier=-1), R=[dCONST], W=[dCONST])
        dma("sp", cs[:], cs_d, W=[dCS])
        dma("sp", b31[:], rel_bias[31, :].partition_broadcast(128), W=[dB31])
        xv = x_d.rearrange("(i p) f -> p i f", p=128)
        for q in range(4):
            dma("sp", xs[:, 4 * q:4 * q + 4, :], xv[:, 4 * q:4 * q + 4, :], W=XD[4 * q:4 * q + 4])
        with ExitStack() as s0:
            crow = sbt(s0, "crow", (8, 128), F32); dcrow = Dep()
            jmat = sbt(s0, "jmat", (128, 128), F32)
            rbs = sbt(s0, "rbs", (32, 4), F32)
            ohs = sbt(s0, "ohs", (32, 383), F32)
            stp = sbt(s0, "stp", (4, 383), F32)
            nb31c = sbt(s0, "nb31c", (4, 1), F32)
            gsb = sbt(s0, "gsb", (4, 383), F32)
            hank = sbt(s0, "hank", (128, 2, 128), F32)
            dT5 = Dep(); dHK = [Dep(), Dep()]
            dma("sp", crow[:], c_d.rearrange("(k p) -> k p", p=128), W=[dcrow])
            op("pe", lambda e: e.transpose(out=PB[0][:, 0:8], in_=crow[:], identity=identf[0:8, 0:8]), R=[dcrow, dCONST], W=[PD[0]])
            op("act", lambda e: e.activation(out=cact[:], in_=PB[0][:, 0:8], func=AF.Silu), R=[PD[0]], W=[dCACT])
            op("pool", lambda e: e.affine_select(out=jmat[:], in_=onesf[:], pattern=[[1, 128]], compare_op=ALU.is_equal,
                                                 fill=0.0, base=-127, channel_multiplier=1), R=[dCONST], W=[dT5])
            dma("sp", rbs[:], rel_bias, W=[dT5])
            dma("sp", ohs[:], oh_d, W=[dT5])
            dma("sp", stp[:], step_d, W=[dT5])
            dma("sp", nb31c[:], rel_bias[31:32, :].rearrange("a h -> h a"), W=[dT5], allow_slow_non_contiguous=True)
            S32 = 32 ** 0.5
            ngt = sbt(s0, "ngt", (4, 383), F32)
            dma("sp", ngt[:], neg_d, W=[dT5])
            op("dve", lambda e: e.tensor_scalar(out=nb31c[:], in0=nb31c[:], scalar1=-S32, scalar2=None, op0=ALU.mult), R=[dT5], W=[dT5])
            op("pe", lambda e: e.matmul(PB[1][0:4, 0:383], lhsT=rbs[:, :], rhs=ohs[:, :], start=True, stop=True), R=[dT5], W=[PD[1]])
            op("act", lambda e: e.activation(out=gsb[:], in_=PB[1][0:4, 0:383], func=AF.Identity, bias=nb31c[:, 0:1], scale=S32), R=[PD[1], dT5], W=[dT5])
            op("dve", lambda e: e.tensor_tensor(out=gsb[:], in0=gsb[:], in1=stp[:], op=ALU.mult), R=[dT5], W=[dT5])
            op("dve", lambda e: e.tensor_tensor(out=gsb[:], in0=gsb[:], in1=ngt[:], op=ALU.add), R=[dT5], W=[dT5])
            dma("sp", g_scr.ap(), gsb[:], R=[dT5], W=[dGSCR])
            for h in range(4):
                for dl in range(2):
                    hk = dHK[dl]
                    dma("sp", hank[:, dl, :], bass.AP(tensor=g_scr, offset=h * 383 + 128 * dl, ap=[[1, 128], [1, 128]]),
                        R=[dGSCR], W=[hk])
                    pb = 2 + dl
                    op("pe", lambda e: e.matmul(PB[pb][:, 0:128], lhsT=jmat[:, :], rhs=hank[:, dl, :], start=True, stop=True),
                       R=[dT5, hk], W=[PD[pb]])
                    op("act", lambda e: e.activation(out=mt5[:, h, dl, :], in_=PB[pb][:, 0:128], func=AF.Identity), R=[PD[pb]], W=[dMT5])
            S.barrier()

        def adaln(l):
            with ExitStack() as s1:
                wb = [sbt(s1, "adaw%d" % i, (128, 3072), F32) for i in range(5)]
                dwb = [Dep() for _ in range(5)]
                brow = sbt(s1, "brow", (1, 6 * D), F32); dbrow = Dep()
                mrow = [sbt(s1, "mrow%d" % i, (1, 512), F32) for i in range(2)]
                dmrow = [Dep(), Dep()]
                rows = sbt(s1, "rows", (64, 128), F32); drows = Dep()
                dma("sp", brow[:], b_ada[l:l + 1, :], W=[dbrow])
                nb = 0
                for half in range(2):
                    for k in range(8):
                        b = nb % 5
                        nb += 1
                        dma(("sp", "pool", "act")[nb % 3], wb[b][:], w_ada[l, k * 128:(k + 1) * 128, half * 3072:(half + 1) * 3072], W=[dwb[b]])
                        for cb in range(6):
                            op("pe", lambda e: e.matmul(PB[cb][0:1, :], lhsT=cact[:, k:k + 1], rhs=wb[b][:, cb * 512:(cb + 1) * 512],
                                                        start=(k == 0), stop=(k == 7)),
                               R=[dCACT, dwb[b]], W=[PD[cb]], inc=(k == 7 or cb == 5))
                    for cb in range(6):
                        cc = half * 6 + cb
                        m_ = cb % 2
                        op("dve", lambda e: e.tensor_tensor(out=mrow[m_][:], in0=PB[cb][0:1, :], in1=brow[0:1, cc * 512:(cc + 1) * 512],
                                                            op=ALU.add), R=[PD[cb], dbrow], W=[dmrow[m_]])
                        dma("sp", mod_scr.ap()[l:l + 1, cc * 512:(cc + 1) * 512], mrow[m_][:], R=[dmrow[m_]], W=[dMODSCR])
                dma("sp", rows[0:48, :], mod_scr.ap()[l, :].rearrange("(j p) -> j p", p=128), R=[dMODSCR], W=[drows])
                dma("sp", rows[48:56, :], attn_norm[l, :].rearrange("(j p) -> j p", p=128), W=[drows])
                dma("sp", rows[56:64, :], ffn_norm[l, :].rearrange("(j p) -> j p", p=128), W=[drows])
                op("pe", lambda e: e.transpose(out=PB[2][:, 0:64], in_=rows[:, :], identity=identf[0:64, 0:64]),
                   R=[drows, dCONST], W=[PD[2]])
                op("act", lambda e: e.activation(out=cols[:], in_=PB[2][:, 0:64], func=AF.Identity), R=[PD[2]], W=[dCOLS])
                op("dve", lambda e: e.scalar_tensor_tensor(out=amod[:, 0, :], in0=cols[:, 8:16], scalar=1.0, in1=cols[:, 48:56],
                                                           op0=ALU.add, op1=ALU.mult), R=[dCOLS], W=[dAMOD])
                op("dve", lambda e: e.tensor_copy(out=amod[:, 1, :], in_=cols[:, 0:8]), R=[dCOLS], W=[dAMOD])
                op("dve", lambda e: e.scalar_tensor_tensor(out=amod[:, 2, :], in0=cols[:, 32:40], scalar=1.0, in1=cols[:, 56:64],
                                                           op0=ALU.add, op1=ALU.mult), R=[dCOLS], W=[dAMOD])
                op("dve", lambda e: e.tensor_copy(out=amod[:, 3, :], in_=cols[:, 24:32]), R=[dCOLS], W=[dAMOD])
                dma("sp", gab[:], mod_scr.ap()[l, 2 * D:3 * D].partition_broadcast(128), R=[dMODSCR], W=[dGAB])
                dma("sp", gfb[:], mod_scr.ap()[l, 5 * D:6 * D].partition_broadcast(128), R=[dMODSCR], W=[dGFB])
                S.barrier()

        def norm_phase(stack, which, fp32_router=None):
            ia, ish = 2 * which, 2 * which + 1
            with ExitStack() as s1:
                junk = sbt(s1, "junk", (128, D), BF16); djunk = Dep()
                if fp32_router is None:
                    xn = [sbt(s1, "xn%d" % i, (128, D), BF16) for i in range(2)]
                else:
                    xn = [sbt(s1, "xn%d" % i, (128, D), F32) for i in range(2)]
                    h2f = [sbt(s1, "h2f%d" % i, (128, 8, 128), F32) for i in range(2)]
                    dh2f = [Dep(), Dep()]
                    rws, drws, logits, dlog = fp32_router
                dxn = [Dep(), Dep()]
                op("dve", lambda e: e.memset(ssq[:], 0.0), W=[dSSQ])
                for i in range(NT):
                    op("act", lambda e: e.activation(out=junk[:], in_=xs[:, i, :], func=AF.Square, accum_out=ssq[:, i:i + 1]),
                       R=[XD[i]], W=[djunk, dSSQ])
                op("act", lambda e: e.activation(out=rstd[:], in_=ssq[:], func=AF.Ln, bias=epsc[:, 0:1], scale=1.0 / D),
                   R=[dSSQ, dCONST], W=[dRSTD])
                op("act", lambda e: e.activation(out=rstd[:], in_=rstd[:], func=AF.Exp, scale=-0.5), R=[dRSTD], W=[dRSTD])
                for i in range(NT):
                    b = i % 2
                    op("dve", lambda e: e.tensor_scalar(out=xn[b][:], in0=xs[:, i, :], scalar1=rstd[:, i:i + 1], scalar2=None,
                                                        op0=ALU.mult), R=[XD[i], dRSTD], W=[dxn[b]])
                    if fp32_router is None:
                        pb = i % 2
                        pv = PB[pb][:, :].bitcast(BF16)
                        for k in range(8):
                            op("pe", lambda e: e.transpose(out=pv[:, k * 128:(k + 1) * 128], in_=xn[b][:, k * 128:(k + 1) * 128],
                                                           identity=identb[:]), R=[dxn[b], dCONST], W=[PD[pb]], inc=(k == 7))
                        for k in range(8):
                            if i % 2 == 0:
                                op("act", lambda e: e.activation(out=hT[:, k, i * 128:(i + 1) * 128], in_=pv[:, k * 128:(k + 1) * 128],
                                                                 func=AF.Identity, scale=amod[:, ia, k:k + 1], bias=amod[:, ish, k:k + 1]),
                                   R=[PD[pb], dAMOD], W=[HDa[i]])
                            else:
                                op("dve", lambda e: e.tensor_scalar(out=hT[:, k, i * 128:(i + 1) * 128], in0=pv[:, k * 128:(k + 1) * 128],
                                                                    scalar1=amod[:, ia, k:k + 1], scalar2=amod[:, ish, k:k + 1],
                                                                    op0=ALU.mult, op1=ALU.add), R=[PD[pb], dAMOD], W=[HDb[i]])
                    else:
                        for half in range(2):
                            pb = half
                            for kk in range(4):
                                k = half * 4 + kk
                                op("pe", lambda e: e.transpose(out=PB[pb][:, kk * 128:(kk + 1) * 128], in_=xn[b][:, k * 128:(k + 1) * 128],
                                                               identity=identf[:]), R=[dxn[b], dCONST], W=[PD[pb]], inc=(kk == 3))
                            for kk in range(4):
                                k = half * 4 + kk
                                op("act", lambda e: e.activation(out=h2f[b][:, k, :], in_=PB[pb][:, kk * 128:(kk + 1) * 128],
                                                                 func=AF.Identity, scale=amod[:, ia, k:k + 1], bias=amod[:, ish, k:k + 1]),
                                   R=[PD[pb], dAMOD], W=[dh2f[b]])
                        op("dve", lambda e: e.tensor_copy(out=hT[:, :, i * 128:(i + 1) * 128], in_=h2f[b][:, :, :]), R=[dh2f[b]], W=[HDa[i], HDb[i]])
                        for k in range(8):
                            op("pe", lambda e: e.matmul(PB[2][:, 0:8], lhsT=h2f[b][:, k, :], rhs=rws[:, k, :], start=(k == 0), stop=(k == 7)),
                               R=[dh2f[b], drws], W=[PD[2]], inc=(k == 7))
                        op("dve", lambda e: e.tensor_copy(out=logits[:, i, :], in_=PB[2][:, 0:8]), R=[PD[2]], W=[dlog])
                S.barrier()

        def attention(l):
            lam_init = 0.8 - 0.6 * math.exp(-0.3 * l)
            win_v = w_in[l].rearrange("(k p) n -> p k n", p=128)
            with ExitStack() as s1:
                KT = sbt(s1, "KT", (128, 3, S_LEN), BF16); dKT = [[Dep() for _ in range(4)] for _ in range(3)]
                QT = sbt(s1, "QT", (128, 3, 2, 512), BF16); dQT = [[Dep(), Dep()] for _ in range(3)]
                V = sbt(s1, "V", (128, NT, 3, 65), BF16); dV = [Dep() for _ in range(NT)]
                OT = sbt(s1, "OT", (64, 3, 512), BF16); dOT = [Dep() for _ in range(3)]
                PT = [sbt(s1, "PT%d" % i, (128, 512), BF16) for i in range(4)]; dPT = [Dep() for _ in range(4)]
                wkv = sbt(s1, "wkv", (128, 8, 387), BF16); dwkv = Dep()
                wq = sbt(s1, "wq", (128, 8, 256), BF16); dwq = Dep()
                wo = sbt(s1, "wo", (64, 3, D), BF16); dwo = Dep()
                fa = [sbt(s1, "fa%d" % i, (128, 512), F32) for i in range(6)]; dfa = [Dep() for _ in range(6)]
                dl_b = sbt(s1, "dl_b", (128, 128), F32); ddl = Dep()
                lt = sbt(s1, "lt", (128, 8), F32)
                negb = sbt(s1, "negb", (3, 1), F32); dnegb = Dep()
                ftok = sbt(s1, "ftok", (128, NT, 3), F32); dftok = Dep()
                cb = sbt(s1, "cb", (128, NT, 3), F32); dcb = Dep()
                biask = sbt(s1, "biask", (128, 3, 4, NT), F32); dbk = Dep()
                carry = sbt(s1, "carry", (3, 1), F32); dcarry = Dep()
                ones3 = sbt(s1, "ones3", (3, 512), F32)
                gq_f = sbt(s1, "gq_f", (3, S_LEN), BF16); dgqf = Dep()
                selg = sbt(s1, "selg", (3, 3, 65), BF16)
                wukv_f = sbt(s1, "wukv_f", (128, 768), F32); wukv_b = sbt(s1, "wukv_b", (128, 768), BF16); dwukv = Dep()
                wuq_f = sbt(s1, "wuq_f", (128, 2, 576), F32); wuq_b = sbt(s1, "wuq_b", (128, 2, 576), BF16); dwuq = Dep()
                gq = sbt(s1, "gq", (128, 2), F32); gkv = sbt(s1, "gkv", (128, 1), F32); dgn = Dep()
                ckvT = sbt(s1, "ckvT", (128, 512), BF16); dckvT = Dep()
                cqT = sbt(s1, "cqT", (128, 2, 512), BF16); dcqT = Dep()
                rtok = sbt(s1, "rtok", (128, 4), F32); drtok = Dep()
                krp4 = sbt(s1, "krp4", (128, 4, 96), BF16); dkrp = Dep()
                rk = [sbt(s1, "rk%d" % i, (128, 4, 16), F32) for i in range(4)]
                qsb4 = sbt(s1, "qsb4", (128, 4, 3, 96), F32)
                qrot4 = sbt(s1, "qrot4", (128, 4, 3, 96), BF16)
                rq = [sbt(s1, "rq%d" % i, (128, 4, 3, 16), F32) for i in range(4)]
                dqsb = Dep(); dqrot = Dep(); drt = Dep()

                op("pool", lambda e: e.memset(V[:, :, :, 64:65], 1.0), W=dV)
                op("pool", lambda e: e.memset(krp4[:], 0.0), W=[dkrp])
                op("pool", lambda e: e.memset(ones3[:], 1.0), W=[dcarry])
                op("pool", lambda e: e.memset(selg[:], 0.0), W=[dgqf])
                for hl_ in range(3):
                    op("pool", lambda e: e.tensor_copy(out=selg[0:3, hl_, 64:65], in_=identf[0:3, hl_:hl_ + 1]), R=[dCONST], W=[dgqf])
                dma("sp", dl_b[:], diff_lambda[l].rearrange("a b -> (a b)").partition_broadcast(128), W=[ddl])
                op("dve", lambda e: e.tensor_tensor(out=dl_b[:, 0:32], in0=dl_b[:, 0:32], in1=dl_b[:, 32:64], op=ALU.mult), R=[ddl], W=[ddl])
                op("dve", lambda e: e.tensor_tensor(out=dl_b[:, 64:96], in0=dl_b[:, 64:96], in1=dl_b[:, 96:128], op=ALU.mult), R=[ddl], W=[ddl])
                op("dve", lambda e: e.reduce_sum(out=lt[:, 0:1], in_=dl_b[:, 0:32], axis=AX.X), R=[ddl], W=[ddl])
                op("dve", lambda e: e.reduce_sum(out=lt[:, 1:2], in_=dl_b[:, 64:96], axis=AX.X), R=[ddl], W=[ddl])
                op("act", lambda e: e.activation(out=lt[:, 2:4], in_=lt[:, 0:2], func=AF.Exp), R=[ddl], W=[ddl])
                op("dve", lambda e: e.tensor_tensor(out=lt[:, 4:5], in0=lt[:, 3:4], in1=lt[:, 2:3], op=ALU.subtract), R=[ddl], W=[ddl])
                op("dve", lambda e: e.tensor_scalar(out=neglam[:], in0=lt[:, 4:5], scalar1=-lam_init, scalar2=None, op0=ALU.add), R=[ddl], W=[dLAM])
                dma("sp", gsub[:], diff_subln[l:l + 1, :].rearrange("a d -> d a"), W=[dGSUB], allow_slow_non_contiguous=True)
                op("dve", lambda e: e.tensor_scalar(out=gsub[:], in0=gsub[:], scalar1=1.0 - lam_init, scalar2=None, op0=ALU.mult), R=[dGSUB], W=[dGSUB])
                dma("sp", gq[:], mla_q_norm[l, :].rearrange("(r p) -> p r", p=128), W=[dgn], allow_slow_non_contiguous=True)
                dma("sp", gkv[:], mla_kv_norm[l:l + 1, :].rearrange("a p -> p a"), W=[dgn], allow_slow_non_contiguous=True)

                state = {"s": 0, "p": 0, "a": 0, "f": 0, "x": 0}

                fring = [0, 6]

                def nxt(key, n, base):
                    if key == "f":
                        base, n = fring
                    v = base + state[key] % n
                    state[key] += 1
                    if key == "f":
                        ensure_free("f%d" % v)
                    elif key == "a":
                        ensure_free("a%d" % v)
                    return v

                pending = []

                def pop_pending(all_=False):
                    if all_:
                        while pending:
                            pending.pop(0)[1]()
                        return
                    if pending:
                        pending[0][0] -= 1
                        if pending[0][0] <= 0:
                            pending.pop(0)[1]()

                def ensure_free(res):
                    while any(res in p_[2] for p_ in pending if len(p_) > 2):
                        pending.pop(0)[1]()

                def normalize_to(acc_pb, dest_fn, last=False):
                    f1 = nxt("f", 6, 0)
                    if last:
                        op("act", lambda e: e.activation(out=fa[f1][64:65, :], in_=PB[acc_pb][64:65, :], func=AF.Ln), R=[PD[acc_pb]], W=[dfa[f1]])
                        op("act", lambda e: e.activation(out=fa[f1][64:65, :], in_=fa[f1][64:65, :], func=AF.Exp, scale=-1.0), R=[dfa[f1]], W=[dfa[f1]])
                    else:
                        op("dve", lambda e: e.reciprocal(out=fa[f1][64:65, :], in_=PB[acc_pb][64:65, :]), R=[PD[acc_pb]], W=[dfa[f1]])
                    f2 = nxt("f", 6, 0)

                    def stage_b():
                        op("pe", lambda e: e.matmul(PB[7][0:64, :], lhsT=onesf[64:65, 0:64], rhs=fa[f1][64:65, :], start=True, stop=True),
                           R=[dCONST, dfa[f1]], W=[PD[7]])
                        op("dve", lambda e: e.tensor_copy(out=fa[f2][0:64, :], in_=PB[7][0:64, :]), R=[PD[7]], W=[dfa[f2]])
                        dest_fn(acc_pb, f2)
                    pending.append([2 if last else 6, stage_b, {"a%d" % acc_pb, "f%d" % f1, "f%d" % f2}])

                def attn_chunk(c, maps):
                    nj = 4 * c + 4
                    seq = [(mi, j) for mi in range(len(maps)) for j in range(nj)]
                    LA = 2
                    sbank = {}
                    accs = {}

                    def qk(idx):
                        mi, j = seq[idx]
                        M = maps[mi]
                        r = j - 4 * c
                        lo = max(0, r) * 128
                        sb_ = nxt("s", 3, 2)
                        sbank[idx] = sb_
                        fx = M["fix"](r)
                        op("pe", lambda e: e.matmul(PB[sb_][:, lo:512], lhsT=M["kfn"](j), rhs=M["qfn"](lo), start=True, stop=(len(fx) == 0)),
                           R=[M["kdeps"][j // 4], M["qdep"]], W=[PD[sb_]], inc=(len(fx) == 0))
                        for n_, (rr, tile_ap) in enumerate(fx):
                            op("pe", lambda e: e.matmul(PB[sb_][:, rr * 128:(rr + 1) * 128], lhsT=identb[:, :], rhs=tile_ap,
                                                        start=False, stop=(n_ == len(fx) - 1)),
                               R=[dCONST, dMT5], W=[PD[sb_]], inc=(n_ == len(fx) - 1))
                    for idx in range(min(LA, len(seq))):
                        qk(idx)
                    for idx, (mi, j) in enumerate(seq):
                        M = maps[mi]
                        if j == 0:
                            accs[mi] = nxt("a", 2, 5)
                        acc = accs[mi]
                        r = j - 4 * c
                        lo = max(0, r) * 128
                        p = nxt("p", 4, 0)
                        M["act_fn"](j, p, sbank[idx], lo)
                        if idx + LA < len(seq):
                            qk(idx + LA)
                        op("pe", lambda e: e.matmul(PB[acc][0:65, lo:512], lhsT=V[:, j, M["hl"], :], rhs=PT[p][:, lo:512],
                                                    start=(j == 0), stop=(j == nj - 1)),
                           R=[dV[j], dPT[p]], W=[PD[acc]])
                        if j == nj - 1:
                            M["done"](acc, mi == len(maps) - 1)
                        pop_pending()

                def wout_group(c, nh, t, half):
                    pb = nxt("x", 2, 0)
                    for hl in range(nh):
                        op("pe", lambda e: e.matmul(PB[pb][:, :], lhsT=OT[0:64, hl, t * 128:(t + 1) * 128],
                                                    rhs=wo[0:64, hl, half * 512:(half + 1) * 512], start=(hl == 0), stop=(hl == nh - 1)),
                           R=[dOT[hl], dwo], W=[PD[pb]], inc=(hl == nh - 1))
                    i = 4 * c + t
                    op("dve", lambda e: e.tensor_tensor(out=xs[:, i, half * 512:(half + 1) * 512], in0=PB[pb][:, :],
                                                        in1=xs[:, i, half * 512:(half + 1) * 512], op=ALU.add),
                       R=[PD[pb]], W=[XD[i]])

                def wout_phase(c, nh):
                    for t in range(4):
                        for half in range(2):
                            if c < 3:
                                pending.append([1, lambda c=c, nh=nh, t=t, half=half: wout_group(c, nh, t, half)])
                            else:
                                wout_group(c, nh, t, half)

                def load_wo(row0, nh):
                    dma("pool", wo[0:64, 0:nh, :], w_out[l, row0:row0 + 64 * nh, :].rearrange("(h d) n -> d h n", d=64), W=[dwo])
                    for hl in range(nh):
                        op("pool", lambda e: e.tensor_tensor(out=wo[0:64, hl, :], in0=wo[0:64, hl, :], in1=gab[0:64, :], op=ALU.mult),
                           R=[dGAB], W=[dwo])

                def proj_fm(pb, wt, dw, col0, m, c):
                    for k in range(8):
                        op("pe", lambda e: e.matmul(PB[pb][0:m, :], lhsT=wt[:, k, col0:col0 + m], rhs=hT[:, k, c * 512:(c + 1) * 512],
                                                    start=(k == 0), stop=(k == 7)),
                           R=[dw] + HD[4 * c:4 * c + 4], W=[PD[pb]], inc=(k == 7))

                def proj_tm(pb, wt, dw, col0, n, i):
                    for k in range(8):
                        op("pe", lambda e: e.matmul(PB[pb][:, 0:n], lhsT=hT[:, k, i * 128:(i + 1) * 128], rhs=wt[:, k, col0:col0 + n],
                                                    start=(k == 0), stop=(k == 7)),
                           R=[dw] + HD[i:i + 1], W=[PD[pb]], inc=(k == 7))

                def evac(eng, out, in_, R, W):
                    if eng == "act":
                        op("act", lambda e: e.activation(out=out, in_=in_, func=AF.Identity), R=R, W=W)
                    else:
                        op(eng, lambda e: e.tensor_copy(out=out, in_=in_), R=R, W=W)

                def causal_fix(r):
                    return [(r, negmask[:, :])] if r >= 0 else []

                for sg in range(2):
                    heads = [2 * sg, 2 * sg + 1]
                    dma("pool", wkv[:, :, 0:128], win_v[:, :, O_DK + 128 * sg:O_DK + 128 * sg + 128], W=[dwkv])
                    dma("pool", wkv[:, :, 128:256], win_v[:, :, O_DV + 128 * sg:O_DV + 128 * sg + 128], W=[dwkv])
                    dma("pool", wq[:, :, 0:128], win_v[:, :, O_DQ + 128 * sg:O_DQ + 128 * sg + 128], W=[dwq])
                    load_wo(128 * sg, 2)
                    for c in range(4):
                        for hl in range(2):
                            pbx = nxt("x", 2, 0)
                            proj_fm(pbx, wkv, dwkv, 64 * hl, 64, c)
                            evac("dve", KT[0:64, hl, c * 512:(c + 1) * 512], PB[pbx][0:64, :], [PD[pbx]], [dKT[hl][c]])
                        for t in range(4):
                            i = 4 * c + t
                            pby = nxt("s", 3, 2)
                            proj_tm(pby, wkv, dwkv, 128, 128, i)
                            op("act", lambda e: e.activation(out=V[:, i, 0:2, 0:64], in_=PB[pby][:, 0:128].rearrange("p (h d) -> p h d", h=2), func=AF.Identity),
                               R=[PD[pby]], W=[dV[i]])
                    def qprep(c):
                        for hl in range(2):
                            pbx = nxt("x", 2, 0)
                            proj_fm(pbx, wq, dwq, 64 * hl, 64, c)
                            evac("dve", QT[0:64, hl, c % 2, :], PB[pbx][0:64, :], [PD[pbx]], [dQT[hl][c % 2]])
                            pop_pending()
                    qprep(0)
                    for c in range(4):
                        maps = []
                        for hl in range(2):
                            h = heads[hl]
                            om = []
                            for m in range(2):
                                def act_fn(j, p, sb_, lo, h=h):
                                    op("act", lambda e: e.activation(out=PT[p][:, lo:512], in_=PB[sb_][:, lo:512], func=AF.Exp,
                                                                     bias=b31[:, h:h + 1], scale=32 ** -0.5),
                                       R=[PD[sb_], dB31], W=[dPT[p]])

                                def fix_fn(r, h=h):
                                    return [(r + dlt, mt5[:, h, dlt, :]) for dlt in range(2) if 0 <= r + dlt <= 3]

                                def done(acc, last, m=m, hl=hl, om=om):
                                    fo = nxt("f", 6, 0)

                                    def dest(acc_pb, f2, fo=fo):
                                        op("dve", lambda e: e.tensor_tensor(out=fa[fo][0:64, :], in0=PB[acc_pb][0:64, :], in1=fa[f2][0:64, :],
                                                                            op=ALU.mult), R=[PD[acc_pb], dfa[f2]], W=[dfa[fo]])
                                    normalize_to(acc, dest, last)
                                    om.append(fo)
                                    if m == 0:
                                        return
                                    fo = nxt("f", 6, 0)
                                    while fo in om:
                                        fo = nxt("f", 6, 0)
                                    fs = nxt("f", 6, 0)
                                    while fs in om or fs == fo:
                                        fs = nxt("f", 6, 0)

                                    def stage_d(om=om, fo=fo, fs=fs):
                                        op("dve", lambda e: e.scalar_tensor_tensor(out=fa[fo][0:64, :], in0=fa[om[1]][0:64, :], scalar=neglam[0:64, 0:1],
                                                                                   in1=fa[om[0]][0:64, :], op0=ALU.mult, op1=ALU.add),
                                           R=[dfa[om[0]], dfa[om[1]], dLAM], W=[dfa[fo]])
                                        op("act", lambda e: e.activation(out=fa[fs][0:64, :], in_=fa[fo][0:64, :], func=AF.Square), R=[dfa[fo]], W=[dfa[fs]])

                                    def stage_e(fo=fo, fs=fs, hl=hl):
                                        op("pe", lambda e: e.matmul(PB[7][0:64, :], lhsT=onesf[0:64, 0:64], rhs=fa[fs][0:64, :], start=True, stop=True),
                                           R=[dCONST, dfa[fs]], W=[PD[7]])
                                        op("act", lambda e: e.activation(out=fa[fs][0:64, :], in_=PB[7][0:64, :], func=AF.Ln, bias=epsc[0:64, 0:1],
                                                                         scale=1.0 / 64), R=[PD[7], dCONST], W=[dfa[fs]])
                                        op("act", lambda e: e.activation(out=fa[fs][0:64, :], in_=fa[fs][0:64, :], func=AF.Exp, scale=-0.5), R=[dfa[fs]], W=[dfa[fs]])
                                        op("dve", lambda e: e.scalar_tensor_tensor(out=OT[0:64, hl, :], in0=fa[fo][0:64, :], scalar=gsub[0:64, 0:1],
                                                                                   in1=fa[fs][0:64, :], op0=ALU.mult, op1=ALU.mult),
                                           R=[dfa[fo], dfa[fs], dGSUB], W=[dOT[hl]])
                                    pending.append([1, stage_d, {"f%d" % om[0], "f%d" % om[1], "f%d" % fo, "f%d" % fs}])
                                    pending.append([4, stage_e, {"f%d" % fo, "f%d" % fs}])
                                maps.append(dict(kfn=lambda j, m=m, hl=hl: KT[32 * m:32 * m + 32, hl, j * 128:(j + 1) * 128], kdeps=dKT[hl],
                                                 qfn=lambda lo, m=m, hl=hl, c=c: QT[32 * m:32 * m + 32, hl, c % 2, lo:512], qdep=dQT[hl][c % 2],
                                                 hl=hl, act_fn=act_fn, fix=fix_fn, done=done))
                        attn_chunk(c, maps)
                        if c < 3:
                            qprep(c + 1)
                        else:
                            pop_pending(all_=True)
                        wout_phase(c, 2)
                if stage == "diff":
                    S.barrier()
                    return

                for sg in range(2):
                    h0 = 3 * sg
                    dma("pool", wkv[:, :, 0:192], win_v[:, :, O_FK + 64 * h0:O_FK + 64 * h0 + 192], W=[dwkv])
                    dma("pool", wkv[:, :, 192:384], win_v[:, :, O_FV + 64 * h0:O_FV + 64 * h0 + 192], W=[dwkv])
                    dma("pool", wkv[:, :, 384:387], win_v[:, :, O_FF + h0:O_FF + h0 + 3], W=[dwkv])
                    op("pool", lambda e: e.memset(wq[:, :, :], 0.0), W=[dwq])
                    for hl in range(3):
                        dma("pool", wq[:, :, 65 * hl:65 * hl + 64], win_v[:, :, O_FQ + 64 * (h0 + hl):O_FQ + 64 * (h0 + hl) + 64], W=[dwq])
                    load_wo(256 + 64 * h0, 3)
                    dma("sp", negb[:], b_forget[l:l + 1, h0:h0 + 3].rearrange("a h -> h a"), W=[dnegb], allow_slow_non_contiguous=True)
                    op("dve", lambda e: e.tensor_scalar(out=negb[:], in0=negb[:], scalar1=-1.0, scalar2=None, op0=ALU.mult), R=[dnegb], W=[dnegb])
                    for hl in range(3):
                        op("pool", lambda e: e.memset(KT[64:65, hl, :], 1.0), W=dKT[hl])
                    for c in range(4):
                        for hl in range(3):
                            pbx = nxt("x", 2, 0)
                            proj_fm(pbx, wkv, dwkv, 64 * hl, 64, c)
                            evac("dve", KT[0:64, hl, c * 512:(c + 1) * 512], PB[pbx][0:64, :], [PD[pbx]], [dKT[hl][c]])
                        for t in range(4):
                            i = 4 * c + t
                            pby = nxt("s", 3, 2)
                            proj_tm(pby, wkv, dwkv, 192, 192, i)
                            op("act", lambda e: e.activation(out=V[:, i, 0:3, 0:64], in_=PB[pby][:, 0:192].rearrange("p (h d) -> p h d", h=3), func=AF.Identity),
                               R=[PD[pby]], W=[dV[i]])
                        proj_fm(0, wkv, dwkv, 384, 3, c)
                        op("act", lambda e: e.activation(out=fa[0][0:3, :], in_=PB[0][0:3, :], func=AF.Exp, bias=negb[:, 0:1], scale=-1.0),
                           R=[PD[0], dnegb], W=[dfa[0]])
                        op("act", lambda e: e.activation(out=fa[1][0:3, :], in_=fa[0][0:3, :], func=AF.Ln, bias=1.0), R=[dfa[0]], W=[dfa[1]])
                        if c == 0:
                            op("dve", lambda e: e.tensor_tensor_scan(out=fa[2][0:3, :], data0=ones3[:, :], data1=fa[1][0:3, :], initial=0.0,
                                                                     op0=ALU.mult, op1=ALU.add), R=[dfa[1], dcarry], W=[dfa[2]])
                        else:
                            op("dve", lambda e: e.tensor_tensor_scan(out=fa[2][0:3, :], data0=ones3[:, :], data1=fa[1][0:3, :],
                                                                     initial=carry[:, 0:1], op0=ALU.mult, op1=ALU.add),
                               R=[dfa[1], dcarry], W=[dfa[2]])
                        op("dve", lambda e: e.tensor_copy(out=carry[:, 0:1], in_=fa[2][0:3, 511:512]), R=[dfa[2]], W=[dcarry])
                        op("dve", lambda e: e.tensor_scalar(out=gq_f[0:3, c * 512:(c + 1) * 512], in0=fa[2][0:3, :], scalar1=fa[2][0:3, 0:1], scalar2=-8.0,
                                                            op0=ALU.subtract, op1=ALU.mult), R=[dfa[2]], W=[dgqf])
                        for t in range(4):
                            op("pe", lambda e: e.transpose(out=PB[1][:, 3 * t:3 * t + 3], in_=fa[2][0:3, t * 128:(t + 1) * 128],
                                                           identity=identf[0:3, 0:3]), R=[dfa[2], dCONST], W=[PD[1]], inc=(t == 3))
                        op("dve", lambda e: e.tensor_copy(out=ftok[:, 4 * c:4 * c + 4, :], in_=PB[1][:, 0:12].rearrange("p (t h) -> p t h", h=3)),
                           R=[PD[1]], W=[dftok])
                    op("pe", lambda e: e.matmul(PB[0][:, 0:48], lhsT=onesf[0:1, 0:128], rhs=ftok[0:1, :, :].rearrange("p t h -> p (t h)"),
                                                start=True, stop=True), R=[dCONST, dftok], W=[PD[0]])
                    op("dve", lambda e: e.tensor_copy(out=cb[:, :, :], in_=PB[0][:, 0:48].rearrange("p (t h) -> p t h", h=3)), R=[PD[0]], W=[dcb])
                    for hl in range(3):
                        for c in range(4):
                            op("dve", lambda e: e.tensor_scalar(out=biask[:, hl, c, 0:4 * c + 4], in0=ftok[:, 0:4 * c + 4, hl], scalar1=cb[:, 4 * c, hl:hl + 1],
                                                                scalar2=None, op0=ALU.subtract), R=[dftok, dcb], W=[dbk])
                    fring[:] = [2, 4]

                    def qprep(c):
                        for hl in range(3):
                            pbx = nxt("x", 2, 0)
                            for k in range(8):
                                op("pe", lambda e: e.matmul(PB[pbx][0:65, :], lhsT=wq[:, k, 65 * hl:65 * hl + 65], rhs=hT[:, k, c * 512:(c + 1) * 512],
                                                            start=(k == 0), stop=False),
                                   R=[dwq] + HD[4 * c:4 * c + 4], W=[PD[pbx]], inc=False)
                            op("pe", lambda e: e.matmul(PB[pbx][0:65, :], lhsT=selg[0:3, hl, :], rhs=gq_f[0:3, c * 512:(c + 1) * 512], start=False, stop=True),
                               R=[dgqf], W=[PD[pbx]])
                            evac("dve", QT[0:65, hl, c % 2, :], PB[pbx][0:65, :], [PD[pbx]], [dQT[hl][c % 2]])
                            pop_pending()
                    qprep(0)
                    for c in range(4):
                        maps = []
                        for hl in range(3):
                            def act_fn(j, p, sb_, lo, hl=hl, c=c):
                                op("act", lambda e: e.activation(out=PT[p][:, lo:512], in_=PB[sb_][:, lo:512],
                                                                 func=AF.Exp, bias=biask[:, hl, c, j:j + 1], scale=64 ** -0.5),
                                   R=[PD[sb_], dbk], W=[dPT[p]])

                            def dest(acc_pb, f2, hl=hl):
                                op("dve", lambda e: e.tensor_tensor(out=OT[0:64, hl, :], in0=PB[acc_pb][0:64, :], in1=fa[f2][0:64, :],
                                                                    op=ALU.mult), R=[PD[acc_pb], dfa[f2]], W=[dOT[hl]])
                            maps.append(dict(kfn=lambda j, hl=hl: KT[0:65, hl, j * 128:(j + 1) * 128], kdeps=dKT[hl],
                                             qfn=lambda lo, hl=hl, c=c: QT[0:65, hl, c % 2, lo:512], qdep=dQT[hl][c % 2],
                                             hl=hl, act_fn=act_fn, fix=causal_fix, done=lambda acc, last, dest=dest: normalize_to(acc, dest, last)))
                        attn_chunk(c, maps)
                        if c < 3:
                            qprep(c + 1)
                        else:
                            pop_pending(all_=True)
                        wout_phase(c, 3)
                if stage == "fox":
                    S.barrier()
                    return

                dma("sp", wukv_f[:], w_ukv[l], W=[dwukv])
                op("pool", lambda e: e.tensor_scalar(out=wukv_b[:], in0=wukv_f[:], scalar1=gkv[:, 0:1], scalar2=None, op0=ALU.mult),
                   R=[dgn], W=[dwukv])
                dma("sp", wuq_f[:], w_uq[l].rearrange("(r p) n -> p r n", p=128), W=[dwuq])
                for r_ in range(2):
                    op("pool", lambda e: e.tensor_scalar(out=wuq_b[:, r_, :], in0=wuq_f[:, r_, :], scalar1=gq[:, r_:r_ + 1], scalar2=None,
                                                         op0=ALU.mult), R=[dgn], W=[dwuq])
                for sg in range(2):
                    h0 = 3 * sg
                    dma("pool", wkv[:, :, 0:160], win_v[:, :, O_CKV:O_CKV + 160], W=[dwkv])
                    dma("pool", wq[:, :, 0:256], win_v[:, :, O_CQ:O_CQ + 256], W=[dwq])
                    load_wo(640 + 64 * h0, 3)
                    for c in range(4):
                        proj_fm(0, wkv, dwkv, 0, 128, c)
                        evac("dve", ckvT[:, :], PB[0][:, :], [PD[0]], [dckvT])
                        op("act", lambda e: e.activation(out=fa[0][:, :], in_=PB[0][:, :], func=AF.Square), R=[PD[0]], W=[dfa[0]])
                        op("pe", lambda e: e.matmul(PB[1][:, :], lhsT=onesf[:, :], rhs=fa[0][:, :], start=True, stop=True),
                           R=[dCONST, dfa[0]], W=[PD[1]])
                        op("act", lambda e: e.activation(out=fa[1][:, :], in_=PB[1][:, :], func=AF.Ln, bias=epsc[:, 0:1], scale=1.0 / 128),
                           R=[PD[1], dCONST], W=[dfa[1]])
                        op("act", lambda e: e.activation(out=fa[1][:, :], in_=fa[1][:, :], func=AF.Exp, scale=-0.5), R=[dfa[1]], W=[dfa[1]])
                        for t in range(4):
                            op("pe", lambda e: e.matmul(PB[1][:, t:t + 1], lhsT=fa[0][:, t * 128:(t + 1) * 128], rhs=onesf[:, 0:1],
                                                        start=True, stop=True), R=[dCONST, dfa[0]], W=[PD[1]], inc=(t == 3))
                        op("act", lambda e: e.activation(out=rtok[:, :], in_=PB[1][:, 0:4], func=AF.Ln, bias=epsc[:, 0:1], scale=1.0 / 128),
                           R=[PD[1], dCONST], W=[drtok])
                        op("act", lambda e: e.activation(out=rtok[:, :], in_=rtok[:, :], func=AF.Exp, scale=-0.5), R=[drtok], W=[drtok])
                        for hl in range(3):
                            h = h0 + hl
                            op("pe", lambda e: e.matmul(PB[0][0:64, :], lhsT=wukv_b[:, h * 128:h * 128 + 64], rhs=ckvT[:, :], start=True, stop=True),
                               R=[dwukv, dckvT], W=[PD[0]])
                            op("dve", lambda e: e.tensor_tensor(out=KT[0:64, hl, c * 512:(c + 1) * 512], in0=PB[0][0:64, :], in1=fa[1][0:64, :],
                                                                op=ALU.mult), R=[PD[0], dfa[1]], W=[dKT[hl][c]])
                        for t in range(4):
                            i = 4 * c + t
                            wv3 = wukv_b[:, h0 * 128:(h0 + 3) * 128].rearrange("p (h d) -> p h d", h=3)[:, :, 64:128]
                            op("pe", lambda e: e.matmul(PB[0][:, 0:192].rearrange("p (h d) -> p h d", h=3), lhsT=ckvT[:, t * 128:(t + 1) * 128], rhs=wv3,
                                                        start=True, stop=True), R=[dwukv, dckvT], W=[PD[0]])
                            op("dve", lambda e: e.tensor_scalar(out=V[:, i, 0:3, 0:64], in0=PB[0][:, 0:192].rearrange("p (h d) -> p h d", h=3),
                                                                scalar1=rtok[:, t:t + 1], scalar2=None, op0=ALU.mult),
                               R=[PD[0], drtok], W=[dV[i]])
                            for k in range(8):
                                op("pe", lambda e: e.matmul(PB[1][:, 32 * t:32 * t + 32], lhsT=hT[:, k, i * 128:(i + 1) * 128], rhs=wkv[:, k, 128:160],
                                                            start=(k == 0), stop=(k == 7)),
                                   R=[dwkv] + HD[i:i + 1], W=[PD[1]], inc=(k == 7))
                        krv = PB[1][:, 0:128].rearrange("p (t d) -> p t d", t=4)
                        cos4, sin4 = cs[:, 4 * c:4 * c + 4, 0:16], cs[:, 4 * c:4 * c + 4, 16:32]
                        op("dve", lambda e: e.tensor_tensor(out=rk[0][:, :, :], in0=krv[:, :, 0:16], in1=cos4, op=ALU.mult), R=[PD[1], dCS], W=[drt])
                        op("dve", lambda e: e.tensor_tensor(out=rk[1][:, :, :], in0=krv[:, :, 16:32], in1=sin4, op=ALU.mult), R=[PD[1], dCS], W=[drt])
                        op("dve", lambda e: e.tensor_tensor(out=rk[2][:, :, :], in0=krv[:, :, 0:16], in1=sin4, op=ALU.mult), R=[PD[1], dCS], W=[drt])
                        op("dve", lambda e: e.tensor_tensor(out=rk[3][:, :, :], in0=krv[:, :, 16:32], in1=cos4, op=ALU.mult), R=[PD[1], dCS], W=[drt])
                        op("dve", lambda e: e.tensor_tensor(out=krp4[:, :, 64:80], in0=rk[0][:, :, :], in1=rk[1][:, :, :], op=ALU.subtract), R=[drt], W=[dkrp])
                        op("dve", lambda e: e.tensor_tensor(out=krp4[:, :, 80:96], in0=rk[2][:, :, :], in1=rk[3][:, :, :], op=ALU.add), R=[drt], W=[dkrp])
                        pv = PB[7][:, :].bitcast(BF16)
                        for t in range(4):
                            op("pe", lambda e: e.transpose(out=pv[0:96, t * 128:(t + 1) * 128], in_=krp4[:, t, :], identity=identb[:]),
                               R=[dkrp, dCONST], W=[PD[7]], inc=(t == 3))
                        pv = PB[7][:, :].bitcast(BF16)
                        for hl in range(3):
                            evac("dve", KT[64:96, hl, c * 512:(c + 1) * 512], pv[64:96, 0:512], [PD[7]], [dKT[hl][c]])
                    def qprep(c):
                        for r_ in range(2):
                            proj_fm(r_, wq, dwq, 128 * r_, 128, c)
                            evac("dve", cqT[:, r_, :], PB[r_][:, :], [PD[r_]], [dcqT])
                            op("act", lambda e: e.activation(out=fa[r_][:, :], in_=PB[r_][:, :], func=AF.Square), R=[PD[r_]], W=[dfa[r_]])
                        for t in range(4):
                            for r_ in range(2):
                                op("pe", lambda e: e.matmul(PB[1][:, t:t + 1], lhsT=fa[r_][:, t * 128:(t + 1) * 128], rhs=onesf[:, 0:1],
                                                            start=(r_ == 0), stop=(r_ == 1)), R=[dCONST, dfa[r_]], W=[PD[1]], inc=(t == 3 and r_ == 1))
                        op("act", lambda e: e.activation(out=rtok[:, :], in_=PB[1][:, 0:4], func=AF.Ln, bias=epsc[:, 0:1], scale=1.0 / 256),
                           R=[PD[1], dCONST], W=[drtok])
                        op("act", lambda e: e.activation(out=rtok[:, :], in_=rtok[:, :], func=AF.Exp, scale=-0.5), R=[drtok], W=[drtok])
                        pq = [PB[0][:, :].bitcast(BF16), PB[1][:, :].bitcast(BF16), PB[7][:, :].bitcast(BF16)]
                        pqd = [PD[0], PD[1], PD[7]]
                        for t in range(4):
                            sb_ = nxt("s", 3, 2)
                            for r_ in range(2):
                                op("pe", lambda e: e.matmul(PB[sb_][:, 0:288], lhsT=cqT[:, r_, t * 128:(t + 1) * 128], rhs=wuq_b[:, r_, h0 * 96:(h0 + 3) * 96],
                                                            start=(r_ == 0), stop=(r_ == 1)), R=[dcqT, dwuq], W=[PD[sb_]], inc=(r_ == 1))
                            op("act", lambda e: e.activation(out=qsb4[:, t, :, :], in_=PB[sb_][:, 0:288].rearrange("p (h d) -> p h d", h=3), func=AF.Identity,
                                                             scale=rtok[:, t:t + 1]), R=[PD[sb_], drtok], W=[dqsb])

                        def b43(a_):
                            l_ = [list(v) for v in a_.ap]
                            return bass.AP(tensor=a_.tensor, offset=a_.offset, ap=[l_[0], l_[1], [0, 3], l_[2]])
                        cos43, sin43 = b43(cs[:, 4 * c:4 * c + 4, 0:16]), b43(cs[:, 4 * c:4 * c + 4, 16:32])
                        op("pool", lambda e: e.tensor_copy(out=qrot4[:, :, :, 0:64], in_=qsb4[:, :, :, 0:64]), R=[dqsb], W=[dqrot])
                        op("dve", lambda e: e.tensor_tensor(out=rq[0][:], in0=qsb4[:, :, :, 64:80], in1=cos43, op=ALU.mult), R=[dqsb, dCS], W=[drt])
                        op("dve", lambda e: e.tensor_tensor(out=rq[1][:], in0=qsb4[:, :, :, 80:96], in1=sin43, op=ALU.mult), R=[dqsb, dCS], W=[drt])
                        op("dve", lambda e: e.tensor_tensor(out=rq[2][:], in0=qsb4[:, :, :, 64:80], in1=sin43, op=ALU.mult), R=[dqsb, dCS], W=[drt])
                        op("dve", lambda e: e.tensor_tensor(out=rq[3][:], in0=qsb4[:, :, :, 80:96], in1=cos43, op=ALU.mult), R=[dqsb, dCS], W=[drt])
                        op("dve", lambda e: e.tensor_tensor(out=qrot4[:, :, :, 64:80], in0=rq[0][:], in1=rq[1][:], op=ALU.subtract), R=[drt], W=[dqrot])
                        op("dve", lambda e: e.tensor_tensor(out=qrot4[:, :, :, 80:96], in0=rq[2][:], in1=rq[3][:], op=ALU.add), R=[drt], W=[dqrot])
                        for t in range(4):
                            for hl in range(3):
                                op("pe", lambda e: e.transpose(out=pq[hl][0:96, t * 128:(t + 1) * 128], in_=qrot4[:, t, hl, :], identity=identb[:]),
                                   R=[dqrot, dCONST], W=[pqd[hl]])
                        for hl in range(3):
                            evac("act" if hl % 2 == 0 else "dve", QT[0:96, hl, c % 2, :], pq[hl][0:96, 0:512], [pqd[hl]], [dQT[hl][c % 2]])
                        pop_pending()
                    qprep(0)
                    for c in range(4):
                        maps = []
                        for hl in range(3):
                            def act_fn(j, p, sb_, lo):
                                op("act", lambda e: e.activation(out=PT[p][:, lo:512], in_=PB[sb_][:, lo:512], func=AF.Exp, scale=96 ** -0.5),
                                   R=[PD[sb_]], W=[dPT[p]])

                            def dest(acc_pb, f2, hl=hl):
                                op("dve", lambda e: e.tensor_tensor(out=OT[0:64, hl, :], in0=PB[acc_pb][0:64, :], in1=fa[f2][0:64, :],
                                                                    op=ALU.mult), R=[PD[acc_pb], dfa[f2]], W=[dOT[hl]])
                            maps.append(dict(kfn=lambda j, hl=hl: KT[0:96, hl, j * 128:(j + 1) * 128], kdeps=dKT[hl],
                                             qfn=lambda lo, hl=hl, c=c: QT[0:96, hl, c % 2, lo:512], qdep=dQT[hl][c % 2],
                                             hl=hl, act_fn=act_fn, fix=causal_fix, done=lambda acc, last, dest=dest: normalize_to(acc, dest, last)))
                        attn_chunk(c, maps)
                        if c < 3:
                            qprep(c + 1)
                        else:
                            pop_pending(all_=True)
                        wout_phase(c, 3)
                S.barrier()

        def ffn(l):
            moe = (l % 2 == 1)
            with ExitStack() as s1:
                if moe:
                    rws = sbt(s1, "rws", (128, 8, NEXP), F32); drws = Dep()
                    logits = sbt(s1, "logits", (128, NT, NEXP), F32); dlog = Dep()
                    comb = sbt(s1, "comb", (128, NT, NEXP), F32); dcomb = Dep()
                    t8 = sbt(s1, "t8", (128, 8), F32); dt8 = Dep()
                    sel = sbt(s1, "sel", (128, 8), F32); ex = sbt(s1, "ex", (128, 8), F32); den = sbt(s1, "den", (128, 2), F32)
                    dma("sp", rws[:], router_w[0].rearrange("(k p) e -> p k e", p=128), W=[drws])
                    norm_phase(s1, 1, fp32_router=(rws, drws, logits, dlog))
                    for i in range(NT):
                        op("dve", lambda e: e.max(out=t8[:], in_=logits[:, i, :]), R=[dlog], W=[dt8])
                        op("dve", lambda e: e.tensor_scalar(out=sel[:], in0=logits[:, i, :], scalar1=t8[:, 1:2], scalar2=None, op0=ALU.is_ge),
                           R=[dlog, dt8], W=[dt8])
                        op("dve", lambda e: e.tensor_scalar(out=den[:, 0:1], in0=t8[:, 0:1], scalar1=-1.0, scalar2=None, op0=ALU.mult), R=[dt8], W=[dt8])
                        op("act", lambda e: e.activation(out=ex[:], in_=logits[:, i, :], func=AF.Exp, bias=den[:, 0:1], scale=1.0), R=[dlog, dt8], W=[dt8])
                        op("dve", lambda e: e.tensor_tensor(out=ex[:], in0=ex[:], in1=sel[:], op=ALU.mult), R=[dt8], W=[dt8])
                        op("dve", lambda e: e.reduce_sum(out=den[:, 1:2], in_=ex[:], axis=AX.X), R=[dt8], W=[dt8])
                        op("dve", lambda e: e.reciprocal(out=den[:, 1:2], in_=den[:, 1:2]), R=[dt8], W=[dt8])
                        op("dve", lambda e: e.tensor_scalar(out=comb[:, i, :], in0=ex[:], scalar1=den[:, 1:2], scalar2=None, op0=ALU.mult),
                           R=[dt8], W=[dcomb])
                else:
                    norm_phase(s1, 1)
                G = 4 if moe else 2
                GW = 128 * G
                wg = [sbt(s1, "wg%d" % i, (128, 8, GW), BF16) for i in range(2)]
                wu = [sbt(s1, "wu%d" % i, (128, 8, GW), BF16) for i in range(2)]
                wdf = [sbt(s1, "wdf%d" % i, (128, G, D), F32) for i in range(2)]
                wd = [sbt(s1, "wd%d" % i, (128, G, D), BF16) for i in range(2)]
                dwg = [Dep(), Dep()]; dwu = [Dep(), Dep()]; dwdf = [Dep(), Dep()]; dwd = [Dep(), Dep()]
                sg_ = [sbt(s1, "sg%d" % i, (128, 256), F32) for i in range(2)]; dsg = [Dep(), Dep()]
                aT = [sbt(s1, "aT%d" % i, (128, G, 256), BF16) for i in range(2)]; daT = [Dep(), Dep()]
                if moe:
                    groups = [(e_, g_) for e_ in range(NEXP) for g_ in range(DFE // GW)]
                else:
                    groups = [(None, g_) for g_ in range(DFF // GW)]
                cnt = 0
                prev_down = [None]
                for gi, (e_, g_) in enumerate(groups):
                    b = gi % 2
                    if moe:
                        gsrc = moe_w_gate[0, e_].rearrange("(k p) n -> p k n", p=128)[:, :, g_ * GW:(g_ + 1) * GW]
                        usrc = moe_w_up[0, e_].rearrange("(k p) n -> p k n", p=128)[:, :, g_ * GW:(g_ + 1) * GW]
                        dsrc = moe_w_down[0, e_, g_ * GW:(g_ + 1) * GW, :].rearrange("(j p) n -> p j n", p=128)
                    else:
                        gsrc = ffn_w_gate[0].rearrange("(k p) n -> p k n", p=128)[:, :, g_ * GW:(g_ + 1) * GW]
                        usrc = ffn_w_up[0].rearrange("(k p) n -> p k n", p=128)[:, :, g_ * GW:(g_ + 1) * GW]
                        dsrc = ffn_w_down[0, g_ * GW:(g_ + 1) * GW, :].rearrange("(j p) n -> p j n", p=128)
                    dma("pool", wg[b][:], gsrc, W=[dwg[b]])
                    dma("pool", wu[b][:], usrc, W=[dwu[b]])
                    dma("sp", wdf[b][:], dsrc, W=[dwdf[b]])
                    for j in range(G):
                        op("pool", lambda e: e.tensor_tensor(out=wd[b][:, j, :], in0=wdf[b][:, j, :], in1=gfb[:, :], op=ALU.mult),
                           R=[dwdf[b], dGFB], W=[dwd[b]])
                    for T in range(8):
                        ab = cnt % 2
                        cnt += 1
                        for j in range(G):
                            pg, pu = 2 * (j % 2), 2 * (j % 2) + 1
                            for k in range(8):
                                op("pe", lambda e: e.matmul(PB[pg][:, 0:256], lhsT=wg[b][:, k, j * 128:(j + 1) * 128], rhs=hT[:, k, T * 256:(T + 1) * 256],
                                                            start=(k == 0), stop=(k == 7)), R=[dwg[b]] + HD[2 * T:2 * T + 2], W=[PD[pg]], inc=(k == 7))
                            for k in range(8):
                                op("pe", lambda e: e.matmul(PB[pu][:, 0:256], lhsT=wu[b][:, k, j * 128:(j + 1) * 128], rhs=hT[:, k, T * 256:(T + 1) * 256],
                                                            start=(k == 0), stop=(k == 7)), R=[dwu[b]] + HD[2 * T:2 * T + 2], W=[PD[pu]], inc=(k == 7))
                            op("act", lambda e: e.activation(out=sg_[j % 2][:, :], in_=PB[pg][:, 0:256], func=AF.Silu), R=[PD[pg]], W=[dsg[j % 2]])
                            op("dve", lambda e: e.tensor_tensor(out=aT[ab][:, j, :], in0=PB[pu][:, 0:256], in1=sg_[j % 2][:, :], op=ALU.mult),
                               R=[PD[pu], dsg[j % 2]], W=[daT[ab]])
                            if j == 0 and prev_down[0] is not None:
                                prev_down[0]()
                                prev_down[0] = None

                        def down(T=T, ab=ab, b=b, e_=e_):
                            for t in range(2):
                                for half in range(2):
                                    pa = 4 + 2 * t + half
                                    i = 2 * T + t
                                    for j in range(G):
                                        op("pe", lambda e: e.matmul(PB[pa][:, :], lhsT=aT[ab][:, j, t * 128:(t + 1) * 128], rhs=wd[b][:, j, half * 512:(half + 1) * 512],
                                                                    start=(j == 0), stop=(j == G - 1)), R=[daT[ab], dwd[b]], W=[PD[pa]], inc=(j == G - 1))
                                    xsl = xs[:, i, half * 512:(half + 1) * 512]
                                    if moe:
                                        op("dve", lambda e: e.scalar_tensor_tensor(out=xsl, in0=PB[pa][:, :], scalar=comb[:, i, e_:e_ + 1], in1=xsl,
                                                                                   op0=ALU.mult, op1=ALU.add), R=[PD[pa], dcomb], W=[XD[i]])
                                    else:
                                        op("dve", lambda e: e.tensor_tensor(out=xsl, in0=PB[pa][:, :], in1=xsl, op=ALU.add), R=[PD[pa]], W=[XD[i]])
                        prev_down[0] = down
                if prev_down[0] is not None:
                    prev_down[0]()
                    prev_down[0] = None
                S.barrier()

        def final():
            with ExitStack() as s1:
                fnb = sbt(s1, "fnb", (128, D), F32); dfnb = Dep()
                junk = sbt(s1, "junkf", (128, D), BF16); djunk = Dep()
                dY = Dep()
                if stage == "full":
                    dma("sp", fnb[:], final_norm.partition_broadcast(128), W=[dfnb])
                    op("dve", lambda e: e.memset(ssq[:], 0.0), W=[dSSQ])
                    for i in range(NT):
                        op("act", lambda e: e.activation(out=junk[:], in_=xs[:, i, :], func=AF.Square, accum_out=ssq[:, i:i + 1]),
                           R=[XD[i]], W=[djunk, dSSQ])
                    op("act", lambda e: e.activation(out=rstd[:], in_=ssq[:], func=AF.Ln, bias=epsc[:, 0:1], scale=1.0 / D),
                       R=[dSSQ, dCONST], W=[dRSTD])
                    op("act", lambda e: e.activation(out=rstd[:], in_=rstd[:], func=AF.Exp, scale=-0.5), R=[dRSTD], W=[dRSTD])
                    for i in range(NT):
                        op("dve", lambda e: e.scalar_tensor_tensor(out=xs[:, i, :], in0=xs[:, i, :], scalar=rstd[:, i:i + 1], in1=fnb[:, :],
                                                                   op0=ALU.mult, op1=ALU.mult), R=[XD[i], dRSTD, dfnb], W=[XD[i]])
                yv = y_d.rearrange("(i p) f -> p i f", p=128)
                for q in range(4):
                    dma("sp", yv[:, 4 * q:4 * q + 4, :], xs[:, 4 * q:4 * q + 4, :], R=XD[4 * q:4 * q + 4], W=[dY])
                S.barrier()

        stop = False
        for l in range(DEPTH):
            if stage == "setup":
                break
            adaln(l)
            if stage == "adaln":
                break
            with ExitStack() as sn:
                norm_phase(sn, 0)
            if stage == "norm":
                break
            attention(l)
            if stage in ("diff", "fox", "attn0"):
                break
            ffn(l)
            if stage == "layer0":
                break
        final()
        print("instructions:", S.ninst)
    return nc


_CACHE = {}


def kernel(**inputs):
    stage = inputs.pop("_stage", "full")
    ncores = inputs.pop("_ncores", 8)
    if stage not in _CACHE:
        _CACHE[stage] = build(stage)
    nc = _CACHE[stage]
    consts = host_consts()
    shared = {k: np.ascontiguousarray(np.asarray(v, dtype=np.float32)) for k, v in inputs.items() if k not in ("x", "c")}
    shared.update(consts)
    x = np.asarray(inputs["x"], dtype=np.float32)
    c = np.asarray(inputs["c"], dtype=np.float32)
    in_maps = []
    for b in range(ncores):
        m = dict(shared)
        m["x"] = np.ascontiguousarray(x[b])
        m["c"] = np.ascontiguousarray(c[b])
        in_maps.append(m)
    res = run_bass_kernel_spmd(nc, in_maps, core_ids=list(range(ncores)))
    out = np.stack([np.asarray(r["y"], dtype=np.float32) for r in res.results], axis=0)
    return out
```
